# Optimizing a Trainium2 kernel written in Bass

```python
import math
import jax, jax.numpy as jnp
from jax import lax
import numpy as np

D_MODEL = 4096
BATCH = 1
SEQ = 8192
DEPTH = 1

HEAD_DIM = 128
D_MIX = D_MODEL
DIFF_WIDTH = D_MIX // 2
DIL_WIDTH = D_MIX - DIFF_WIDTH
DIFF_HEADS = DIFF_WIDTH // (2 * HEAD_DIM)
DIL_HEADS = DIL_WIDTH // HEAD_DIM
IN_COLS = 3 * DIFF_WIDTH + 3 * DIL_WIDTH
ROT_DIM = HEAD_DIM // 4
ROPE_THETA = 500000.0
DILATED_CONFIGS = ((128, 1), (512, 4), (2048, 16))
BLK = 128
PEER_HEADS = 8
N_KEYS = 128
N_EXPERTS = N_KEYS * N_KEYS
PEER_QDIM = 256
PEER_TOPK = 16
PEER_BLK = 32
NORM_EPS = 1e-6
NEG_INF = -1e30

kernel_name = "hymba_diffattn_longnet_peer_adaln"


def rmsnorm(x, g):
    xf = x.astype(jnp.float32)
    y = xf * lax.rsqrt(jnp.mean(xf * xf, axis=-1, keepdims=True) + NORM_EPS)
    return (y * g.astype(jnp.float32)).astype(x.dtype)


def modulate(h, shift, scale):
    return h * (1 + scale[:, None, :]) + shift[:, None, :]


def rotary_tables(positions):
    inv_freq = jnp.power(ROPE_THETA, -jnp.arange(0, ROT_DIM, 2, dtype=jnp.float32) / ROT_DIM)
    ang = positions.astype(jnp.float32)[..., None] * inv_freq
    return jnp.cos(ang)[:, :, None, :], jnp.sin(ang)[:, :, None, :]


def apply_partial_rotary(t, cos, sin):
    half = ROT_DIM // 2
    t1 = t[..., :half].astype(jnp.float32)
    t2 = t[..., half:ROT_DIM].astype(jnp.float32)
    rot = jnp.concatenate([t1 * cos - t2 * sin, t2 * cos + t1 * sin], axis=-1).astype(t.dtype)
    return jnp.concatenate([rot, t[..., ROT_DIM:]], axis=-1)


def diff_attention(q1, q2, k1, k2, v, lam, lam_init, subln_g):
    B, S, H, dh = q1.shape
    nqb = S // BLK
    scale = dh ** -0.5
    kpos = jnp.arange(S)

    def qblock(args):
        b, qb1, qb2 = args
        qpos = b * BLK + jnp.arange(BLK)
        mask = kpos[None, :] <= qpos[:, None]

        def probs(qb, k):
            s = jnp.einsum('bqhd,bkhd->bhqk', qb, k, preferred_element_type=jnp.float32) * scale
            return jax.nn.softmax(jnp.where(mask, s, NEG_INF), axis=-1)

        a = probs(qb1, k1) - lam * probs(qb2, k2)
        return jnp.einsum('bhqk,bkhe->bqhe', a.astype(v.dtype), v)

    blocks = lambda t: t.reshape(B, nqb, BLK, H, t.shape[-1]).transpose(1, 0, 2, 3, 4)
    out = lax.map(qblock, (jnp.arange(nqb), blocks(q1), blocks(q2)))
    out = out.transpose(1, 0, 2, 3, 4).reshape(B, S, H, 2 * dh)
    return rmsnorm(out, subln_g) * (1 - lam_init)


def dilated_branch(q, k, v, window, dilation):
    B, S, H, dh = q.shape
    reach = window // dilation
    L = S // dilation
    nb = -(-L // BLK)
    Lp = nb * BLK

    def to_sub(t):
        t = t.reshape(B, L, dilation, H, dh).transpose(0, 2, 1, 3, 4)
        return jnp.pad(t, ((0, 0), (0, 0), (0, Lp - L), (0, 0), (0, 0)))

    blk = lambda u: u.reshape(B, dilation, nb, BLK, H, dh)

    def with_prev(t):
        prev = jnp.pad(t, ((0, 0), (0, 0), (BLK, 0), (0, 0), (0, 0)))[:, :, :Lp]
        return jnp.concatenate([blk(prev), blk(t)], axis=3)

    qs, ks, vs = to_sub(q), to_sub(k), to_sub(v)
    qb, kb, vb = blk(qs), with_prev(ks), with_prev(vs)
    qi = jnp.arange(BLK)[:, None]
    kj = jnp.arange(2 * BLK)[None, :]
    dist = qi + BLK - kj
    band = (dist >= 0) & (dist <= reach)
    has_prev = (jnp.arange(nb) > 0)[:, None, None]
    mask = band[None] & (has_prev | (kj >= BLK)[None])
    s = jnp.einsum('brnqhd,brnkhd->brnhqk', qb, kb, preferred_element_type=jnp.float32) * (dh ** -0.5)
    s = jnp.where(mask[:, None], s, NEG_INF)
    m = jnp.max(s, axis=-1, keepdims=True)
    p = jnp.exp(s - m)
    l = jnp.sum(p, axis=-1, keepdims=True)
    o = jnp.einsum('brnhqk,brnkhd->brnqhd', (p / l).astype(v.dtype), vb)
    lse = (m + jnp.log(l))[..., 0]
    o = o.reshape(B, dilation, Lp, H, dh)[:, :, :L].transpose(0, 2, 1, 3, 4).reshape(B, S, H, dh)
    lse = lse.transpose(0, 1, 2, 4, 3).reshape(B, dilation, Lp, H)[:, :, :L]
    lse = lse.transpose(0, 2, 1, 3).reshape(B, S, H)
    return o, lse


def dilated_attention(q, k, v):
    outs, lses = zip(*[dilated_branch(q, k, v, w, d) for (w, d) in DILATED_CONFIGS])
    wts = jax.nn.softmax(jnp.stack(lses, axis=0), axis=0)
    return jnp.einsum('gbsh,gbshd->bshd', wts.astype(q.dtype), jnp.stack(outs, axis=0))


def hybrid_mixer(h, cos, sin, w_in, w_out, lam_q1, lam_k1, lam_q2, lam_k2,
                 diff_subln_g, dil_out_g, lam_init):
    B, S, _ = h.shape
    proj = jnp.einsum('bsd,de->bse', h, w_in)
    cuts = [DIFF_WIDTH, 2 * DIFF_WIDTH, 3 * DIFF_WIDTH,
            3 * DIFF_WIDTH + DIL_WIDTH, 3 * DIFF_WIDTH + 2 * DIL_WIDTH]
    dq, dk, dv, sq, sk, sv = jnp.split(proj, cuts, axis=-1)
    rope = lambda t: apply_partial_rotary(t, cos, sin)
    dq = dq.reshape(B, S, DIFF_HEADS, 2, HEAD_DIM)
    dk = dk.reshape(B, S, DIFF_HEADS, 2, HEAD_DIM)
    q1, q2 = rope(dq[..., 0, :]), rope(dq[..., 1, :])
    k1, k2 = rope(dk[..., 0, :]), rope(dk[..., 1, :])
    dv = dv.reshape(B, S, DIFF_HEADS, 2 * HEAD_DIM)
    f32 = jnp.float32
    lam = (jnp.exp(jnp.sum(lam_q1.astype(f32) * lam_k1.astype(f32)))
           - jnp.exp(jnp.sum(lam_q2.astype(f32) * lam_k2.astype(f32))) + lam_init)
    y_diff = diff_attention(q1, q2, k1, k2, dv, lam, lam_init, diff_subln_g).reshape(B, S, DIFF_WIDTH)
    sq = rope(sq.reshape(B, S, DIL_HEADS, HEAD_DIM))
    sk = rope(sk.reshape(B, S, DIL_HEADS, HEAD_DIM))
    sv = sv.reshape(B, S, DIL_HEADS, HEAD_DIM)
    y_dil = rmsnorm(dilated_attention(sq, sk, sv), dil_out_g.reshape(DIL_HEADS, HEAD_DIM))
    y_dil = y_dil.reshape(B, S, DIL_WIDTH)
    return jnp.einsum('bse,ed->bsd', jnp.concatenate([y_diff, y_dil], axis=-1), w_out)


def peer_ffn(h, w_q, subkeys, u, v):
    B, S, D = h.shape
    q = jnp.einsum('bsd,de->bse', h, w_q).reshape(B, S, PEER_HEADS, 2, PEER_QDIM // 2)
    sc = jnp.einsum('bshpk,hpnk->bshpn', q, subkeys, preferred_element_type=jnp.float32)
    top_v, top_i = lax.top_k(sc, PEER_TOPK)
    cand = (top_v[..., 0, :, None] + top_v[..., 1, None, :]).reshape(B, S, PEER_HEADS, PEER_TOPK ** 2)
    cand_id = (top_i[..., 0, :, None] * N_KEYS + top_i[..., 1, None, :]).reshape(B, S, PEER_HEADS, PEER_TOPK ** 2)
    fin_v, fin_pos = lax.top_k(cand, PEER_TOPK)
    expert_id = jnp.take_along_axis(cand_id, fin_pos, axis=-1)
    gate = jax.nn.softmax(fin_v, axis=-1)
    T = B * S
    nblk = T // PEER_BLK
    E = PEER_HEADS * PEER_TOPK
    hb = h.reshape(nblk, PEER_BLK, D)
    ib = expert_id.reshape(nblk, PEER_BLK, E)
    gb = gate.reshape(nblk, PEER_BLK, E)

    def block(args):
        ht, it, gt = args
        ue = jnp.take(u, it, axis=0)
        act = jax.nn.gelu(jnp.einsum('td,ted->te', ht, ue, preferred_element_type=jnp.float32),
                          approximate=False)
        ve = jnp.take(v, it, axis=0)
        return jnp.einsum('te,ted->td', (gt * act).astype(v.dtype), ve)

    return lax.map(block, (hb, ib, gb)).reshape(B, S, D)


def setup_inputs(seed: int = 0) -> dict:
    key = jax.random.key(seed)
    ks = jax.random.split(key, 20)
    nrm = lambda k, shape, s: jax.random.normal(k, shape, jnp.float32) * s
    return {
        "x": nrm(ks[0], (BATCH, SEQ, D_MODEL), 1.0),
        "c": nrm(ks[1], (BATCH, D_MODEL), 1.0),
        "positions": jnp.broadcast_to(jnp.arange(SEQ, dtype=jnp.int32), (BATCH, SEQ)),
        "norm1_g": 1.0 + nrm(ks[2], (DEPTH, D_MODEL), 0.02),
        "norm2_g": 1.0 + nrm(ks[3], (DEPTH, D_MODEL), 0.02),
        "w_ada": nrm(ks[4], (DEPTH, D_MODEL, 6 * D_MODEL), 0.5 * D_MODEL ** -0.5),
        "b_ada": nrm(ks[5], (DEPTH, 6 * D_MODEL), 0.01),
        "w_in": nrm(ks[6], (DEPTH, D_MODEL, IN_COLS), D_MODEL ** -0.5),
        "lam_q1": nrm(ks[7], (DEPTH, HEAD_DIM), 0.1),
        "lam_k1": nrm(ks[8], (DEPTH, HEAD_DIM), 0.1),
        "lam_q2": nrm(ks[9], (DEPTH, HEAD_DIM), 0.1),
        "lam_k2": nrm(ks[10], (DEPTH, HEAD_DIM), 0.1),
        "diff_subln_g": 1.0 + nrm(ks[11], (DEPTH, 2 * HEAD_DIM), 0.02),
        "dil_out_g": 1.0 + nrm(ks[12], (DEPTH, DIL_WIDTH), 0.02),
        "w_out": nrm(ks[13], (DEPTH, D_MIX, D_MODEL), D_MIX ** -0.5),
        "peer_wq": nrm(ks[14], (DEPTH, D_MODEL, PEER_HEADS * PEER_QDIM), D_MODEL ** -0.5),
        "peer_subkeys": nrm(ks[15], (DEPTH, PEER_HEADS, 2, N_KEYS, PEER_QDIM // 2), (PEER_QDIM // 2) ** -0.5),
        "peer_u": nrm(ks[16], (DEPTH, N_EXPERTS, D_MODEL), D_MODEL ** -0.5),
        "peer_v": nrm(ks[17], (DEPTH, N_EXPERTS, D_MODEL), 0.5),
        "final_g": 1.0 + nrm(ks[18], (D_MODEL,), 0.02),
    }


def reference(x, c, positions, norm1_g, norm2_g, w_ada, b_ada, w_in, lam_q1, lam_k1,
              lam_q2, lam_k2, diff_subln_g, dil_out_g, w_out, peer_wq, peer_subkeys,
              peer_u, peer_v, final_g):
    cos, sin = rotary_tables(positions)
    c_act = jax.nn.silu(c)
    for l in range(DEPTH):
        lam_init = 0.8 - 0.6 * math.exp(-0.3 * l)
        mod = jnp.einsum('bd,de->be', c_act, w_ada[l]) + b_ada[l]
        sh1, sc1, g1, sh2, sc2, g2 = jnp.split(mod, 6, axis=-1)
        h = modulate(rmsnorm(x, norm1_g[l]), sh1, sc1)
        x = x + g1[:, None, :] * hybrid_mixer(h, cos, sin, w_in[l], w_out[l], lam_q1[l], lam_k1[l],
                                              lam_q2[l], lam_k2[l], diff_subln_g[l], dil_out_g[l], lam_init)
        h = modulate(rmsnorm(x, norm2_g[l]), sh2, sc2)
        x = x + g2[:, None, :] * peer_ffn(h, peer_wq[l], peer_subkeys[l], peer_u[l], peer_v[l])
    return rmsnorm(x, final_g)
```

```python
from contextlib import ExitStack
import math
import numpy as np
import ml_dtypes
import concourse.bass as bass
import concourse.mybir as mybir
from concourse.bass_utils import run_bass_kernel_spmd

F32 = mybir.dt.float32
BF16 = mybir.dt.bfloat16
I32 = mybir.dt.int32
U32 = mybir.dt.uint32
AF = mybir.ActivationFunctionType
ALU = mybir.AluOpType
AX = mybir.AxisListType

NCORES = 8
EPS = 1e-6
LAM_INIT = 0.2
TOPK = 16
PH = 8
SCALE = 128 ** -0.5
TWO_PI = 2.0 * math.pi
C1 = 6.28125
C2 = TWO_PI - C1


class Cfg:
    def __init__(self, D=4096, SEQ=8192, NK=128):
        self.D, self.SEQ, self.NK = D, SEQ, NK
        self.KC = D // 128
        self.DIFFW = D // 2
        self.DILW = D - self.DIFFW
        self.NDH = self.DIFFW // 256
        self.NSH = self.DILW // 128
        self.NB = SEQ // 128
        self.NBL = self.NB // NCORES
        self.TL = self.NBL * 128
        self.NEXP = NK * NK
        self.NMOD = 6 * self.KC
        self.CH = min(512, SEQ)
        self.NCH = SEQ // self.CH


class Track:
    def __init__(self, nc, name, unit):
        self.sem = nc.semaphore(name).__enter__()
        self.unit = unit
        self.cnt = 0


class Buf:
    __slots__ = ("w", "r")

    def __init__(self):
        self.w = {}
        self.r = {}


class Eng:
    def __init__(self, name, h, track):
        self.name, self.h, self.track, self.waited = name, h, track, {}


class K:
    def __init__(self, nc, n_dma_tracks=48):
        self.nc = nc
        self.eng = {}
        for name, h in (("pe", nc.tensor), ("act", nc.scalar), ("dve", nc.vector),
                        ("pool", nc.gpsimd), ("sp", nc.sync)):
            self.eng[name] = Eng(name, h, Track(nc, "t_" + name, 1))
        self.free_dma = [Track(nc, "d%d" % i, 16) for i in range(n_dma_tracks)]
        self.used_dma = []
        self.all_tracks = [e.track for e in self.eng.values()] + list(self.free_dma)

    def dtrack(self):
        t = self.free_dma.pop()
        self.used_dma.append(t)
        return t

    def release_dma(self):
        self.free_dma.extend(self.used_dma)
        self.used_dma = []

    def _wait(self, e, deps):
        for tr, c in deps.items():
            if e.waited.get(tr, 0) < c:
                e.h.wait_ge(tr.sem, c * tr.unit)
                e.waited[tr] = c

    def op(self, eng, fn, reads=(), writes=(), track=None):
        e = self.eng[eng]
        tr = track or e.track
        deps = {}
        for b in reads:
            for t, c in b.w.items():
                if deps.get(t, 0) < c:
                    deps[t] = c
        for b in writes:
            for t, c in b.w.items():
                if deps.get(t, 0) < c:
                    deps[t] = c
            for t, c in b.r.items():
                if deps.get(t, 0) < c:
                    deps[t] = c
        if eng == "pe":
            deps.pop(e.track, None)
        self._wait(e, deps)
        ins = fn()
        ins.then_inc(tr.sem, tr.unit)
        tr.cnt += 1
        for b in reads:
            b.r[tr] = tr.cnt
        for b in writes:
            b.w[tr] = tr.cnt

    def barrier(self):
        for e in self.eng.values():
            self._wait(e, {t: t.cnt for t in self.all_tracks if t.cnt > 0})


def build(cfg=None, debug=False, upto="all"):
    cfg = cfg or Cfg()
    D, SEQ, KC, TL, NB, NBL, NK = cfg.D, cfg.SEQ, cfg.KC, cfg.TL, cfg.NB, cfg.NBL, cfg.NK
    nc = bass.Bass("TRN2", target_bir_lowering=False)
    k = K(nc)
    io = {}
    V, A, P, T, S = nc.vector, nc.scalar, nc.gpsimd, nc.tensor, nc.sync
    DBG = ("modf", "KT", "QT", "Vs", "yT", "x1") if debug else ()

    def din(name, shape, dt):
        io[name] = nc.dram_tensor(name, list(shape), dt, kind="ExternalInput").ap()

    def dout(name, shape, dt):
        io[name] = nc.dram_tensor(name, list(shape), dt, kind="ExternalOutput").ap()

    def dint(name, shape, dt):
        kind = "ExternalOutput" if name in DBG else "Internal"
        io[name] = nc.dram_tensor(name, list(shape), dt, kind=kind).ap()

    din("x_all", [SEQ, D], F32)
    din("x_own", [TL, D], F32)
    din("pos_all", [1, SEQ], I32)
    din("pos_own", [1, TL], I32)
    din("c_row", [1, D], F32)
    din("w_adaT", [cfg.NMOD, 128, D], F32)
    din("b_adaT", [128, cfg.NMOD], F32)
    din("n1gT", [128, KC], F32)
    din("n2gT", [128, KC], F32)
    din("n2g_row", [1, D], F32)
    din("fg_row", [1, D], F32)
    din("w_q", [D, D], F32)
    din("w_k", [D, D], F32)
    din("w_v", [D, D], F32)
    din("w_out", [D, D], F32)
    din("lam4", [4, 128], F32)
    din("subln_row", [1, 256], F32)
    din("dilg_row", [1, cfg.DILW], F32)
    din("peer_wq", [D, PH * 256], F32)
    din("skT", [2 * PH, 128, NK], F32)
    din("peer_u", [cfg.NEXP, D], F32)
    din("peer_v", [cfg.NEXP, D], F32)
    din("ident", [128, 128], BF16)
    din("perm", [128, 128], BF16)
    din("invf", [32, 1], F32)
    din("mdiff", [128, 8, 128], BF16)
    din("mdil", [128, 24, 128], BF16)
    din("iota16", [128, 16], F32)
    dout("out", [TL, D], F32)

    dint("modf", [cfg.NMOD * 128], F32)
    dint("wq_bf", [KC, 128, KC, 128], BF16)
    dint("wk_bf", [KC, 128, KC, 128], BF16)
    dint("wv_bf", [D // 256, 128, KC, 256], BF16)
    dint("KT", [KC, 128, SEQ], BF16)
    dint("QT", [KC, 128, TL], BF16)
    dint("Vs", [SEQ, D], BF16)
    dint("yT", [KC, 128, TL], BF16)
    dint("x1", [TL, D], F32)
    dint("u_bf", [cfg.NEXP, D], BF16)
    dint("v_bf", [cfg.NEXP, D], BF16)

    gs = ExitStack()

    def gsb(name, shape, dt):
        return gs.enter_context(nc.sbuf_tensor(name, list(shape), dt))

    ident = gsb("ident_sb", [128, 128], BF16)
    perm = gsb("perm_sb", [128, 128], BF16)
    modT = gsb("modT", [128, cfg.NMOD], F32)
    A1 = gsb("A1", [128, KC], F32)
    A2 = gsb("A2", [128, KC], F32)
    invf = gsb("invf_sb", [32, 1], F32)
    b_const, b_modT, b_A = Buf(), Buf(), Buf()
    tr0 = k.free_dma.pop()
    k.op("sp", lambda: S.dma_start(out=ident[:], in_=io["ident"]), writes=[b_const], track=tr0)
    k.op("sp", lambda: S.dma_start(out=perm[:], in_=io["perm"]), writes=[b_const], track=tr0)
    k.op("sp", lambda: S.dma_start(out=invf[:], in_=io["invf"]), writes=[b_const], track=tr0)

    def new_phase():
        es = ExitStack()

        def sb(name, shape, dt):
            return es.enter_context(nc.sbuf_tensor(name, list(shape), dt))

        def ps(name, shape, dt):
            return es.enter_context(nc.psum_tensor(name, list(shape), dt))
        return es, sb, ps

    def end_phase(es):
        k.barrier()
        es.close()
        k.release_dma()

    def finish():
        k.barrier()
        gs.close()
        return nc

    es, sb, ps = new_phase()
    cact = sb("cact", [128, D], F32)
    b_cact = Buf()
    trc = k.dtrack()
    k.op("sp", lambda: S.dma_start(out=cact[:], in_=io["c_row"].to_broadcast([128, D])), writes=[b_cact], track=trc)
    k.op("act", lambda: A.activation(out=cact[:], in_=cact[:], func=AF.Silu), reads=[b_cact], writes=[b_cact])
    junk = sb("junk0", [128, D], BF16)
    b_junk = Buf()
    badaT = sb("badaT", [128, cfg.NMOD], F32)
    n1gT = sb("n1gT_sb", [128, KC], F32)
    n2gT = sb("n2gT_sb", [128, KC], F32)
    b_ld = Buf()
    k.op("sp", lambda: S.dma_start(out=badaT[:], in_=io["b_adaT"]), writes=[b_ld], track=trc)
    k.op("sp", lambda: S.dma_start(out=n1gT[:], in_=io["n1gT"]), writes=[b_ld], track=trc)
    k.op("sp", lambda: S.dma_start(out=n2gT[:], in_=io["n2gT"]), writes=[b_ld], track=trc)
    NWB = 4
    wa = [sb("wa%d" % i, [128, D], F32) for i in range(NWB)]
    b_wa = [Buf() for _ in range(NWB)]
    tr_wa = [k.dtrack() for _ in range(NWB)]
    for j in range(cfg.NMOD):
        i = j % NWB
        k.op("sp", lambda i=i, j=j: S.dma_start(out=wa[i][:], in_=io["w_adaT"][j]), writes=[b_wa[i]], track=tr_wa[i])
        k.op("dve", lambda i=i, j=j: V.scalar_tensor_tensor(
            out=junk[:], in0=wa[i][:], scalar=1.0, in1=cact[:], op0=ALU.mult, op1=ALU.mult,
            accum_out=modT[:, j:j + 1]), reads=[b_wa[i], b_cact], writes=[b_junk, b_modT])
    k.op("dve", lambda: V.tensor_tensor(out=modT[:], in0=modT[:], in1=badaT[:], op=ALU.add),
         reads=[b_ld, b_modT], writes=[b_modT])
    k.op("dve", lambda: V.scalar_tensor_tensor(out=A1[:], in0=modT[:, KC:2 * KC], scalar=1.0, in1=n1gT[:],
                                               op0=ALU.add, op1=ALU.mult), reads=[b_modT, b_ld], writes=[b_A])
    k.op("dve", lambda: V.scalar_tensor_tensor(out=A2[:], in0=modT[:, 4 * KC:5 * KC], scalar=1.0, in1=n2gT[:],
                                               op0=ALU.add, op1=ALU.mult), reads=[b_modT, b_ld], writes=[b_A])
    B1 = modT[:, 0:KC]
    B2 = modT[:, 3 * KC:4 * KC]
    b_modf = Buf()
    with nc.allow_non_contiguous_dma(reason="one-off mod vector relayout"):
        modfv = io["modf"].rearrange("(j p) -> p j", p=128)
        for j0 in range(0, cfg.NMOD, 8):
            k.op("sp", lambda j0=j0: S.dma_start(out=modfv[:, j0:j0 + 8], in_=modT[:, j0:j0 + 8]),
                 reads=[b_modT], writes=[b_modf], track=trc)
    end_phase(es)
    if upto == "P0":
        return finish()

    es, sb, ps = new_phase()
    b_wbf = Buf()
    stg = [sb("pc_f%d" % i, [128, 2, D], F32) for i in range(2)]
    stb = [sb("pc_b%d" % i, [128, 2, D], BF16) for i in range(2)]
    b_stg = [Buf() for _ in range(2)]
    b_stb = [Buf() for _ in range(2)]
    tr_stg = [k.dtrack() for _ in range(2)]
    tr_stb = [k.dtrack() for _ in range(2)]
    it = 0
    for (src, dst, cw) in (("w_q", "wq_bf", 128), ("w_k", "wk_bf", 128), ("w_v", "wv_bf", 256)):
        srcv = io[src].rearrange("(kk p) c -> p kk c", p=128)
        dstv = io[dst].rearrange("m p kk c -> p kk m c")
        for k0 in range(0, KC, 2):
            i = it % 2
            it += 1
            k.op("sp", lambda i=i, k0=k0, srcv=srcv: S.dma_start(out=stg[i][:], in_=srcv[:, k0:k0 + 2, :]),
                 writes=[b_stg[i]], track=tr_stg[i])
            eng = ("act", "pool", "dve")[it % 3]
            if eng == "act":
                k.op("act", lambda i=i: A.copy(out=stb[i][:], in_=stg[i][:]), reads=[b_stg[i]], writes=[b_stb[i]])
            elif eng == "pool":
                k.op("pool", lambda i=i: P.tensor_copy(out=stb[i][:], in_=stg[i][:]), reads=[b_stg[i]], writes=[b_stb[i]])
            else:
                k.op("dve", lambda i=i: V.tensor_copy(out=stb[i][:], in_=stg[i][:]), reads=[b_stg[i]], writes=[b_stb[i]])
            nm = D // cw
            for kk in range(2):
                ms = min(8, nm)
                for m0 in range(0, nm, ms):
                    k.op("sp", lambda i=i, kk=kk, k0=k0, dstv=dstv, cw=cw, m0=m0, ms=ms: S.dma_start(
                        out=dstv[:, k0 + kk, m0:m0 + ms],
                        in_=stb[i][:, kk, m0 * cw:(m0 + ms) * cw].rearrange("p (m c) -> p m c", c=cw)),
                        reads=[b_stb[i]], writes=[b_wbf], track=tr_stb[i])
    end_phase(es)

    es, sb, ps = new_phase()
    CHm = cfg.CH
    hT = sb("hT", [128, KC, CHm], BF16)
    b_hT = Buf()
    xt = [sb("xt%d" % i, [128, D], F32) for i in range(2)]
    b_xt = [Buf() for _ in range(2)]
    tr_x = [k.dtrack() for _ in range(2)]
    xn = [sb("xn%d" % i, [128, D], BF16) for i in range(2)]
    b_xn = [Buf() for _ in range(2)]
    junk = sb("junk1", [128, D], BF16)
    b_junk = Buf()
    stat = sb("stat1", [128, 4], F32)
    b_stat = Buf()
    HK = KC // 2
    pT = [ps("pT%d" % i, [128, HK * 128], BF16) for i in range(2)]
    b_pT = [Buf() for _ in range(2)]
    pp = [ps("pp%d" % i, [128, 512], F32) for i in range(3)]
    b_pp = [Buf() for _ in range(3)]
    psw = ps("psw", [128, 512], F32)
    b_psw = Buf()
    posi = sb("posi", [32, CHm], I32)
    rw = [sb("rw%d" % i, [32, CHm], F32) for i in range(5)]
    rki = sb("rki", [32, CHm], I32)
    cos_t = sb("cos_t", [32, CHm], F32)
    sin_t = sb("sin_t", [32, CHm], F32)
    b_rot, b_tab = Buf(), Buf()
    tr_pos = k.dtrack()
    wkb = [sb("wkb%d" % i, [128, KC, 128], BF16) for i in range(2)]
    b_wkb = [Buf() for _ in range(2)]
    tr_wkb = [k.dtrack() for _ in range(2)]
    wvb = [sb("wvb%d" % i, [128, KC, 256], BF16) for i in range(2)]
    b_wvb = [Buf() for _ in range(2)]
    tr_wvb = [k.dtrack() for _ in range(2)]
    NST = 5
    kst = [sb("kst%d" % i, [128, 512], BF16) for i in range(NST)]
    b_kst = [Buf() for _ in range(NST)]
    tr_kst = [k.dtrack() for _ in range(NST)]
    vst = [sb("vst%d" % i, [128, 256], BF16) for i in range(NST)]
    b_vst = [Buf() for _ in range(NST)]
    tr_vst = [k.dtrack() for _ in range(NST)]
    rt = [sb("rt%d" % i, [32, 512], F32) for i in range(2)]
    b_rt = Buf()
    b_KT, b_QT, b_Vs = Buf(), Buf(), Buf()
    cnt = {"x": 0, "pp": 0, "kst": 0, "vst": 0, "wk": 0, "wv": 0, "ev": 0}

    pcf = [sb("pcf%d" % i, [128, D], F32) for i in range(2)]
    pcb = [sb("pcb0", [128, D], BF16)] * 2
    b_pcf = [Buf() for _ in range(2)]
    b_pcb = [Buf()] * 2
    tr_pcf = [k.dtrack() for _ in range(2)]
    tr_pcb = [k.dtrack()] * 2
    b_tabs = Buf()
    NBLK = cfg.NEXP // 128
    pc_jobs = [(src, dst, b) for (src, dst) in (("peer_u", "u_bf"), ("peer_v", "v_bf")) for b in range(NBLK)]
    pc_state = {"next": 0, "tick": 0}
    NQ = 4
    QW = D // NQ
    pc_every = max(1, (4 * cfg.NCH * KC) // (NQ * (len(pc_jobs) + 2)))

    def pc_steps(n):
        for _ in range(n):
            st = pc_state["next"]
            jn, q = st // NQ, st % NQ
            if jn >= len(pc_jobs):
                return
            if st == 0:
                src, dst, b = pc_jobs[0]
                k.op("sp", lambda src=src, b=b: S.dma_start(out=pcf[0][:], in_=io[src][b * 128:(b + 1) * 128, :]),
                     writes=[b_pcf[0]], track=tr_pcf[0])
            if q == 0 and jn + 1 < len(pc_jobs):
                src, dst, b = pc_jobs[jn + 1]
                i2 = (jn + 1) % 2
                k.op("sp", lambda i2=i2, src=src, b=b: S.dma_start(out=pcf[i2][:], in_=io[src][b * 128:(b + 1) * 128, :]),
                     writes=[b_pcf[i2]], track=tr_pcf[i2])
            i = jn % 2
            k.op("act", lambda i=i, q=q: A.copy(out=pcb[i][:, q * QW:(q + 1) * QW], in_=pcf[i][:, q * QW:(q + 1) * QW]),
                 reads=[b_pcf[i]], writes=[b_pcb[i]])
            if q == NQ - 1:
                src, dst, b = pc_jobs[jn]
                k.op("pool", lambda i=i, dst=dst, b=b: P.dma_start(out=io[dst][b * 128:(b + 1) * 128, :], in_=pcb[i][:]),
                     reads=[b_pcb[i]], writes=[b_tabs], track=tr_pcb[i])
            pc_state["next"] = st + 1

    def pc_tick():
        pc_state["tick"] += 1
        if pc_state["tick"] % pc_every == 0:
            pc_steps(1)

    def rms_to_hT(xsrc, ntok, Acol, Bcol, hT_t, b_hT_t, readsA):
        for t in range(ntok // 128):
            i = cnt["x"] % 2
            cnt["x"] += 1
            k.op("sp", lambda i=i, t=t: S.dma_start(out=xt[i][:], in_=xsrc[t * 128:(t + 1) * 128, :]),
                 writes=[b_xt[i]], track=tr_x[i])
            k.op("dve", lambda i=i: V.scalar_tensor_tensor(
                out=junk[:], in0=xt[i][:], scalar=1.0, in1=xt[i][:], op0=ALU.mult, op1=ALU.mult,
                accum_out=stat[:, 0:1]), reads=[b_xt[i]], writes=[b_junk, b_stat])
            k.op("act", lambda: A.activation(out=stat[:, 1:2], in_=stat[:, 0:1], func=AF.Ln, scale=1.0 / D, bias=EPS),
                 reads=[b_stat], writes=[b_stat])
            k.op("act", lambda: A.activation(out=stat[:, 2:3], in_=stat[:, 1:2], func=AF.Exp, scale=-0.5),
                 reads=[b_stat], writes=[b_stat])
            k.op("pool", lambda i=i: P.tensor_scalar(out=xn[i][:], in0=xt[i][:], scalar1=stat[:, 2:3], scalar2=None,
                                                    op0=ALU.mult), reads=[b_xt[i], b_stat], writes=[b_xn[i]])
            for hh in range(2):
                for q in range(HK):
                    kk = hh * HK + q
                    k.op("pe", lambda i=i, kk=kk, q=q, hh=hh: T.transpose(
                        out=pT[hh][:, q * 128:(q + 1) * 128], in_=xn[i][:, kk * 128:(kk + 1) * 128], identity=ident[:]),
                        reads=[b_xn[i], b_const], writes=[b_pT[hh]])
                for q in range(HK):
                    kk = hh * HK + q
                    if q % 2 == 0:
                        k.op("act", lambda kk=kk, q=q, hh=hh, t=t: A.activation(
                            out=hT_t[:, kk, t * 128:(t + 1) * 128], in_=pT[hh][:, q * 128:(q + 1) * 128],
                            func=AF.Identity, scale=Acol[:, kk:kk + 1], bias=Bcol[:, kk:kk + 1]),
                            reads=[b_pT[hh]] + readsA, writes=[b_hT_t])
                    else:
                        k.op("dve", lambda kk=kk, q=q, hh=hh, t=t: V.tensor_scalar(
                            out=hT_t[:, kk, t * 128:(t + 1) * 128], in0=pT[hh][:, q * 128:(q + 1) * 128],
                            scalar1=Acol[:, kk:kk + 1], scalar2=Bcol[:, kk:kk + 1], op0=ALU.mult, op1=ALU.add),
                            reads=[b_pT[hh]] + readsA, writes=[b_hT_t])

    def rot_tables(pos_ap, ntok):
        k.op("sp", lambda: S.dma_start(out=posi[:, :ntok], in_=pos_ap.to_broadcast([32, ntok])),
             reads=[b_rot], writes=[b_rot], track=tr_pos)
        ang, a, y, kf, r = [w[:, :ntok] for w in rw]
        dv = lambda fn: k.op("dve", fn, reads=[b_rot, b_const], writes=[b_rot])
        dv(lambda: V.tensor_copy(out=ang, in_=posi[:, :ntok]))
        dv(lambda: V.tensor_scalar(out=ang, in0=ang, scalar1=invf[:, 0:1], scalar2=None, op0=ALU.mult))
        for tab, shift in ((sin_t, 0.0), (cos_t, 0.5 * math.pi)):
            dv(lambda shift=shift: V.tensor_scalar(out=a, in0=ang, scalar1=shift, scalar2=None, op0=ALU.add))
            dv(lambda: V.tensor_scalar(out=y, in0=a, scalar1=1.0 / TWO_PI, scalar2=0.5, op0=ALU.mult, op1=ALU.add))
            dv(lambda: V.tensor_copy(out=rki[:, :ntok], in_=y))
            dv(lambda: V.tensor_copy(out=kf, in_=rki[:, :ntok]))
            dv(lambda: V.scalar_tensor_tensor(out=r, in0=kf, scalar=-C1, in1=a, op0=ALU.mult, op1=ALU.add))
            dv(lambda: V.scalar_tensor_tensor(out=r, in0=kf, scalar=-C2, in1=r, op0=ALU.mult, op1=ALU.add))
            dv(lambda: V.tensor_scalar(out=y, in0=r, scalar1=-math.pi, scalar2=None, op0=ALU.is_lt))
            dv(lambda: V.scalar_tensor_tensor(out=r, in0=y, scalar=TWO_PI, in1=r, op0=ALU.mult, op1=ALU.add))
            dv(lambda: V.tensor_scalar(out=y, in0=r, scalar1=math.pi, scalar2=None, op0=ALU.is_gt))
            dv(lambda: V.scalar_tensor_tensor(out=r, in0=y, scalar=-TWO_PI, in1=r, op0=ALU.mult, op1=ALU.add))
            dv(lambda: V.tensor_scalar(out=r, in0=r, scalar1=-3.14159, scalar2=3.14159, op0=ALU.max, op1=ALU.min))
            k.op("act", lambda tab=tab: A.activation(out=tab[:, :ntok], in_=r, func=AF.Sin),
                 reads=[b_rot], writes=[b_tab])

    def proj_rot(wsrc, dst, b_dst, ntok, tok0):
        nn = min(512, ntok)
        for mp in range(KC):
            if wsrc == "wk_bf":
                for _ in range(4):
                    pc_tick()
            wi = cnt["wk"] % 2
            cnt["wk"] += 1
            k.op("sp", lambda wi=wi, mp=mp: S.dma_start(out=wkb[wi][:], in_=io[wsrc][mp]),
                 reads=[b_wbf], writes=[b_wkb[wi]], track=tr_wkb[wi])
            for hf in range(ntok // nn):
                pi = cnt["pp"] % 3
                cnt["pp"] += 1
                si = cnt["kst"] % NST
                cnt["kst"] += 1
                for kk in range(KC):
                    k.op("pe", lambda wi=wi, kk=kk, hf=hf, pi=pi: T.matmul(
                        pp[pi][:, :nn], lhsT=wkb[wi][:, kk, :], rhs=hT[:, kk, hf * nn:(hf + 1) * nn],
                        start=(kk == 0), stop=(kk == KC - 1)), reads=[b_wkb[wi], b_hT], writes=[b_pp[pi]])
                k.op("act", lambda pi=pi, si=si: A.copy(out=kst[si][:, :nn], in_=pp[pi][:, :nn]),
                     reads=[b_pp[pi]], writes=[b_kst[si]])
                k.op("pe", lambda si=si: T.matmul(psw[:, :nn], lhsT=perm[:], rhs=kst[si][:, :nn], start=True, stop=True),
                     reads=[b_kst[si], b_const], writes=[b_psw])
                sl = slice(hf * nn, (hf + 1) * nn)
                k.op("dve", lambda sl=sl: V.tensor_tensor(out=rt[0][:, :nn], in0=psw[0:32, :nn], in1=sin_t[:, sl], op=ALU.mult),
                     reads=[b_psw, b_tab], writes=[b_rt])
                k.op("dve", lambda sl=sl, pi=pi: V.tensor_tensor(out=rt[1][:, :nn], in0=pp[pi][0:32, :nn], in1=cos_t[:, sl], op=ALU.mult),
                     reads=[b_pp[pi], b_tab], writes=[b_rt])
                k.op("dve", lambda si=si: V.tensor_tensor(out=kst[si][0:32, :nn], in0=rt[0][:, :nn], in1=rt[1][:, :nn], op=ALU.add),
                     reads=[b_rt], writes=[b_kst[si]])
                k.op("pool", lambda si=si, mp=mp, hf=hf: P.dma_start(
                    out=io[dst][mp][:, tok0 + hf * nn: tok0 + (hf + 1) * nn], in_=kst[si][:, :nn]),
                    reads=[b_kst[si]], writes=[b_dst], track=tr_kst[si])

    def proj_v(ntok, tok0):
        for vc in range(D // 256):
            wi = cnt["wv"] % 2
            cnt["wv"] += 1
            k.op("sp", lambda wi=wi, vc=vc: S.dma_start(out=wvb[wi][:], in_=io["wv_bf"][vc]),
                 reads=[b_wbf], writes=[b_wvb[wi]], track=tr_wvb[wi])
            for tt in range(ntok // 128):
                pi = cnt["pp"] % 3
                cnt["pp"] += 1
                si = cnt["vst"] % NST
                cnt["vst"] += 1
                for kk in range(KC):
                    k.op("pe", lambda wi=wi, kk=kk, tt=tt, pi=pi: T.matmul(
                        pp[pi][:, :256], lhsT=hT[:, kk, tt * 128:(tt + 1) * 128], rhs=wvb[wi][:, kk, :],
                        start=(kk == 0), stop=(kk == KC - 1)), reads=[b_wvb[wi], b_hT], writes=[b_pp[pi]])
                cnt["ev"] += 1
                if cnt["ev"] % 2 == 0:
                    k.op("act", lambda pi=pi, si=si: A.copy(out=vst[si][:], in_=pp[pi][:, :256]),
                         reads=[b_pp[pi]], writes=[b_vst[si]])
                else:
                    k.op("dve", lambda pi=pi, si=si: V.tensor_copy(out=vst[si][:], in_=pp[pi][:, :256]),
                         reads=[b_pp[pi]], writes=[b_vst[si]])
                k.op("pool", lambda si=si, vc=vc, tt=tt: P.dma_start(
                    out=io["Vs"][tok0 + tt * 128: tok0 + (tt + 1) * 128, vc * 256:(vc + 1) * 256], in_=vst[si][:]),
                    reads=[b_vst[si]], writes=[b_Vs], track=tr_vst[si])

    OCH = min(cfg.CH, TL)
    for oc in range(TL // OCH):
        t0 = oc * OCH
        rms_to_hT(io["x_own"][t0:t0 + OCH, :], OCH, A1, B1, hT, b_hT, [b_A, b_modT])
        rot_tables(io["pos_own"][:, t0:t0 + OCH], OCH)
        proj_rot("wq_bf", "QT", b_QT, OCH, t0)
    for ch in range(cfg.NCH):
        t0 = ch * cfg.CH
        rms_to_hT(io["x_all"][t0:t0 + cfg.CH, :], cfg.CH, A1, B1, hT, b_hT, [b_A, b_modT])
        rot_tables(io["pos_all"][:, t0:t0 + cfg.CH], cfg.CH)
        proj_rot("wk_bf", "KT", b_KT, cfg.CH, t0)
        proj_v(cfg.CH, t0)
    pc_steps(NQ * (len(pc_jobs) + 2))
    end_phase(es)
    if upto == "P1":
        return finish()

    es, sb, ps = new_phase()
    mdiff = sb("mdiff_sb", [128, 8, 128], BF16)
    mdil = sb("mdil_sb", [128, 24, 128], BF16)
    lamt = sb("lamt", [128, 4, 128], F32)
    lamw = sb("lamw", [128, 8], F32)
    gsub = sb("gsub", [128, 256], F32)
    gdil = sb("gdil", [128, cfg.DILW], F32)
    b_c2 = Buf()
    trc = k.dtrack()
    k.op("sp", lambda: S.dma_start(out=mdiff[:], in_=io["mdiff"]), writes=[b_c2], track=trc)
    k.op("sp", lambda: S.dma_start(out=mdil[:], in_=io["mdil"]), writes=[b_c2], track=trc)
    for i in range(4):
        k.op("sp", lambda i=i: S.dma_start(out=lamt[:, i, :], in_=io["lam4"][i:i + 1, :].to_broadcast([128, 128])),
             writes=[b_c2], track=trc)
    k.op("sp", lambda: S.dma_start(out=gsub[:], in_=io["subln_row"].to_broadcast([128, 256])), writes=[b_c2], track=trc)
    k.op("sp", lambda: S.dma_start(out=gdil[:], in_=io["dilg_row"].to_broadcast([128, cfg.DILW])), writes=[b_c2], track=trc)
    junk2 = sb("junk2", [128, 256], F32)
    b_j2 = Buf()
    for i in range(2):
        k.op("dve", lambda i=i: V.scalar_tensor_tensor(out=junk2[:, :128], in0=lamt[:, 2 * i, :], scalar=1.0,
                                                      in1=lamt[:, 2 * i + 1, :], op0=ALU.mult, op1=ALU.mult,
                                                      accum_out=lamw[:, i:i + 1]), reads=[b_c2], writes=[b_c2, b_j2])
    k.op("act", lambda: A.activation(out=lamw[:, 2:4], in_=lamw[:, 0:2], func=AF.Exp), reads=[b_c2], writes=[b_c2])
    k.op("dve", lambda: V.tensor_tensor(out=lamw[:, 4:5], in0=lamw[:, 3:4], in1=lamw[:, 2:3], op=ALU.subtract),
         reads=[b_c2], writes=[b_c2])
    k.op("dve", lambda: V.tensor_scalar(out=lamw[:, 5:6], in0=lamw[:, 4:5], scalar1=-LAM_INIT, scalar2=None, op0=ALU.add),
         reads=[b_c2], writes=[b_c2])
    k.op("dve", lambda: V.tensor_scalar(out=gsub[:], in0=gsub[:], scalar1=1.0 - LAM_INIT, scalar2=None, op0=ALU.mult),
         reads=[b_c2], writes=[b_c2])
    nlam = lamw[:, 5:6]

    ktb = [sb("ktb%d" % i, [128, SEQ], BF16) for i in range(4)]
    b_ktb = [Buf() for _ in range(4)]
    tr_ktb = [k.dtrack() for _ in range(4)]
    qtb = [sb("qtb%d" % i, [128, TL], BF16) for i in range(4)]
    b_qtb = [Buf() for _ in range(4)]
    tr_qtb = [k.dtrack() for _ in range(4)]
    vab = [sb("vab%d" % i, [128, NB, 257], BF16) for i in range(2)]
    b_vab = [Buf() for _ in range(2)]
    tr_vab = [k.dtrack() for _ in range(2)]
    for i in range(2):
        k.op("pool", lambda i=i: P.memset(vab[i][:], 1.0), writes=[b_vab[i]])
    sps = [ps("sps%d" % i, [128, 512], F32) for i in range(3)]
    b_sps = [Buf() for _ in range(3)]
    Ops = [ps("Ops%d" % i, [128, 512], F32) for i in range(4)]
    b_O = [Buf() for _ in range(4)]
    tps = ps("tps", [128, 256], BF16)
    b_tps = Buf()
    ptb = [sb("ptb%d" % i, [128, 512], BF16) for i in range(3)]
    b_ptb = [Buf() for _ in range(3)]
    fw = sb("fw", [128, 16], F32)
    b_fw = Buf()
    ft = [sb("ft%d" % i, [128, 256], F32) for i in range(2)]
    b_ft = Buf()
    yb = sb("yb", [128, 256], BF16)
    b_yb = Buf()
    yts = [sb("yts%d" % i, [128, 2, 128], BF16) for i in range(2)]
    b_yts = [Buf() for _ in range(2)]
    tr_yts = [k.dtrack() for _ in range(2)]
    b_yT = Buf()
    c2 = {"s": 0, "kq": 0, "v": 0, "y": 0, "o": 0}
    def attend(kt, b_kt, qt, b_qt, va, b_va, dv, m, kbs, mask_of, O, b_Oa):
        groups = [kbs[i:i + 4] for i in range(0, len(kbs), 4)]
        first = True
        for gi, grp in enumerate(groups):
            si = c2["s"] % 3
            c2["s"] += 1
            n = len(grp) * 128
            for j, kb in enumerate(grp):
                k.op("pe", lambda j=j, kb=kb, si=si: T.matmul(
                    sps[si][:, j * 128:(j + 1) * 128], lhsT=kt[:, kb * 128:(kb + 1) * 128],
                    rhs=qt[:, m * 128:(m + 1) * 128], start=True, stop=True),
                    reads=[b_kt, b_qt], writes=[b_sps[si]])
            k.op("act", lambda si=si, n=n: A.activation(out=ptb[si][:, :n], in_=sps[si][:, :n], func=AF.Exp, scale=SCALE),
                 reads=[b_sps[si]], writes=[b_ptb[si]])
            mk = mask_of(grp[0], len(grp))
            if mk is not None:
                k.op("dve", lambda si=si, n=n, mk=mk: V.tensor_tensor(out=ptb[si][:, :n], in0=ptb[si][:, :n], in1=mk, op=ALU.mult),
                     reads=[b_ptb[si], b_c2], writes=[b_ptb[si]])
            for j, kb in enumerate(grp):
                last = (gi == len(groups) - 1) and (j == len(grp) - 1)
                k.op("pe", lambda j=j, kb=kb, si=si, first=first, last=last: T.matmul(
                    O[:, :dv + 1], lhsT=ptb[si][:, j * 128:(j + 1) * 128], rhs=va[:, kb, 0:dv + 1],
                    start=first, stop=last), reads=[b_ptb[si], b_va], writes=[b_Oa])
                first = False

    def rstd_from_ss(ss_ap, n, out_ap):
        k.op("act", lambda: A.activation(out=fw[:, 8:9], in_=ss_ap, func=AF.Ln, scale=1.0 / n, bias=EPS),
             reads=[b_fw], writes=[b_fw])
        k.op("act", lambda: A.activation(out=out_ap, in_=fw[:, 8:9], func=AF.Exp, scale=-0.5),
             reads=[b_fw], writes=[b_fw])

    def emit_yT(width, chunk0, m):
        yi = c2["y"] % 2
        c2["y"] += 1
        nj = width // 128
        for j in range(nj):
            k.op("pe", lambda j=j: T.transpose(out=tps[:, j * 128:(j + 1) * 128], in_=yb[:, j * 128:(j + 1) * 128], identity=ident[:]),
                 reads=[b_yb, b_const], writes=[b_tps])
        k.op("act", lambda yi=yi, nj=nj: A.copy(out=yts[yi][:, :nj, :].rearrange("p j q -> p (j q)"), in_=tps[:, :nj * 128]),
             reads=[b_tps], writes=[b_yts[yi]])
        for j in range(nj):
            k.op("pool", lambda yi=yi, j=j: P.dma_start(out=io["yT"][chunk0 + j][:, m * 128:(m + 1) * 128], in_=yts[yi][:, j, :]),
                 reads=[b_yts[yi]], writes=[b_yT], track=tr_yts[yi])

    for h in range(cfg.NDH):
        vi = c2["v"] % 2
        c2["v"] += 1
        for b0 in range(0, NB, 8):
            k.op("sp", lambda vi=vi, h=h, b0=b0: S.dma_start(
                out=vab[vi][:, b0:b0 + 8, 0:256],
                in_=io["Vs"][b0 * 128:(b0 + 8) * 128, 256 * h:256 * (h + 1)].rearrange("(b p) c -> p b c", p=128)),
                reads=[b_Vs], writes=[b_vab[vi]], track=tr_vab[vi])
        kq = []
        for s in range(2):
            bi = c2["kq"] % 4
            c2["kq"] += 1
            k.op("sp", lambda bi=bi, h=h, s=s: S.dma_start(out=ktb[bi][:], in_=io["KT"][2 * h + s]),
                 reads=[b_KT], writes=[b_ktb[bi]], track=tr_ktb[bi])
            k.op("sp", lambda bi=bi, h=h, s=s: S.dma_start(out=qtb[bi][:], in_=io["QT"][2 * h + s]),
                 reads=[b_QT], writes=[b_qtb[bi]], track=tr_qtb[bi])
            kq.append(bi)
        for m in range(NBL):
            kbs = list(range(0, 8 * m + 8))

            def mask_of(kb0, n, m=m):
                r0 = kb0 - 8 * m
                if r0 < 0:
                    return None
                return mdiff[:, r0:r0 + n, :].rearrange("p r q -> p (r q)")
            ob = (m % 2) * 2
            for s in range(2):
                bi = kq[s]
                attend(ktb[bi], b_ktb[bi], qtb[bi], b_qtb[bi], vab[vi], b_vab[vi], 256, m, kbs, mask_of, Ops[ob + s], b_O[ob + s])
            O1, O2, bO1, bO2 = Ops[ob], Ops[ob + 1], b_O[ob], b_O[ob + 1]
            fin = lambda fn, r=(), w=(): k.op("dve", fn, reads=[b_fw, b_ft, b_c2] + list(r), writes=[b_fw, b_ft] + list(w))
            fin(lambda O1=O1: V.reciprocal(out=fw[:, 0:1], in_=O1[:, 256:257]), r=[bO1])
            fin(lambda O2=O2: V.reciprocal(out=fw[:, 1:2], in_=O2[:, 256:257]), r=[bO2])
            fin(lambda: V.tensor_tensor(out=fw[:, 2:3], in0=fw[:, 1:2], in1=nlam, op=ALU.mult))
            fin(lambda O2=O2: V.tensor_scalar(out=ft[0][:], in0=O2[:, 0:256], scalar1=fw[:, 2:3], scalar2=None, op0=ALU.mult), r=[bO2])
            fin(lambda O1=O1: V.scalar_tensor_tensor(out=ft[1][:], in0=O1[:, 0:256], scalar=fw[:, 0:1], in1=ft[0][:],
                                                     op0=ALU.mult, op1=ALU.add), r=[bO1])
            fin(lambda: V.scalar_tensor_tensor(out=junk2[:], in0=ft[1][:], scalar=1.0, in1=ft[1][:], op0=ALU.mult, op1=ALU.mult,
                                               accum_out=fw[:, 3:4]), w=[b_j2])
            rstd_from_ss(fw[:, 3:4], 256, fw[:, 4:5])
            fin(lambda: V.scalar_tensor_tensor(out=yb[:], in0=ft[1][:], scalar=fw[:, 4:5], in1=gsub[:], op0=ALU.mult, op1=ALU.mult),
                w=[b_yb])
            emit_yT(256, 2 * h, m)

    for hd in range(cfg.NSH):
        vi = c2["v"] % 2
        c2["v"] += 1
        for b0 in range(0, NB, 8):
            k.op("sp", lambda vi=vi, hd=hd, b0=b0: S.dma_start(
                out=vab[vi][:, b0:b0 + 8, 0:128],
                in_=io["Vs"][b0 * 128:(b0 + 8) * 128, cfg.DIFFW + 128 * hd: cfg.DIFFW + 128 * (hd + 1)].rearrange("(b p) c -> p b c", p=128)),
                reads=[b_Vs], writes=[b_vab[vi]], track=tr_vab[vi])
        k.op("pool", lambda vi=vi: P.memset(vab[vi][:, :, 128:129], 1.0), writes=[b_vab[vi]])
        bi = c2["kq"] % 4
        c2["kq"] += 1
        mpi = cfg.DIFFW // 128 + hd
        k.op("sp", lambda bi=bi, mpi=mpi: S.dma_start(out=ktb[bi][:], in_=io["KT"][mpi]),
             reads=[b_KT], writes=[b_ktb[bi]], track=tr_ktb[bi])
        k.op("sp", lambda bi=bi, mpi=mpi: S.dma_start(out=qtb[bi][:], in_=io["QT"][mpi]),
             reads=[b_QT], writes=[b_qtb[bi]], track=tr_qtb[bi])
        for m in range(NBL):
            kb_lo = max(0, 8 * m - 16)
            kbs = list(range(kb_lo, 8 * m + 8))

            def mask_of(kb0, n, m=m):
                r0 = kb0 - (8 * m - 16)
                return mdil[:, r0:r0 + n, :].rearrange("p r q -> p (r q)")
            oi = c2["o"] % 4
            c2["o"] += 1
            O1, bO1 = Ops[oi], b_O[oi]
            attend(ktb[bi], b_ktb[bi], qtb[bi], b_qtb[bi], vab[vi], b_vab[vi], 128, m, kbs, mask_of, O1, bO1)
            fin = lambda fn, r=(), w=(): k.op("dve", fn, reads=[b_fw, b_ft, b_c2] + list(r), writes=[b_fw, b_ft] + list(w))
            fin(lambda O1=O1: V.reciprocal(out=fw[:, 0:1], in_=O1[:, 128:129]), r=[bO1])
            fin(lambda O1=O1: V.tensor_scalar(out=ft[1][:, :128], in0=O1[:, 0:128], scalar1=fw[:, 0:1], scalar2=None, op0=ALU.mult),
                r=[bO1])
            fin(lambda: V.scalar_tensor_tensor(out=junk2[:, :128], in0=ft[1][:, :128], scalar=1.0, in1=ft[1][:, :128],
                                               op0=ALU.mult, op1=ALU.mult, accum_out=fw[:, 3:4]), w=[b_j2])
            rstd_from_ss(fw[:, 3:4], 128, fw[:, 4:5])
            fin(lambda hd=hd: V.scalar_tensor_tensor(out=yb[:, :128], in0=ft[1][:, :128], scalar=fw[:, 4:5],
                                                     in1=gdil[:, 128 * hd:128 * (hd + 1)], op0=ALU.mult, op1=ALU.mult), w=[b_yb])
            emit_yT(128, cfg.DIFFW // 128 + hd, m)
    end_phase(es)
    if upto == "P2":
        return finish()

    es, sb, ps = new_phase()
    yTs = sb("yTs", [128, KC, TL], BF16)
    b_yTs = Buf()
    trc = k.dtrack()
    yTv = io["yT"].rearrange("c p t -> p c t")
    for c0 in range(0, KC, 8):
        k.op("sp", lambda c0=c0: S.dma_start(out=yTs[:, c0:c0 + 8, :], in_=yTv[:, c0:c0 + 8, :]), reads=[b_yT], writes=[b_yTs], track=trc)
    g1row = sb("g1row", [128, D], F32)
    b_g1 = Buf()
    k.op("sp", lambda: S.dma_start(out=g1row[:], in_=io["modf"][2 * D:3 * D].rearrange("(o d) -> o d", o=1).to_broadcast([128, D])),
         reads=[b_modf], writes=[b_g1], track=trc)
    wof = [sb("wof%d" % i, [128, 4, 512], F32) for i in range(2)]
    b_wof = [Buf() for _ in range(2)]
    tr_wof = [k.dtrack() for _ in range(2)]
    wob = [sb("wob%d" % i, [128, KC, 512], BF16) for i in range(2)]
    b_wob = [Buf() for _ in range(2)]
    xs = [sb("xs%d" % i, [128, 512], F32) for i in range(2)]
    b_xs = [Buf() for _ in range(2)]
    tr_xs = [k.dtrack() for _ in range(2)]
    x1s = [sb("x1s%d" % i, [128, 512], F32) for i in range(2)]
    b_x1s = [Buf() for _ in range(2)]
    tr_x1s = [k.dtrack() for _ in range(2)]
    po = [ps("po%d" % i, [128, 512], F32) for i in range(2)]
    b_po = [Buf() for _ in range(2)]
    b_x1 = Buf()
    wov = io["w_out"].rearrange("(kk p) c -> p kk c", p=128)
    c3 = {"f": 0, "x": 0}
    for dc in range(D // 512):
        wi = dc % 2
        for k0 in range(0, KC, 4):
            fi = c3["f"] % 2
            c3["f"] += 1
            k.op("sp", lambda fi=fi, k0=k0, dc=dc: S.dma_start(out=wof[fi][:], in_=wov[:, k0:k0 + 4, dc * 512:(dc + 1) * 512]),
                 writes=[b_wof[fi]], track=tr_wof[fi])
            if c3["f"] % 2 == 0:
                k.op("pool", lambda fi=fi, wi=wi, k0=k0: P.tensor_copy(out=wob[wi][:, k0:k0 + 4, :], in_=wof[fi][:]),
                     reads=[b_wof[fi]], writes=[b_wob[wi]])
            else:
                k.op("act", lambda fi=fi, wi=wi, k0=k0: A.copy(out=wob[wi][:, k0:k0 + 4, :], in_=wof[fi][:]),
                     reads=[b_wof[fi]], writes=[b_wob[wi]])
        for t in range(NBL):
            xi = c3["x"] % 2
            c3["x"] += 1
            k.op("sp", lambda xi=xi, t=t, dc=dc: S.dma_start(out=xs[xi][:], in_=io["x_own"][t * 128:(t + 1) * 128, dc * 512:(dc + 1) * 512]),
                 writes=[b_xs[xi]], track=tr_xs[xi])
            for kk in range(KC):
                k.op("pe", lambda xi=xi, kk=kk, t=t, wi=wi: T.matmul(
                    po[xi][:], lhsT=yTs[:, kk, t * 128:(t + 1) * 128], rhs=wob[wi][:, kk, :],
                    start=(kk == 0), stop=(kk == KC - 1)), reads=[b_yTs, b_wob[wi]], writes=[b_po[xi]])
            k.op("dve", lambda xi=xi, dc=dc: V.tensor_tensor(out=x1s[xi][:], in0=po[xi][:], in1=g1row[:, dc * 512:(dc + 1) * 512], op=ALU.mult),
                 reads=[b_po[xi], b_g1], writes=[b_x1s[xi]])
            k.op("dve", lambda xi=xi: V.tensor_tensor(out=x1s[xi][:], in0=x1s[xi][:], in1=xs[xi][:], op=ALU.add),
                 reads=[b_xs[xi], b_x1s[xi]], writes=[b_x1s[xi]])
            k.op("pool", lambda xi=xi, t=t, dc=dc: P.dma_start(out=io["x1"][t * 128:(t + 1) * 128, dc * 512:(dc + 1) * 512], in_=x1s[xi][:]),
                 reads=[b_x1s[xi]], writes=[b_x1], track=tr_x1s[xi])
    end_phase(es)
    if upto == "P3":
        return finish()

    es4, sb4, ps4 = new_phase()
    NKH = 2 * PH
    NSL = PH * TOPK
    rstd2 = sb4("rstd2", [128, NBL], F32)
    topv = sb4("topv", [128, NBL, NKH, 16], F32)
    topi = sb4("topi", [128, NBL, NKH, 16], U32)
    eidf = sb4("eidf", [128, NBL, NSL], F32)
    eidi = sb4("eidi", [128, NBL, NSL], I32)
    gate = sb4("gate", [128, NBL, NSL], F32)
    es_ab, sb_ab, ps_ab = new_phase()
    h2T = sb_ab("h2T", [128, KC, TL], BF16)
    es, sb, ps = new_phase()
    b_h2T = Buf()
    xt = [sb("xt4_%d" % i, [128, D], F32) for i in range(2)]
    b_xt = [Buf() for _ in range(2)]
    tr_x = [k.dtrack() for _ in range(2)]
    xn = [sb("xn4_%d" % i, [128, D], BF16) for i in range(2)]
    b_xn = [Buf() for _ in range(2)]
    junk = sb("junk4", [128, D], BF16)
    b_junk = Buf()
    stat = sb("stat4", [128, 4], F32)
    b_stat = Buf()
    b_rstd2 = Buf()
    HK = KC // 2
    pT = [ps("pT4_%d" % i, [128, HK * 128], BF16) for i in range(2)]
    b_pT = [Buf() for _ in range(2)]
    cnt = {"x": 0}
    for t in range(NBL):
        i = cnt["x"] % 2
        cnt["x"] += 1
        k.op("sp", lambda i=i, t=t: S.dma_start(out=xt[i][:], in_=io["x1"][t * 128:(t + 1) * 128, :]),
             reads=[b_x1], writes=[b_xt[i]], track=tr_x[i])
        k.op("dve", lambda i=i: V.scalar_tensor_tensor(out=junk[:], in0=xt[i][:], scalar=1.0, in1=xt[i][:], op0=ALU.mult,
                                                      op1=ALU.mult, accum_out=stat[:, 0:1]), reads=[b_xt[i]], writes=[b_junk, b_stat])
        k.op("act", lambda: A.activation(out=stat[:, 1:2], in_=stat[:, 0:1], func=AF.Ln, scale=1.0 / D, bias=EPS),
             reads=[b_stat], writes=[b_stat])
        k.op("act", lambda t=t: A.activation(out=rstd2[:, t:t + 1], in_=stat[:, 1:2], func=AF.Exp, scale=-0.5),
             reads=[b_stat], writes=[b_rstd2])
        k.op("pool", lambda i=i, t=t: P.tensor_scalar(out=xn[i][:], in0=xt[i][:], scalar1=rstd2[:, t:t + 1], scalar2=None, op0=ALU.mult),
             reads=[b_xt[i], b_rstd2], writes=[b_xn[i]])
        for hh in range(2):
            for q in range(HK):
                kk = hh * HK + q
                k.op("pe", lambda i=i, kk=kk, q=q, hh=hh: T.transpose(
                    out=pT[hh][:, q * 128:(q + 1) * 128], in_=xn[i][:, kk * 128:(kk + 1) * 128], identity=ident[:]),
                    reads=[b_xn[i], b_const], writes=[b_pT[hh]])
            for q in range(HK):
                kk = hh * HK + q
                if q % 2 == 0:
                    k.op("act", lambda kk=kk, q=q, hh=hh, t=t: A.activation(
                        out=h2T[:, kk, t * 128:(t + 1) * 128], in_=pT[hh][:, q * 128:(q + 1) * 128],
                        func=AF.Identity, scale=A2[:, kk:kk + 1], bias=B2[:, kk:kk + 1]),
                        reads=[b_pT[hh], b_A, b_modT], writes=[b_h2T])
                else:
                    k.op("dve", lambda kk=kk, q=q, hh=hh, t=t: V.tensor_scalar(
                        out=h2T[:, kk, t * 128:(t + 1) * 128], in0=pT[hh][:, q * 128:(q + 1) * 128],
                        scalar1=A2[:, kk:kk + 1], scalar2=B2[:, kk:kk + 1], op0=ALU.mult, op1=ALU.add),
                        reads=[b_pT[hh], b_A, b_modT], writes=[b_h2T])
    k.barrier()
    es.close()
    es, sb, ps = new_phase()
    b_top = Buf()
    wqf = [sb("wqf%d" % i, [128, KC, 128], F32) for i in range(2)]
    b_wqf = [Buf() for _ in range(2)]
    tr_wqf = [k.dtrack() for _ in range(2)]
    wqb = [sb("wqb%d" % i, [128, KC, 128], BF16) for i in range(2)]
    b_wqb = [Buf() for _ in range(2)]
    skf = sb("skf", [128, NKH, NK], F32)
    skb = sb("skb", [128, NKH, NK], BF16)
    b_sk = Buf()
    trc = k.dtrack()
    skv = io["skT"].rearrange("j kk n -> kk j n")
    for j0 in range(0, NKH, 8):
        k.op("sp", lambda j0=j0: S.dma_start(out=skf[:, j0:j0 + 8, :], in_=skv[:, j0:j0 + 8, :]), writes=[b_sk], track=trc)
    k.op("dve", lambda: V.tensor_copy(out=skb[:], in_=skf[:]), reads=[b_sk], writes=[b_sk])
    qTs = sb("qTs", [128, TL], BF16)
    b_qTs = Buf()
    pq = [ps("pq%d" % i, [128, 512], F32) for i in range(2)]
    b_pq = [Buf() for _ in range(2)]
    psc = [ps("psc%d" % i, [128, NK], F32) for i in range(2)]
    b_psc = [Buf() for _ in range(2)]
    scs = [sb("scs%d" % i, [128, NK], F32) for i in range(2)]
    b_scs = [Buf() for _ in range(2)]
    wqv = io["peer_wq"].rearrange("(kk p) c -> p kk c", p=128)
    nn = min(512, TL)
    c4 = {"q": 0, "s": 0}
    for j in range(NKH):
        wi = j % 2
        for c0 in range(0, KC, 8):
            k.op("sp", lambda wi=wi, j=j, c0=c0: S.dma_start(out=wqf[wi][:, c0:c0 + 8, :], in_=wqv[:, c0:c0 + 8, j * 128:(j + 1) * 128]),
                 writes=[b_wqf[wi]], track=tr_wqf[wi])
        if j % 2 == 0:
            k.op("act", lambda wi=wi: A.copy(out=wqb[wi][:], in_=wqf[wi][:]), reads=[b_wqf[wi]], writes=[b_wqb[wi]])
        else:
            k.op("pool", lambda wi=wi: P.tensor_copy(out=wqb[wi][:], in_=wqf[wi][:]), reads=[b_wqf[wi]], writes=[b_wqb[wi]])
        for hf in range(TL // nn):
            qi = c4["q"] % 2
            c4["q"] += 1
            for kk in range(KC):
                k.op("pe", lambda wi=wi, kk=kk, hf=hf, qi=qi: T.matmul(
                    pq[qi][:, :nn], lhsT=wqb[wi][:, kk, :], rhs=h2T[:, kk, hf * nn:(hf + 1) * nn],
                    start=(kk == 0), stop=(kk == KC - 1)), reads=[b_wqb[wi], b_h2T], writes=[b_pq[qi]])
            k.op("act", lambda qi=qi, hf=hf: A.copy(out=qTs[:, hf * nn:(hf + 1) * nn], in_=pq[qi][:, :nn]),
                 reads=[b_pq[qi]], writes=[b_qTs])
        for t in range(NBL):
            si = c4["s"] % 2
            c4["s"] += 1
            k.op("pe", lambda si=si, t=t, j=j: T.matmul(psc[si][:], lhsT=qTs[:, t * 128:(t + 1) * 128], rhs=skb[:, j, :],
                                                        start=True, stop=True), reads=[b_qTs, b_sk], writes=[b_psc[si]])
            k.op("act", lambda si=si: A.copy(out=scs[si][:], in_=psc[si][:]), reads=[b_psc[si]], writes=[b_scs[si]])
            dv = lambda fn, si=si: k.op("dve", fn, reads=[b_scs[si], b_top], writes=[b_scs[si], b_top])
            dv(lambda si=si, t=t, j=j: V.max(out=topv[:, t, j, 0:8], in_=scs[si][:]))
            dv(lambda si=si, t=t, j=j: V.max_index(out=topi[:, t, j, 0:8], in_max=topv[:, t, j, 0:8], in_values=scs[si][:]))
            dv(lambda si=si, t=t, j=j: V.match_replace(out=scs[si][:], in_to_replace=topv[:, t, j, 0:8], in_values=scs[si][:], imm_value=-1e30))
            dv(lambda si=si, t=t, j=j: V.max(out=topv[:, t, j, 8:16], in_=scs[si][:]))
            dv(lambda si=si, t=t, j=j: V.max_index(out=topi[:, t, j, 8:16], in_max=topv[:, t, j, 8:16], in_values=scs[si][:]))
    k.barrier()
    es.close()
    es_ab.close()
    es, sb, ps = new_phase()
    b_sel = Buf()
    topif = sb("topif", [128, NBL, NKH, 16], F32)
    iota16 = sb("iota16_sb", [128, 16], F32)
    k.op("sp", lambda: S.dma_start(out=iota16[:], in_=io["iota16"]), writes=[b_sk], track=trc)
    k.op("dve", lambda: V.tensor_copy(out=topif[:], in_=topi[:]), reads=[b_top], writes=[b_top])
    cand = sb("cand", [128, 256], F32)
    cand2 = sb("cand2", [128, 256], F32)
    fv = sb("fv", [128, 16], F32)
    fpos = sb("fpos", [128, 16], U32)
    fa = sb("fa", [128, 16], U32)
    fbb = sb("fbb", [128, 16], U32)
    faf = sb("faf", [128, 16], F32)
    fbf = sb("fbf", [128, 16], F32)
    oh = sb("oh", [128, 16, 16], F32)
    sel0 = sb("sel0", [128, 16], F32)
    sel1 = sb("sel1", [128, 16], F32)
    gw = sb("gw", [128, 4], F32)
    b_w = Buf()
    for t in range(NBL):
        for h in range(PH):
            dv = lambda fn: k.op("dve", fn, reads=[b_w, b_top, b_sk], writes=[b_w])
            v0 = topv[:, t, 2 * h, :]
            v1 = topv[:, t, 2 * h + 1, :]
            i0 = topif[:, t, 2 * h, :]
            i1 = topif[:, t, 2 * h + 1, :]
            cand3 = cand[:].rearrange("p (a b) -> p a b", b=16)
            dv(lambda v0=v0, v1=v1, cand3=cand3: V.tensor_tensor(out=cand3, in0=v0.unsqueeze(2).to_broadcast([128, 16, 16]),
                                                                 in1=v1.unsqueeze(1).to_broadcast([128, 16, 16]), op=ALU.add))
            dv(lambda: V.max(out=fv[:, 0:8], in_=cand[:]))
            dv(lambda: V.max_index(out=fpos[:, 0:8], in_max=fv[:, 0:8], in_values=cand[:]))
            dv(lambda: V.match_replace(out=cand2[:], in_to_replace=fv[:, 0:8], in_values=cand[:], imm_value=-1e30))
            dv(lambda: V.max(out=fv[:, 8:16], in_=cand2[:]))
            dv(lambda: V.max_index(out=fpos[:, 8:16], in_max=fv[:, 8:16], in_values=cand2[:]))
            dv(lambda: V.tensor_single_scalar(out=fa[:], in_=fpos[:], scalar=4, op=ALU.logical_shift_right))
            dv(lambda: V.tensor_single_scalar(out=fbb[:], in_=fpos[:], scalar=15, op=ALU.bitwise_and))
            dv(lambda: V.tensor_copy(out=faf[:], in_=fa[:]))
            dv(lambda: V.tensor_copy(out=fbf[:], in_=fbb[:]))
            for (pf, ix, sel) in ((faf, i0, sel0), (fbf, i1, sel1)):
                dv(lambda pf=pf: V.tensor_tensor(out=oh[:], in0=pf[:].unsqueeze(2).to_broadcast([128, 16, 16]),
                                                 in1=iota16[:].unsqueeze(1).to_broadcast([128, 16, 16]), op=ALU.is_equal))
                dv(lambda ix=ix: V.tensor_tensor(out=oh[:], in0=oh[:], in1=ix.unsqueeze(1).to_broadcast([128, 16, 16]), op=ALU.mult))
                dv(lambda sel=sel: V.tensor_reduce(out=sel[:], in_=oh[:], axis=AX.X, op=ALU.add))
            k.op("dve", lambda t=t, h=h: V.scalar_tensor_tensor(out=eidf[:, t, h * 16:(h + 1) * 16], in0=sel0[:], scalar=float(NK),
                                                              in1=sel1[:], op0=ALU.mult, op1=ALU.add),
                 reads=[b_w], writes=[b_sel])
            dv(lambda: V.tensor_scalar(out=gw[:, 0:1], in0=fv[:, 0:1], scalar1=-1.0, scalar2=None, op0=ALU.mult))
            k.op("act", lambda: A.activation(out=cand2[:, 0:16], in_=fv[:], func=AF.Exp, bias=gw[:, 0:1], scale=1.0,
                                             accum_out=gw[:, 1:2]), reads=[b_w], writes=[b_w])
            dv(lambda: V.reciprocal(out=gw[:, 2:3], in_=gw[:, 1:2]))
            k.op("dve", lambda t=t, h=h: V.tensor_scalar(out=gate[:, t, h * 16:(h + 1) * 16], in0=cand2[:, 0:16],
                                                       scalar1=gw[:, 2:3], scalar2=None, op0=ALU.mult),
                 reads=[b_w], writes=[b_sel])
    k.op("dve", lambda: V.tensor_copy(out=eidi[:], in_=eidf[:]), reads=[b_sel], writes=[b_sel])
    if debug:
        dout("dbg_eid", [TL, NSL], I32)
        dout("dbg_gate", [TL, NSL], F32)
        k.op("sp", lambda: S.dma_start(out=io["dbg_eid"].rearrange("(t p) s -> p t s", p=128), in_=eidi[:]), reads=[b_sel], track=trc)
        k.op("sp", lambda: S.dma_start(out=io["dbg_gate"].rearrange("(t p) s -> p t s", p=128), in_=gate[:]), reads=[b_sel], track=trc)
    k.barrier()
    es.close()
    es, sb, ps = new_phase()
    xt = [sb("xt4d", [128, D], F32)]
    b_xt = [Buf()]
    junk = sb("junk4d", [128, D], BF16)
    b_junk = Buf()
    stat = sb("stat4d", [128, 4], F32)
    b_stat = Buf()
    A2row = sb("A2row", [128, D], F32)
    B2row = sb("B2row", [128, D], F32)
    g2row = sb("g2row", [128, D], F32)
    fgrow = sb("fgrow", [128, D], F32)
    b_rows = Buf()
    mrow = lambda q: io["modf"][q * D:(q + 1) * D].rearrange("(o d) -> o d", o=1).to_broadcast([128, D])
    k.op("sp", lambda: S.dma_start(out=A2row[:], in_=mrow(4)), reads=[b_modf], writes=[b_rows], track=trc)
    k.op("sp", lambda: S.dma_start(out=B2row[:], in_=mrow(3)), reads=[b_modf], writes=[b_rows], track=trc)
    k.op("sp", lambda: S.dma_start(out=g2row[:], in_=mrow(5)), reads=[b_modf], writes=[b_rows], track=trc)
    k.op("sp", lambda: S.dma_start(out=fgrow[:], in_=io["n2g_row"].to_broadcast([128, D])), writes=[b_rows], track=trc)
    k.op("dve", lambda: V.scalar_tensor_tensor(out=A2row[:], in0=A2row[:], scalar=1.0, in1=fgrow[:], op0=ALU.add, op1=ALU.mult),
         reads=[b_rows], writes=[b_rows])
    b_fg = Buf()
    k.op("sp", lambda: S.dma_start(out=fgrow[:], in_=io["fg_row"].to_broadcast([128, D])), reads=[b_rows], writes=[b_rows, b_fg], track=trc)
    h2 = sb("h2", [128, D], BF16)
    b_h2 = Buf()
    ug = [sb("ug%d" % i, [128, D], BF16) for i in range(3)]
    b_ug = [Buf() for _ in range(3)]
    tr_ug = [k.dtrack() for _ in range(3)]
    pre = sb("pre", [128, NSL], F32)
    coef = sb("coef", [128, NSL], F32)
    b_pre = Buf()
    dg = [sb("dg%d" % i, [128, 128], BF16) for i in range(2)]
    b_dg = [Buf() for _ in range(2)]
    NPB = D // 512
    pacc = [ps("pacc%d" % i, [128, 512], F32) for i in range(min(NPB, 8))]
    b_pacc = Buf()
    x2 = sb("x2", [128, D], F32)
    b_x2 = Buf()
    tr_o = k.dtrack()
    FW = min(D, 2048)
    NA = D // FW
    NA, FW = 1, D
    uv = io["u_bf"]
    vv = io["v_bf"]
    eidx = [eidi]
    c5 = {"g": 0, "d": 0}
    NPS = len(pacc)
    for t in range(NBL):
        i = 0
        k.op("sp", lambda i=i, t=t: S.dma_start(out=xt[i][:], in_=io["x1"][t * 128:(t + 1) * 128, :]),
             reads=[b_x1], writes=[b_xt[i]], track=tr_x[i])
        k.op("dve", lambda i=i, t=t: V.scalar_tensor_tensor(out=x2[:], in0=xt[i][:], scalar=rstd2[:, t:t + 1], in1=A2row[:],
                                                          op0=ALU.mult, op1=ALU.mult), reads=[b_xt[i], b_rstd2, b_rows], writes=[b_x2])
        k.op("dve", lambda: V.tensor_tensor(out=h2[:], in0=x2[:], in1=B2row[:], op=ALU.add), reads=[b_x2, b_rows], writes=[b_h2])
        for s in range(NSL):
            gi = c5["g"] % 3
            c5["g"] += 1
            for a in range(NA):
                k.op("pool", lambda gi=gi, t=t, s=s, a=a: P.indirect_dma_start(
                    out=ug[gi][:, a * FW:(a + 1) * FW], out_offset=None, in_=uv,
                    in_offset=bass.IndirectOffsetOnAxis(ap=eidx[a][:, t, s:s + 1], axis=0)),
                    reads=[b_sel, b_tabs], writes=[b_ug[gi]], track=tr_ug[gi])
            k.op("dve", lambda gi=gi, s=s: V.scalar_tensor_tensor(out=junk[:], in0=ug[gi][:], scalar=1.0, in1=h2[:], op0=ALU.mult,
                                                                op1=ALU.mult, accum_out=pre[:, s:s + 1]),
                 reads=[b_ug[gi], b_h2], writes=[b_junk, b_pre])
        k.op("act", lambda: A.activation(out=coef[:], in_=pre[:], func=AF.Gelu), reads=[b_pre], writes=[b_pre])
        k.op("dve", lambda t=t: V.tensor_tensor(out=coef[:], in0=coef[:], in1=gate[:, t, :], op=ALU.mult), reads=[b_pre, b_sel], writes=[b_pre])
        if debug and t == 0:
            dout("dbg_pre", [128, NSL], F32)
            dout("dbg_coef", [128, NSL], F32)
            dout("dbg_h2", [128, D], BF16)
            dout("dbg_ug", [128, D], BF16)
            k.op("sp", lambda: S.dma_start(out=io["dbg_pre"], in_=pre[:]), reads=[b_pre], track=trc)
            k.op("sp", lambda: S.dma_start(out=io["dbg_coef"], in_=coef[:]), reads=[b_pre], track=trc)
            k.op("sp", lambda: S.dma_start(out=io["dbg_h2"], in_=h2[:]), reads=[b_h2], track=trc)
            k.op("sp", lambda: S.dma_start(out=io["dbg_ug"], in_=ug[(c5["g"] - 1) % 3][:]), reads=b_ug, track=trc)
        for g0 in range(0, NPB, NPS):
            for s in range(NSL):
                gi = c5["g"] % 3
                c5["g"] += 1
                di = c5["d"] % 2
                c5["d"] += 1
                for a in range(NA):
                    k.op("pool", lambda gi=gi, t=t, s=s, a=a: P.indirect_dma_start(
                        out=ug[gi][:, a * FW:(a + 1) * FW], out_offset=None, in_=vv,
                        in_offset=bass.IndirectOffsetOnAxis(ap=eidx[a][:, t, s:s + 1], axis=0)),
                        reads=[b_sel, b_tabs], writes=[b_ug[gi]], track=tr_ug[gi])
                k.op("act", lambda di=di, s=s: A.activation(out=dg[di][:], in_=ident[:], func=AF.Copy, scale=coef[:, s:s + 1]),
                     reads=[b_pre, b_const], writes=[b_dg[di]])
                for q in range(g0, min(NPB, g0 + NPS)):
                    k.op("pe", lambda q=q, gi=gi, di=di, s=s, g0=g0: T.matmul(
                        pacc[q - g0][:], lhsT=dg[di][:], rhs=ug[gi][:, q * 512:(q + 1) * 512],
                        start=(s == 0), stop=(s == NSL - 1)), reads=[b_dg[di], b_ug[gi]], writes=[b_pacc])
            for q in range(g0, min(NPB, g0 + NPS)):
                sl = slice(q * 512, (q + 1) * 512)
                k.op("dve", lambda q=q, sl=sl, g0=g0: V.tensor_tensor(out=x2[:, sl], in0=pacc[q - g0][:], in1=g2row[:, sl], op=ALU.mult),
                     reads=[b_pacc, b_rows], writes=[b_x2])
        if debug and t == 0:
            dout("dbg_x2", [128, D], F32)
            k.op("sp", lambda: S.dma_start(out=io["dbg_x2"], in_=x2[:]), reads=[b_x2], track=trc)
        k.op("dve", lambda i=i: V.tensor_tensor(out=x2[:], in0=x2[:], in1=xt[i][:], op=ALU.add), reads=[b_x2, b_xt[i]], writes=[b_x2])
        k.op("dve", lambda: V.scalar_tensor_tensor(out=junk[:], in0=x2[:], scalar=1.0, in1=x2[:], op0=ALU.mult, op1=ALU.mult,
                                                   accum_out=stat[:, 0:1]), reads=[b_x2], writes=[b_junk, b_stat])
        k.op("act", lambda: A.activation(out=stat[:, 1:2], in_=stat[:, 0:1], func=AF.Ln, scale=1.0 / D, bias=EPS),
             reads=[b_stat], writes=[b_stat])
        k.op("act", lambda: A.activation(out=stat[:, 2:3], in_=stat[:, 1:2], func=AF.Exp, scale=-0.5), reads=[b_stat], writes=[b_stat])
        k.op("dve", lambda: V.scalar_tensor_tensor(out=x2[:], in0=x2[:], scalar=stat[:, 2:3], in1=fgrow[:], op0=ALU.mult, op1=ALU.mult),
             reads=[b_x2, b_stat, b_fg], writes=[b_x2])
        k.op("sp", lambda t=t: S.dma_start(out=io["out"][t * 128:(t + 1) * 128, :], in_=x2[:]), reads=[b_x2], track=tr_o)
    k.barrier()
    es.close()
    end_phase(es4)
    return finish()


def _dil_mult(d):
    m = ((d >= 0) & (d <= 128)).astype(np.float32)
    m += ((d >= 0) & (d <= 512) & (d % 4 == 0)).astype(np.float32)
    m += ((d >= 0) & (d <= 2048) & (d % 16 == 0)).astype(np.float32)
    return m


def make_in_maps(inputs, cfg=None):
    cfg = cfg or Cfg()
    D, SEQ, KC, TL, NBL, NK = cfg.D, cfg.SEQ, cfg.KC, cfg.TL, cfg.NBL, cfg.NK
    f = lambda a: np.ascontiguousarray(np.asarray(a))
    x = np.asarray(inputs["x"])[0]
    pos = np.asarray(inputs["positions"])[0].astype(np.int32)
    w_in = np.asarray(inputs["w_in"])[0]
    DW, LW = cfg.DIFFW, cfg.DILW
    sh = {}
    sh["x_all"] = f(x)
    sh["pos_all"] = f(pos.reshape(1, SEQ))
    sh["c_row"] = f(np.asarray(inputs["c"]).reshape(1, D))
    sh["w_adaT"] = f(np.asarray(inputs["w_ada"])[0].T).reshape(cfg.NMOD, 128, D)
    sh["b_adaT"] = f(np.asarray(inputs["b_ada"])[0].reshape(cfg.NMOD, 128).T)
    sh["n1gT"] = f(np.asarray(inputs["norm1_g"])[0].reshape(KC, 128).T)
    sh["n2gT"] = f(np.asarray(inputs["norm2_g"])[0].reshape(KC, 128).T)
    sh["n2g_row"] = f(np.asarray(inputs["norm2_g"])[0].reshape(1, D))
    sh["fg_row"] = f(np.asarray(inputs["final_g"]).reshape(1, D))
    sh["w_q"] = f(np.concatenate([w_in[:, 0:DW], w_in[:, 3 * DW:3 * DW + LW]], axis=1))
    sh["w_k"] = f(np.concatenate([w_in[:, DW:2 * DW], w_in[:, 3 * DW + LW:3 * DW + 2 * LW]], axis=1))
    sh["w_v"] = f(np.concatenate([w_in[:, 2 * DW:3 * DW], w_in[:, 3 * DW + 2 * LW:3 * DW + 3 * LW]], axis=1))
    sh["w_out"] = f(np.asarray(inputs["w_out"])[0])
    sh["lam4"] = f(np.stack([np.asarray(inputs[n])[0] for n in ("lam_q1", "lam_k1", "lam_q2", "lam_k2")]))
    sh["subln_row"] = f(np.asarray(inputs["diff_subln_g"])[0].reshape(1, 256))
    sh["dilg_row"] = f(np.asarray(inputs["dil_out_g"])[0].reshape(1, LW))
    sh["peer_wq"] = f(np.asarray(inputs["peer_wq"])[0])
    sk = np.asarray(inputs["peer_subkeys"])[0]
    sh["skT"] = f(sk.reshape(2 * PH, NK, 128).transpose(0, 2, 1))
    sh["peer_u"] = f(np.asarray(inputs["peer_u"])[0])
    sh["peer_v"] = f(np.asarray(inputs["peer_v"])[0])
    bf = ml_dtypes.bfloat16
    sh["ident"] = np.eye(128, dtype=np.float32).astype(bf)
    pm = np.zeros((128, 128), np.float32)
    for m_ in range(16):
        pm[m_ + 16, m_] = -1.0
        pm[m_, m_ + 16] = 1.0
    sh["perm"] = pm.astype(bf)
    inv = np.power(np.float32(500000.0), -np.arange(0, 32, 2, dtype=np.float32) / np.float32(32)).astype(np.float32)
    sh["invf"] = f(np.tile(inv, 2).reshape(32, 1))
    sh["iota16"] = f(np.tile(np.arange(16, dtype=np.float32), (128, 1)))
    ki = np.arange(128)[:, None]
    qi = np.arange(128)[None, :]
    maps = []
    for c in range(NCORES):
        m = dict(sh)
        m["x_own"] = f(x.reshape(cfg.NB, 128, D)[c::NCORES].reshape(TL, D))
        m["pos_own"] = f(pos.reshape(cfg.NB, 128)[c::NCORES].reshape(1, TL))
        md = np.zeros((128, 8, 128), np.float32)
        for r in range(8):
            d = (c - r) * 128 + qi - ki
            md[:, r, :] = (d >= 0)
        m["mdiff"] = md.astype(bf)
        ml = np.zeros((128, 24, 128), np.float32)
        for r in range(24):
            d = (16 + c - r) * 128 + qi - ki
            ml[:, r, :] = _dil_mult(d)
        m["mdil"] = ml.astype(bf)
        maps.append(m)
    return maps


def assemble(results, cfg=None):
    cfg = cfg or Cfg()
    out = np.zeros((cfg.NB, 128, cfg.D), np.float32)
    for c in range(NCORES):
        out[c::NCORES] = np.asarray(results[c]["out"]).reshape(cfg.NBL, 128, cfg.D)
    return out.reshape(1, cfg.SEQ, cfg.D)


def kernel(**inputs):
    cfg = Cfg()
    nc = build(cfg)
    in_maps = make_in_maps(inputs, cfg)
    res = run_bass_kernel_spmd(nc, in_maps, core_ids=list(range(NCORES)))
    return assemble(res.results, cfg)
```

```python
from contextlib import ExitStack
import math
import numpy as np
import ml_dtypes
import concourse.bass as bass
import concourse.mybir as mybir
from concourse.bass_utils import run_bass_kernel_spmd

F32 = mybir.dt.float32
BF16 = mybir.dt.bfloat16
I32 = mybir.dt.int32
U32 = mybir.dt.uint32
AF = mybir.ActivationFunctionType
ALU = mybir.AluOpType
AX = mybir.AxisListType

NCORES = 8
EPS = 1e-6
LAM_INIT = 0.2
TOPK = 16
PH = 8
SCALE = 128 ** -0.5
TWO_PI = 2.0 * math.pi
C1 = 6.28125
C2 = TWO_PI - C1


class Cfg:
    def __init__(self, D=4096, SEQ=8192, NK=128):
        self.D, self.SEQ, self.NK = D, SEQ, NK
        self.KC = D // 128
        self.DIFFW = D // 2
        self.DILW = D - self.DIFFW
        self.NDH = self.DIFFW // 256
        self.NSH = self.DILW // 128
        self.NB = SEQ // 128
        self.NBL = self.NB // NCORES
        self.TL = self.NBL * 128
        self.NEXP = NK * NK
        self.NMOD = 6 * self.KC
        self.CH = min(512, SEQ)
        self.NCH = SEQ // self.CH


class Track:
    def __init__(self, nc, name, unit):
        self.sem = nc.semaphore(name).__enter__()
        self.unit = unit
        self.cnt = 0


class Buf:
    __slots__ = ("w", "r")

    def __init__(self):
        self.w = {}
        self.r = {}


class Eng:
    def __init__(self, name, h, track):
        self.name, self.h, self.track, self.waited = name, h, track, {}


class K:
    def __init__(self, nc, n_dma_tracks=48):
        self.nc = nc
        self.eng = {}
        for name, h in (("pe", nc.tensor), ("act", nc.scalar), ("dve", nc.vector),
                        ("pool", nc.gpsimd), ("sp", nc.sync)):
            self.eng[name] = Eng(name, h, Track(nc, "t_" + name, 1))
        self.free_dma = [Track(nc, "d%d" % i, 16) for i in range(n_dma_tracks)]
        self.used_dma = []
        self.all_tracks = [e.track for e in self.eng.values()] + list(self.free_dma)

    def dtrack(self):
        t = self.free_dma.pop()
        self.used_dma.append(t)
        return t

    def release_dma(self):
        self.free_dma.extend(self.used_dma)
        self.used_dma = []

    def _wait(self, e, deps):
        for tr, c in deps.items():
            if e.waited.get(tr, 0) < c:
                e.h.wait_ge(tr.sem, c * tr.unit)
                e.waited[tr] = c

    def op(self, eng, fn, reads=(), writes=(), track=None):
        e = self.eng[eng]
        tr = track or e.track
        deps = {}
        for b in reads:
            for t, c in b.w.items():
                if deps.get(t, 0) < c:
                    deps[t] = c
        for b in writes:
            for t, c in b.w.items():
                if deps.get(t, 0) < c:
                    deps[t] = c
            for t, c in b.r.items():
                if deps.get(t, 0) < c:
                    deps[t] = c
        if eng == "pe":
            deps.pop(e.track, None)
        self._wait(e, deps)
        ins = fn()
        ins.then_inc(tr.sem, tr.unit)
        tr.cnt += 1
        for b in reads:
            b.r[tr] = tr.cnt
        for b in writes:
            b.w[tr] = tr.cnt

    def barrier(self):
        for e in self.eng.values():
            self._wait(e, {t: t.cnt for t in self.all_tracks if t.cnt > 0})


def build(cfg=None, debug=False, upto="all"):
    cfg = cfg or Cfg()
    D, SEQ, KC, TL, NB, NBL, NK = cfg.D, cfg.SEQ, cfg.KC, cfg.TL, cfg.NB, cfg.NBL, cfg.NK
    nc = bass.Bass("TRN2", target_bir_lowering=False)
    k = K(nc)
    io = {}
    V, A, P, T, S = nc.vector, nc.scalar, nc.gpsimd, nc.tensor, nc.sync
    DBG = ("modf", "KT", "QT", "Vs", "yT", "x1") if debug else ()

    def din(name, shape, dt):
        io[name] = nc.dram_tensor(name, list(shape), dt, kind="ExternalInput").ap()

    def dout(name, shape, dt):
        io[name] = nc.dram_tensor(name, list(shape), dt, kind="ExternalOutput").ap()

    def dint(name, shape, dt):
        kind = "ExternalOutput" if name in DBG else "Internal"
        io[name] = nc.dram_tensor(name, list(shape), dt, kind=kind).ap()

    din("x_all", [SEQ, D], F32)
    din("x_own", [TL, D], F32)
    din("pos_all", [1, SEQ], I32)
    din("pos_own", [1, TL], I32)
    din("c_row", [1, D], F32)
    din("w_adaT", [cfg.NMOD, 128, D], F32)
    din("b_adaT", [128, cfg.NMOD], F32)
    din("n1gT", [128, KC], F32)
    din("n2gT", [128, KC], F32)
    din("n2g_row", [1, D], F32)
    din("fg_row", [1, D], F32)
    din("w_q", [D, D], F32)
    din("w_k", [D, D], F32)
    din("w_v", [D, D], F32)
    din("w_out", [D, D], F32)
    din("lam4", [4, 128], F32)
    din("subln_row", [1, 256], F32)
    din("dilg_row", [1, cfg.DILW], F32)
    din("peer_wq", [D, PH * 256], F32)
    din("skT", [2 * PH, 128, NK], F32)
    din("peer_u", [cfg.NEXP, D], F32)
    din("peer_v", [cfg.NEXP, D], F32)
    din("ident", [128, 128], BF16)
    din("perm", [128, 128], BF16)
    din("invf", [32, 1], F32)
    din("mdiff", [128, 8, 128], BF16)
    din("mdil", [128, 24, 128], BF16)
    din("iota16", [128, 16], F32)
    dout("out", [TL, D], F32)

    dint("modf", [cfg.NMOD * 128], F32)
    dint("wq_bf", [KC, 128, KC, 128], BF16)
    dint("wk_bf", [KC, 128, KC, 128], BF16)
    dint("wv_bf", [D // 256, 128, KC, 256], BF16)
    dint("KT", [KC, 128, SEQ], BF16)
    dint("QT", [KC, 128, TL], BF16)
    dint("Vs", [SEQ, D], BF16)
    dint("yT", [KC, 128, TL], BF16)
    dint("x1", [TL, D], F32)
    dint("u_bf", [cfg.NEXP, D], BF16)
    dint("v_bf", [cfg.NEXP, D], BF16)

    gs = ExitStack()

    def gsb(name, shape, dt):
        return gs.enter_context(nc.sbuf_tensor(name, list(shape), dt))

    ident = gsb("ident_sb", [128, 128], BF16)
    perm = gsb("perm_sb", [128, 128], BF16)
    modT = gsb("modT", [128, cfg.NMOD], F32)
    A1 = gsb("A1", [128, KC], F32)
    A2 = gsb("A2", [128, KC], F32)
    invf = gsb("invf_sb", [32, 1], F32)
    b_const, b_modT, b_A = Buf(), Buf(), Buf()
    tr0 = k.free_dma.pop()
    k.op("sp", lambda: S.dma_start(out=ident[:], in_=io["ident"]), writes=[b_const], track=tr0)
    k.op("sp", lambda: S.dma_start(out=perm[:], in_=io["perm"]), writes=[b_const], track=tr0)
    k.op("sp", lambda: S.dma_start(out=invf[:], in_=io["invf"]), writes=[b_const], track=tr0)

    def new_phase():
        es = ExitStack()

        def sb(name, shape, dt):
            return es.enter_context(nc.sbuf_tensor(name, list(shape), dt))

        def ps(name, shape, dt):
            return es.enter_context(nc.psum_tensor(name, list(shape), dt))
        return es, sb, ps

    def end_phase(es):
        k.barrier()
        es.close()
        k.release_dma()

    def finish():
        k.barrier()
        gs.close()
        return nc

    es, sb, ps = new_phase()
    cact = sb("cact", [128, D], F32)
    b_cact = Buf()
    trc = k.dtrack()
    k.op("sp", lambda: S.dma_start(out=cact[:], in_=io["c_row"].to_broadcast([128, D])), writes=[b_cact], track=trc)
    k.op("act", lambda: A.activation(out=cact[:], in_=cact[:], func=AF.Silu), reads=[b_cact], writes=[b_cact])
    junk = sb("junk0", [128, D], BF16)
    b_junk = Buf()
    badaT = sb("badaT", [128, cfg.NMOD], F32)
    n1gT = sb("n1gT_sb", [128, KC], F32)
    n2gT = sb("n2gT_sb", [128, KC], F32)
    b_ld = Buf()
    k.op("sp", lambda: S.dma_start(out=badaT[:], in_=io["b_adaT"]), writes=[b_ld], track=trc)
    k.op("sp", lambda: S.dma_start(out=n1gT[:], in_=io["n1gT"]), writes=[b_ld], track=trc)
    k.op("sp", lambda: S.dma_start(out=n2gT[:], in_=io["n2gT"]), writes=[b_ld], track=trc)
    NWB = 4
    wa = [sb("wa%d" % i, [128, D], F32) for i in range(NWB)]
    b_wa = [Buf() for _ in range(NWB)]
    tr_wa = [k.dtrack() for _ in range(NWB)]
    for j in range(cfg.NMOD):
        i = j % NWB
        k.op("sp", lambda i=i, j=j: S.dma_start(out=wa[i][:], in_=io["w_adaT"][j]), writes=[b_wa[i]], track=tr_wa[i])
        k.op("dve", lambda i=i, j=j: V.scalar_tensor_tensor(
            out=junk[:], in0=wa[i][:], scalar=1.0, in1=cact[:], op0=ALU.mult, op1=ALU.mult,
            accum_out=modT[:, j:j + 1]), reads=[b_wa[i], b_cact], writes=[b_junk, b_modT])
    k.op("dve", lambda: V.tensor_tensor(out=modT[:], in0=modT[:], in1=badaT[:], op=ALU.add),
         reads=[b_ld, b_modT], writes=[b_modT])
    k.op("dve", lambda: V.scalar_tensor_tensor(out=A1[:], in0=modT[:, KC:2 * KC], scalar=1.0, in1=n1gT[:],
                                               op0=ALU.add, op1=ALU.mult), reads=[b_modT, b_ld], writes=[b_A])
    k.op("dve", lambda: V.scalar_tensor_tensor(out=A2[:], in0=modT[:, 4 * KC:5 * KC], scalar=1.0, in1=n2gT[:],
                                               op0=ALU.add, op1=ALU.mult), reads=[b_modT, b_ld], writes=[b_A])
    B1 = modT[:, 0:KC]
    B2 = modT[:, 3 * KC:4 * KC]
    b_modf = Buf()
    with nc.allow_non_contiguous_dma(reason="one-off mod vector relayout"):
        modfv = io["modf"].rearrange("(j p) -> p j", p=128)
        for j0 in range(0, cfg.NMOD, 8):
            k.op("sp", lambda j0=j0: S.dma_start(out=modfv[:, j0:j0 + 8], in_=modT[:, j0:j0 + 8]),
                 reads=[b_modT], writes=[b_modf], track=trc)
    end_phase(es)
    if upto == "P0":
        return finish()

    es, sb, ps = new_phase()
    b_wbf = Buf()
    stg = [sb("pc_f%d" % i, [128, 2, D], F32) for i in range(2)]
    stb = [sb("pc_b%d" % i, [128, 2, D], BF16) for i in range(2)]
    b_stg = [Buf() for _ in range(2)]
    b_stb = [Buf() for _ in range(2)]
    tr_stg = [k.dtrack() for _ in range(2)]
    tr_stb = [k.dtrack() for _ in range(2)]
    it = 0
    for (src, dst, cw) in (("w_q", "wq_bf", 128), ("w_k", "wk_bf", 128), ("w_v", "wv_bf", 256)):
        srcv = io[src].rearrange("(kk p) c -> p kk c", p=128)
        dstv = io[dst].rearrange("m p kk c -> p kk m c")
        for k0 in range(0, KC, 2):
            i = it % 2
            it += 1
            k.op("sp", lambda i=i, k0=k0, srcv=srcv: S.dma_start(out=stg[i][:], in_=srcv[:, k0:k0 + 2, :]),
                 writes=[b_stg[i]], track=tr_stg[i])
            eng = ("act", "pool", "dve")[it % 3]
            if eng == "act":
                k.op("act", lambda i=i: A.copy(out=stb[i][:], in_=stg[i][:]), reads=[b_stg[i]], writes=[b_stb[i]])
            elif eng == "pool":
                k.op("pool", lambda i=i: P.tensor_copy(out=stb[i][:], in_=stg[i][:]), reads=[b_stg[i]], writes=[b_stb[i]])
            else:
                k.op("dve", lambda i=i: V.tensor_copy(out=stb[i][:], in_=stg[i][:]), reads=[b_stg[i]], writes=[b_stb[i]])
            nm = D // cw
            for kk in range(2):
                ms = min(8, nm)
                for m0 in range(0, nm, ms):
                    k.op("sp", lambda i=i, kk=kk, k0=k0, dstv=dstv, cw=cw, m0=m0, ms=ms: S.dma_start(
                        out=dstv[:, k0 + kk, m0:m0 + ms],
                        in_=stb[i][:, kk, m0 * cw:(m0 + ms) * cw].rearrange("p (m c) -> p m c", c=cw)),
                        reads=[b_stb[i]], writes=[b_wbf], track=tr_stb[i])
    end_phase(es)

    es, sb, ps = new_phase()
    CHm = cfg.CH
    hT = sb("hT", [128, KC, CHm], BF16)
    b_hT = Buf()
    xt = [sb("xt%d" % i, [128, D], F32) for i in range(2)]
    b_xt = [Buf() for _ in range(2)]
    tr_x = [k.dtrack() for _ in range(2)]
    xn = [sb("xn%d" % i, [128, D], BF16) for i in range(2)]
    b_xn = [Buf() for _ in range(2)]
    junk = sb("junk1", [128, D], BF16)
    b_junk = Buf()
    stat = sb("stat1", [128, 4], F32)
    b_stat = Buf()
    HK = KC // 2
    pT = [ps("pT%d" % i, [128, HK * 128], BF16) for i in range(2)]
    b_pT = [Buf() for _ in range(2)]
    pp = [ps("pp%d" % i, [128, 512], F32) for i in range(3)]
    b_pp = [Buf() for _ in range(3)]
    psw = ps("psw", [128, 512], F32)
    b_psw = Buf()
    posi = sb("posi", [32, CHm], I32)
    rw = [sb("rw%d" % i, [32, CHm], F32) for i in range(5)]
    rki = sb("rki", [32, CHm], I32)
    cos_t = sb("cos_t", [32, CHm], F32)
    sin_t = sb("sin_t", [32, CHm], F32)
    b_rot, b_tab = Buf(), Buf()
    tr_pos = k.dtrack()
    wkb = [sb("wkb%d" % i, [128, KC, 128], BF16) for i in range(2)]
    b_wkb = [Buf() for _ in range(2)]
    tr_wkb = [k.dtrack() for _ in range(2)]
    wvb = [sb("wvb%d" % i, [128, KC, 256], BF16) for i in range(2)]
    b_wvb = [Buf() for _ in range(2)]
    tr_wvb = [k.dtrack() for _ in range(2)]
    kst = [sb("kst%d" % i, [128, 512], BF16) for i in range(3)]
    b_kst = [Buf() for _ in range(3)]
    tr_kst = [k.dtrack() for _ in range(3)]
    vst = [sb("vst%d" % i, [128, 256], BF16) for i in range(3)]
    b_vst = [Buf() for _ in range(3)]
    tr_vst = [k.dtrack() for _ in range(3)]
    rt = [sb("rt%d" % i, [32, 512], F32) for i in range(2)]
    b_rt = Buf()
    b_KT, b_QT, b_Vs = Buf(), Buf(), Buf()
    cnt = {"x": 0, "pp": 0, "kst": 0, "vst": 0, "wk": 0, "wv": 0, "ev": 0}

    def rms_to_hT(xsrc, ntok, Acol, Bcol, hT_t, b_hT_t, readsA):
        for t in range(ntok // 128):
            i = cnt["x"] % 2
            cnt["x"] += 1
            k.op("sp", lambda i=i, t=t: S.dma_start(out=xt[i][:], in_=xsrc[t * 128:(t + 1) * 128, :]),
                 writes=[b_xt[i]], track=tr_x[i])
            k.op("dve", lambda i=i: V.scalar_tensor_tensor(
                out=junk[:], in0=xt[i][:], scalar=1.0, in1=xt[i][:], op0=ALU.mult, op1=ALU.mult,
                accum_out=stat[:, 0:1]), reads=[b_xt[i]], writes=[b_junk, b_stat])
            k.op("act", lambda: A.activation(out=stat[:, 1:2], in_=stat[:, 0:1], func=AF.Ln, scale=1.0 / D, bias=EPS),
                 reads=[b_stat], writes=[b_stat])
            k.op("act", lambda: A.activation(out=stat[:, 2:3], in_=stat[:, 1:2], func=AF.Exp, scale=-0.5),
                 reads=[b_stat], writes=[b_stat])
            k.op("pool", lambda i=i: P.tensor_scalar(out=xn[i][:], in0=xt[i][:], scalar1=stat[:, 2:3], scalar2=None,
                                                    op0=ALU.mult), reads=[b_xt[i], b_stat], writes=[b_xn[i]])
            for hh in range(2):
                for q in range(HK):
                    kk = hh * HK + q
                    k.op("pe", lambda i=i, kk=kk, q=q, hh=hh: T.transpose(
                        out=pT[hh][:, q * 128:(q + 1) * 128], in_=xn[i][:, kk * 128:(kk + 1) * 128], identity=ident[:]),
                        reads=[b_xn[i], b_const], writes=[b_pT[hh]])
                for q in range(HK):
                    kk = hh * HK + q
                    if q % 2 == 0:
                        k.op("act", lambda kk=kk, q=q, hh=hh, t=t: A.activation(
                            out=hT_t[:, kk, t * 128:(t + 1) * 128], in_=pT[hh][:, q * 128:(q + 1) * 128],
                            func=AF.Identity, scale=Acol[:, kk:kk + 1], bias=Bcol[:, kk:kk + 1]),
                            reads=[b_pT[hh]] + readsA, writes=[b_hT_t])
                    else:
                        k.op("dve", lambda kk=kk, q=q, hh=hh, t=t: V.tensor_scalar(
                            out=hT_t[:, kk, t * 128:(t + 1) * 128], in0=pT[hh][:, q * 128:(q + 1) * 128],
                            scalar1=Acol[:, kk:kk + 1], scalar2=Bcol[:, kk:kk + 1], op0=ALU.mult, op1=ALU.add),
                            reads=[b_pT[hh]] + readsA, writes=[b_hT_t])

    def rot_tables(pos_ap, ntok):
        k.op("sp", lambda: S.dma_start(out=posi[:, :ntok], in_=pos_ap.to_broadcast([32, ntok])),
             reads=[b_rot], writes=[b_rot], track=tr_pos)
        ang, a, y, kf, r = [w[:, :ntok] for w in rw]
        dv = lambda fn: k.op("dve", fn, reads=[b_rot, b_const], writes=[b_rot])
        dv(lambda: V.tensor_copy(out=ang, in_=posi[:, :ntok]))
        dv(lambda: V.tensor_scalar(out=ang, in0=ang, scalar1=invf[:, 0:1], scalar2=None, op0=ALU.mult))
        for tab, shift in ((sin_t, 0.0), (cos_t, 0.5 * math.pi)):
            dv(lambda shift=shift: V.tensor_scalar(out=a, in0=ang, scalar1=shift, scalar2=None, op0=ALU.add))
            dv(lambda: V.tensor_scalar(out=y, in0=a, scalar1=1.0 / TWO_PI, scalar2=0.5, op0=ALU.mult, op1=ALU.add))
            dv(lambda: V.tensor_copy(out=rki[:, :ntok], in_=y))
            dv(lambda: V.tensor_copy(out=kf, in_=rki[:, :ntok]))
            dv(lambda: V.scalar_tensor_tensor(out=r, in0=kf, scalar=-C1, in1=a, op0=ALU.mult, op1=ALU.add))
            dv(lambda: V.scalar_tensor_tensor(out=r, in0=kf, scalar=-C2, in1=r, op0=ALU.mult, op1=ALU.add))
            dv(lambda: V.tensor_scalar(out=y, in0=r, scalar1=-math.pi, scalar2=None, op0=ALU.is_lt))
            dv(lambda: V.scalar_tensor_tensor(out=r, in0=y, scalar=TWO_PI, in1=r, op0=ALU.mult, op1=ALU.add))
            dv(lambda: V.tensor_scalar(out=y, in0=r, scalar1=math.pi, scalar2=None, op0=ALU.is_gt))
            dv(lambda: V.scalar_tensor_tensor(out=r, in0=y, scalar=-TWO_PI, in1=r, op0=ALU.mult, op1=ALU.add))
            dv(lambda: V.tensor_scalar(out=r, in0=r, scalar1=-3.14159, scalar2=3.14159, op0=ALU.max, op1=ALU.min))
            k.op("act", lambda tab=tab: A.activation(out=tab[:, :ntok], in_=r, func=AF.Sin),
                 reads=[b_rot], writes=[b_tab])

    def proj_rot(wsrc, dst, b_dst, ntok, tok0):
        nn = min(512, ntok)
        for mp in range(KC):
            wi = cnt["wk"] % 2
            cnt["wk"] += 1
            k.op("sp", lambda wi=wi, mp=mp: S.dma_start(out=wkb[wi][:], in_=io[wsrc][mp]),
                 reads=[b_wbf], writes=[b_wkb[wi]], track=tr_wkb[wi])
            for hf in range(ntok // nn):
                pi = cnt["pp"] % 3
                cnt["pp"] += 1
                si = cnt["kst"] % 3
                cnt["kst"] += 1
                for kk in range(KC):
                    k.op("pe", lambda wi=wi, kk=kk, hf=hf, pi=pi: T.matmul(
                        pp[pi][:, :nn], lhsT=wkb[wi][:, kk, :], rhs=hT[:, kk, hf * nn:(hf + 1) * nn],
                        start=(kk == 0), stop=(kk == KC - 1)), reads=[b_wkb[wi], b_hT], writes=[b_pp[pi]])
                k.op("act", lambda pi=pi, si=si: A.copy(out=kst[si][:, :nn], in_=pp[pi][:, :nn]),
                     reads=[b_pp[pi]], writes=[b_kst[si]])
                k.op("pe", lambda si=si: T.matmul(psw[:, :nn], lhsT=perm[:], rhs=kst[si][:, :nn], start=True, stop=True),
                     reads=[b_kst[si], b_const], writes=[b_psw])
                sl = slice(hf * nn, (hf + 1) * nn)
                k.op("dve", lambda sl=sl: V.tensor_tensor(out=rt[0][:, :nn], in0=psw[0:32, :nn], in1=sin_t[:, sl], op=ALU.mult),
                     reads=[b_psw, b_tab], writes=[b_rt])
                k.op("dve", lambda sl=sl, pi=pi: V.tensor_tensor(out=rt[1][:, :nn], in0=pp[pi][0:32, :nn], in1=cos_t[:, sl], op=ALU.mult),
                     reads=[b_pp[pi], b_tab], writes=[b_rt])
                k.op("dve", lambda si=si: V.tensor_tensor(out=kst[si][0:32, :nn], in0=rt[0][:, :nn], in1=rt[1][:, :nn], op=ALU.add),
                     reads=[b_rt], writes=[b_kst[si]])
                k.op("pool", lambda si=si, mp=mp, hf=hf: P.dma_start(
                    out=io[dst][mp][:, tok0 + hf * nn: tok0 + (hf + 1) * nn], in_=kst[si][:, :nn]),
                    reads=[b_kst[si]], writes=[b_dst], track=tr_kst[si])

    def proj_v(ntok, tok0):
        for vc in range(D // 256):
            wi = cnt["wv"] % 2
            cnt["wv"] += 1
            k.op("sp", lambda wi=wi, vc=vc: S.dma_start(out=wvb[wi][:], in_=io["wv_bf"][vc]),
                 reads=[b_wbf], writes=[b_wvb[wi]], track=tr_wvb[wi])
            for tt in range(ntok // 128):
                pi = cnt["pp"] % 3
                cnt["pp"] += 1
                si = cnt["vst"] % 3
                cnt["vst"] += 1
                for kk in range(KC):
                    k.op("pe", lambda wi=wi, kk=kk, tt=tt, pi=pi: T.matmul(
                        pp[pi][:, :256], lhsT=hT[:, kk, tt * 128:(tt + 1) * 128], rhs=wvb[wi][:, kk, :],
                        start=(kk == 0), stop=(kk == KC - 1)), reads=[b_wvb[wi], b_hT], writes=[b_pp[pi]])
                cnt["ev"] += 1
                if cnt["ev"] % 2 == 0:
                    k.op("act", lambda pi=pi, si=si: A.copy(out=vst[si][:], in_=pp[pi][:, :256]),
                         reads=[b_pp[pi]], writes=[b_vst[si]])
                else:
                    k.op("dve", lambda pi=pi, si=si: V.tensor_copy(out=vst[si][:], in_=pp[pi][:, :256]),
                         reads=[b_pp[pi]], writes=[b_vst[si]])
                k.op("pool", lambda si=si, vc=vc, tt=tt: P.dma_start(
                    out=io["Vs"][tok0 + tt * 128: tok0 + (tt + 1) * 128, vc * 256:(vc + 1) * 256], in_=vst[si][:]),
                    reads=[b_vst[si]], writes=[b_Vs], track=tr_vst[si])

    OCH = min(cfg.CH, TL)
    for oc in range(TL // OCH):
        t0 = oc * OCH
        rms_to_hT(io["x_own"][t0:t0 + OCH, :], OCH, A1, B1, hT, b_hT, [b_A, b_modT])
        rot_tables(io["pos_own"][:, t0:t0 + OCH], OCH)
        proj_rot("wq_bf", "QT", b_QT, OCH, t0)
    for ch in range(cfg.NCH):
        t0 = ch * cfg.CH
        rms_to_hT(io["x_all"][t0:t0 + cfg.CH, :], cfg.CH, A1, B1, hT, b_hT, [b_A, b_modT])
        rot_tables(io["pos_all"][:, t0:t0 + cfg.CH], cfg.CH)
        proj_rot("wk_bf", "KT", b_KT, cfg.CH, t0)
        proj_v(cfg.CH, t0)
    end_phase(es)
    if upto == "P1":
        return finish()

    es, sb, ps = new_phase()
    mdiff = sb("mdiff_sb", [128, 8, 128], BF16)
    mdil = sb("mdil_sb", [128, 24, 128], BF16)
    lamt = sb("lamt", [128, 4, 128], F32)
    lamw = sb("lamw", [128, 8], F32)
    gsub = sb("gsub", [128, 256], F32)
    gdil = sb("gdil", [128, cfg.DILW], F32)
    b_c2 = Buf()
    trc = k.dtrack()
    k.op("sp", lambda: S.dma_start(out=mdiff[:], in_=io["mdiff"]), writes=[b_c2], track=trc)
    k.op("sp", lambda: S.dma_start(out=mdil[:], in_=io["mdil"]), writes=[b_c2], track=trc)
    for i in range(4):
        k.op("sp", lambda i=i: S.dma_start(out=lamt[:, i, :], in_=io["lam4"][i:i + 1, :].to_broadcast([128, 128])),
             writes=[b_c2], track=trc)
    k.op("sp", lambda: S.dma_start(out=gsub[:], in_=io["subln_row"].to_broadcast([128, 256])), writes=[b_c2], track=trc)
    k.op("sp", lambda: S.dma_start(out=gdil[:], in_=io["dilg_row"].to_broadcast([128, cfg.DILW])), writes=[b_c2], track=trc)
    junk2 = sb("junk2", [128, 256], F32)
    b_j2 = Buf()
    for i in range(2):
        k.op("dve", lambda i=i: V.scalar_tensor_tensor(out=junk2[:, :128], in0=lamt[:, 2 * i, :], scalar=1.0,
                                                      in1=lamt[:, 2 * i + 1, :], op0=ALU.mult, op1=ALU.mult,
                                                      accum_out=lamw[:, i:i + 1]), reads=[b_c2], writes=[b_c2, b_j2])
    k.op("act", lambda: A.activation(out=lamw[:, 2:4], in_=lamw[:, 0:2], func=AF.Exp), reads=[b_c2], writes=[b_c2])
    k.op("dve", lambda: V.tensor_tensor(out=lamw[:, 4:5], in0=lamw[:, 3:4], in1=lamw[:, 2:3], op=ALU.subtract),
         reads=[b_c2], writes=[b_c2])
    k.op("dve", lambda: V.tensor_scalar(out=lamw[:, 5:6], in0=lamw[:, 4:5], scalar1=-LAM_INIT, scalar2=None, op0=ALU.add),
         reads=[b_c2], writes=[b_c2])
    k.op("dve", lambda: V.tensor_scalar(out=gsub[:], in0=gsub[:], scalar1=1.0 - LAM_INIT, scalar2=None, op0=ALU.mult),
         reads=[b_c2], writes=[b_c2])
    nlam = lamw[:, 5:6]

    ktb = [sb("ktb%d" % i, [128, SEQ], BF16) for i in range(4)]
    b_ktb = [Buf() for _ in range(4)]
    tr_ktb = [k.dtrack() for _ in range(4)]
    qtb = [sb("qtb%d" % i, [128, TL], BF16) for i in range(4)]
    b_qtb = [Buf() for _ in range(4)]
    tr_qtb = [k.dtrack() for _ in range(4)]
    vab = [sb("vab%d" % i, [128, NB, 257], BF16) for i in range(2)]
    b_vab = [Buf() for _ in range(2)]
    tr_vab = [k.dtrack() for _ in range(2)]
    for i in range(2):
        k.op("pool", lambda i=i: P.memset(vab[i][:], 1.0), writes=[b_vab[i]])
    sps = [ps("sps%d" % i, [128, 512], F32) for i in range(3)]
    b_sps = [Buf() for _ in range(3)]
    Ops = [ps("Ops%d" % i, [128, 512], F32) for i in range(4)]
    b_O = [Buf() for _ in range(4)]
    tps = ps("tps", [128, 256], BF16)
    b_tps = Buf()
    ptb = [sb("ptb%d" % i, [128, 512], BF16) for i in range(3)]
    b_ptb = [Buf() for _ in range(3)]
    fw = sb("fw", [128, 16], F32)
    b_fw = Buf()
    ft = [sb("ft%d" % i, [128, 256], F32) for i in range(2)]
    b_ft = Buf()
    yb = sb("yb", [128, 256], BF16)
    b_yb = Buf()
    yts = [sb("yts%d" % i, [128, 2, 128], BF16) for i in range(2)]
    b_yts = [Buf() for _ in range(2)]
    tr_yts = [k.dtrack() for _ in range(2)]
    b_yT = Buf()
    c2 = {"s": 0, "kq": 0, "v": 0, "y": 0, "o": 0}
    pcf = [sb("pcf%d" % i, [128, D], F32) for i in range(2)]
    pcb = [sb("pcb0", [128, D], BF16)] * 2
    b_pcf = [Buf() for _ in range(2)]
    b_pcb = [Buf()] * 2
    tr_pcf = [k.dtrack() for _ in range(2)]
    tr_pcb = [k.dtrack()] * 2
    b_tabs = Buf()
    NBLK = cfg.NEXP // 128
    pc_jobs = [(src, dst, b) for (src, dst) in (("peer_u", "u_bf"), ("peer_v", "v_bf")) for b in range(NBLK)]
    pc_state = {"next": 0}
    n_iter = (cfg.NDH + cfg.NSH) * NBL
    pc_per_iter = -(-len(pc_jobs) // n_iter)

    def pc_finish(jn):
        src, dst, b = pc_jobs[jn]
        i = jn % 2
        k.op("pool", lambda i=i: P.tensor_copy(out=pcb[i][:], in_=pcf[i][:]), reads=[b_pcf[i]], writes=[b_pcb[i]])
        k.op("pool", lambda i=i, dst=dst, b=b: P.dma_start(out=io[dst][b * 128:(b + 1) * 128, :], in_=pcb[i][:]),
             reads=[b_pcb[i]], writes=[b_tabs], track=tr_pcb[i])

    def pc_steps(n):
        for _ in range(n):
            jn = pc_state["next"]
            if jn > len(pc_jobs):
                return
            if jn < len(pc_jobs):
                src, dst, b = pc_jobs[jn]
                i = jn % 2
                k.op("pool", lambda i=i, src=src, b=b: P.dma_start(out=pcf[i][:], in_=io[src][b * 128:(b + 1) * 128, :]),
                     writes=[b_pcf[i]], track=tr_pcf[i])
            if jn >= 1:
                pc_finish(jn - 1)
            pc_state["next"] = jn + 1

    def attend(kt, b_kt, qt, b_qt, va, b_va, dv, m, kbs, mask_of, O, b_Oa):
        groups = [kbs[i:i + 4] for i in range(0, len(kbs), 4)]
        first = True
        for gi, grp in enumerate(groups):
            si = c2["s"] % 3
            c2["s"] += 1
            n = len(grp) * 128
            for j, kb in enumerate(grp):
                k.op("pe", lambda j=j, kb=kb, si=si: T.matmul(
                    sps[si][:, j * 128:(j + 1) * 128], lhsT=kt[:, kb * 128:(kb + 1) * 128],
                    rhs=qt[:, m * 128:(m + 1) * 128], start=True, stop=True),
                    reads=[b_kt, b_qt], writes=[b_sps[si]])
            k.op("act", lambda si=si, n=n: A.activation(out=ptb[si][:, :n], in_=sps[si][:, :n], func=AF.Exp, scale=SCALE),
                 reads=[b_sps[si]], writes=[b_ptb[si]])
            mk = mask_of(grp[0], len(grp))
            if mk is not None:
                k.op("dve", lambda si=si, n=n, mk=mk: V.tensor_tensor(out=ptb[si][:, :n], in0=ptb[si][:, :n], in1=mk, op=ALU.mult),
                     reads=[b_ptb[si], b_c2], writes=[b_ptb[si]])
            for j, kb in enumerate(grp):
                last = (gi == len(groups) - 1) and (j == len(grp) - 1)
                k.op("pe", lambda j=j, kb=kb, si=si, first=first, last=last: T.matmul(
                    O[:, :dv + 1], lhsT=ptb[si][:, j * 128:(j + 1) * 128], rhs=va[:, kb, 0:dv + 1],
                    start=first, stop=last), reads=[b_ptb[si], b_va], writes=[b_Oa])
                first = False

    def rstd_from_ss(ss_ap, n, out_ap):
        k.op("act", lambda: A.activation(out=fw[:, 8:9], in_=ss_ap, func=AF.Ln, scale=1.0 / n, bias=EPS),
             reads=[b_fw], writes=[b_fw])
        k.op("act", lambda: A.activation(out=out_ap, in_=fw[:, 8:9], func=AF.Exp, scale=-0.5),
             reads=[b_fw], writes=[b_fw])

    def emit_yT(width, chunk0, m):
        yi = c2["y"] % 2
        c2["y"] += 1
        nj = width // 128
        for j in range(nj):
            k.op("pe", lambda j=j: T.transpose(out=tps[:, j * 128:(j + 1) * 128], in_=yb[:, j * 128:(j + 1) * 128], identity=ident[:]),
                 reads=[b_yb, b_const], writes=[b_tps])
        k.op("act", lambda yi=yi, nj=nj: A.copy(out=yts[yi][:, :nj, :].rearrange("p j q -> p (j q)"), in_=tps[:, :nj * 128]),
             reads=[b_tps], writes=[b_yts[yi]])
        for j in range(nj):
            k.op("pool", lambda yi=yi, j=j: P.dma_start(out=io["yT"][chunk0 + j][:, m * 128:(m + 1) * 128], in_=yts[yi][:, j, :]),
                 reads=[b_yts[yi]], writes=[b_yT], track=tr_yts[yi])

    for h in range(cfg.NDH):
        vi = c2["v"] % 2
        c2["v"] += 1
        for b0 in range(0, NB, 8):
            k.op("sp", lambda vi=vi, h=h, b0=b0: S.dma_start(
                out=vab[vi][:, b0:b0 + 8, 0:256],
                in_=io["Vs"][b0 * 128:(b0 + 8) * 128, 256 * h:256 * (h + 1)].rearrange("(b p) c -> p b c", p=128)),
                reads=[b_Vs], writes=[b_vab[vi]], track=tr_vab[vi])
        kq = []
        for s in range(2):
            bi = c2["kq"] % 4
            c2["kq"] += 1
            k.op("sp", lambda bi=bi, h=h, s=s: S.dma_start(out=ktb[bi][:], in_=io["KT"][2 * h + s]),
                 reads=[b_KT], writes=[b_ktb[bi]], track=tr_ktb[bi])
            k.op("sp", lambda bi=bi, h=h, s=s: S.dma_start(out=qtb[bi][:], in_=io["QT"][2 * h + s]),
                 reads=[b_QT], writes=[b_qtb[bi]], track=tr_qtb[bi])
            kq.append(bi)
        for m in range(NBL):
            kbs = list(range(0, 8 * m + 8))

            def mask_of(kb0, n, m=m):
                r0 = kb0 - 8 * m
                if r0 < 0:
                    return None
                return mdiff[:, r0:r0 + n, :].rearrange("p r q -> p (r q)")
            ob = (m % 2) * 2
            for s in range(2):
                bi = kq[s]
                attend(ktb[bi], b_ktb[bi], qtb[bi], b_qtb[bi], vab[vi], b_vab[vi], 256, m, kbs, mask_of, Ops[ob + s], b_O[ob + s])
            O1, O2, bO1, bO2 = Ops[ob], Ops[ob + 1], b_O[ob], b_O[ob + 1]
            fin = lambda fn, r=(), w=(): k.op("dve", fn, reads=[b_fw, b_ft, b_c2] + list(r), writes=[b_fw, b_ft] + list(w))
            fin(lambda O1=O1: V.reciprocal(out=fw[:, 0:1], in_=O1[:, 256:257]), r=[bO1])
            fin(lambda O2=O2: V.reciprocal(out=fw[:, 1:2], in_=O2[:, 256:257]), r=[bO2])
            fin(lambda: V.tensor_tensor(out=fw[:, 2:3], in0=fw[:, 1:2], in1=nlam, op=ALU.mult))
            fin(lambda O2=O2: V.tensor_scalar(out=ft[0][:], in0=O2[:, 0:256], scalar1=fw[:, 2:3], scalar2=None, op0=ALU.mult), r=[bO2])
            fin(lambda O1=O1: V.scalar_tensor_tensor(out=ft[1][:], in0=O1[:, 0:256], scalar=fw[:, 0:1], in1=ft[0][:],
                                                     op0=ALU.mult, op1=ALU.add), r=[bO1])
            fin(lambda: V.scalar_tensor_tensor(out=junk2[:], in0=ft[1][:], scalar=1.0, in1=ft[1][:], op0=ALU.mult, op1=ALU.mult,
                                               accum_out=fw[:, 3:4]), w=[b_j2])
            rstd_from_ss(fw[:, 3:4], 256, fw[:, 4:5])
            fin(lambda: V.scalar_tensor_tensor(out=yb[:], in0=ft[1][:], scalar=fw[:, 4:5], in1=gsub[:], op0=ALU.mult, op1=ALU.mult),
                w=[b_yb])
            emit_yT(256, 2 * h, m)
            pc_steps(pc_per_iter)

    for hd in range(cfg.NSH):
        vi = c2["v"] % 2
        c2["v"] += 1
        for b0 in range(0, NB, 8):
            k.op("sp", lambda vi=vi, hd=hd, b0=b0: S.dma_start(
                out=vab[vi][:, b0:b0 + 8, 0:128],
                in_=io["Vs"][b0 * 128:(b0 + 8) * 128, cfg.DIFFW + 128 * hd: cfg.DIFFW + 128 * (hd + 1)].rearrange("(b p) c -> p b c", p=128)),
                reads=[b_Vs], writes=[b_vab[vi]], track=tr_vab[vi])
        k.op("pool", lambda vi=vi: P.memset(vab[vi][:, :, 128:129], 1.0), writes=[b_vab[vi]])
        bi = c2["kq"] % 4
        c2["kq"] += 1
        mpi = cfg.DIFFW // 128 + hd
        k.op("sp", lambda bi=bi, mpi=mpi: S.dma_start(out=ktb[bi][:], in_=io["KT"][mpi]),
             reads=[b_KT], writes=[b_ktb[bi]], track=tr_ktb[bi])
        k.op("sp", lambda bi=bi, mpi=mpi: S.dma_start(out=qtb[bi][:], in_=io["QT"][mpi]),
             reads=[b_QT], writes=[b_qtb[bi]], track=tr_qtb[bi])
        for m in range(NBL):
            kb_lo = max(0, 8 * m - 16)
            kbs = list(range(kb_lo, 8 * m + 8))

            def mask_of(kb0, n, m=m):
                r0 = kb0 - (8 * m - 16)
                return mdil[:, r0:r0 + n, :].rearrange("p r q -> p (r q)")
            oi = c2["o"] % 4
            c2["o"] += 1
            O1, bO1 = Ops[oi], b_O[oi]
            attend(ktb[bi], b_ktb[bi], qtb[bi], b_qtb[bi], vab[vi], b_vab[vi], 128, m, kbs, mask_of, O1, bO1)
            fin = lambda fn, r=(), w=(): k.op("dve", fn, reads=[b_fw, b_ft, b_c2] + list(r), writes=[b_fw, b_ft] + list(w))
            fin(lambda O1=O1: V.reciprocal(out=fw[:, 0:1], in_=O1[:, 128:129]), r=[bO1])
            fin(lambda O1=O1: V.tensor_scalar(out=ft[1][:, :128], in0=O1[:, 0:128], scalar1=fw[:, 0:1], scalar2=None, op0=ALU.mult),
                r=[bO1])
            fin(lambda: V.scalar_tensor_tensor(out=junk2[:, :128], in0=ft[1][:, :128], scalar=1.0, in1=ft[1][:, :128],
                                               op0=ALU.mult, op1=ALU.mult, accum_out=fw[:, 3:4]), w=[b_j2])
            rstd_from_ss(fw[:, 3:4], 128, fw[:, 4:5])
            fin(lambda hd=hd: V.scalar_tensor_tensor(out=yb[:, :128], in0=ft[1][:, :128], scalar=fw[:, 4:5],
                                                     in1=gdil[:, 128 * hd:128 * (hd + 1)], op0=ALU.mult, op1=ALU.mult), w=[b_yb])
            emit_yT(128, cfg.DIFFW // 128 + hd, m)
            pc_steps(pc_per_iter)
    pc_steps(len(pc_jobs) + 2)
    end_phase(es)
    if upto == "P2":
        return finish()

    es, sb, ps = new_phase()
    yTs = sb("yTs", [128, KC, TL], BF16)
    b_yTs = Buf()
    trc = k.dtrack()
    yTv = io["yT"].rearrange("c p t -> p c t")
    for c0 in range(0, KC, 8):
        k.op("sp", lambda c0=c0: S.dma_start(out=yTs[:, c0:c0 + 8, :], in_=yTv[:, c0:c0 + 8, :]), reads=[b_yT], writes=[b_yTs], track=trc)
    g1row = sb("g1row", [128, D], F32)
    b_g1 = Buf()
    k.op("sp", lambda: S.dma_start(out=g1row[:], in_=io["modf"][2 * D:3 * D].rearrange("(o d) -> o d", o=1).to_broadcast([128, D])),
         reads=[b_modf], writes=[b_g1], track=trc)
    wof = [sb("wof%d" % i, [128, 4, 512], F32) for i in range(2)]
    b_wof = [Buf() for _ in range(2)]
    tr_wof = [k.dtrack() for _ in range(2)]
    wob = [sb("wob%d" % i, [128, KC, 512], BF16) for i in range(2)]
    b_wob = [Buf() for _ in range(2)]
    xs = [sb("xs%d" % i, [128, 512], F32) for i in range(2)]
    b_xs = [Buf() for _ in range(2)]
    tr_xs = [k.dtrack() for _ in range(2)]
    x1s = [sb("x1s%d" % i, [128, 512], F32) for i in range(2)]
    b_x1s = [Buf() for _ in range(2)]
    tr_x1s = [k.dtrack() for _ in range(2)]
    po = [ps("po%d" % i, [128, 512], F32) for i in range(2)]
    b_po = [Buf() for _ in range(2)]
    b_x1 = Buf()
    wov = io["w_out"].rearrange("(kk p) c -> p kk c", p=128)
    c3 = {"f": 0, "x": 0}
    for dc in range(D // 512):
        wi = dc % 2
        for k0 in range(0, KC, 4):
            fi = c3["f"] % 2
            c3["f"] += 1
            k.op("sp", lambda fi=fi, k0=k0, dc=dc: S.dma_start(out=wof[fi][:], in_=wov[:, k0:k0 + 4, dc * 512:(dc + 1) * 512]),
                 writes=[b_wof[fi]], track=tr_wof[fi])
            if c3["f"] % 2 == 0:
                k.op("pool", lambda fi=fi, wi=wi, k0=k0: P.tensor_copy(out=wob[wi][:, k0:k0 + 4, :], in_=wof[fi][:]),
                     reads=[b_wof[fi]], writes=[b_wob[wi]])
            else:
                k.op("act", lambda fi=fi, wi=wi, k0=k0: A.copy(out=wob[wi][:, k0:k0 + 4, :], in_=wof[fi][:]),
                     reads=[b_wof[fi]], writes=[b_wob[wi]])
        for t in range(NBL):
            xi = c3["x"] % 2
            c3["x"] += 1
            k.op("sp", lambda xi=xi, t=t, dc=dc: S.dma_start(out=xs[xi][:], in_=io["x_own"][t * 128:(t + 1) * 128, dc * 512:(dc + 1) * 512]),
                 writes=[b_xs[xi]], track=tr_xs[xi])
            for kk in range(KC):
                k.op("pe", lambda xi=xi, kk=kk, t=t, wi=wi: T.matmul(
                    po[xi][:], lhsT=yTs[:, kk, t * 128:(t + 1) * 128], rhs=wob[wi][:, kk, :],
                    start=(kk == 0), stop=(kk == KC - 1)), reads=[b_yTs, b_wob[wi]], writes=[b_po[xi]])
            k.op("dve", lambda xi=xi, dc=dc: V.tensor_tensor(out=x1s[xi][:], in0=po[xi][:], in1=g1row[:, dc * 512:(dc + 1) * 512], op=ALU.mult),
                 reads=[b_po[xi], b_g1], writes=[b_x1s[xi]])
            k.op("dve", lambda xi=xi: V.tensor_tensor(out=x1s[xi][:], in0=x1s[xi][:], in1=xs[xi][:], op=ALU.add),
                 reads=[b_xs[xi], b_x1s[xi]], writes=[b_x1s[xi]])
            k.op("pool", lambda xi=xi, t=t, dc=dc: P.dma_start(out=io["x1"][t * 128:(t + 1) * 128, dc * 512:(dc + 1) * 512], in_=x1s[xi][:]),
                 reads=[b_x1s[xi]], writes=[b_x1], track=tr_x1s[xi])
    end_phase(es)
    if upto == "P3":
        return finish()

    es4, sb4, ps4 = new_phase()
    NKH = 2 * PH
    NSL = PH * TOPK
    rstd2 = sb4("rstd2", [128, NBL], F32)
    topv = sb4("topv", [128, NBL, NKH, 16], F32)
    topi = sb4("topi", [128, NBL, NKH, 16], U32)
    eidf = sb4("eidf", [128, NBL, NSL], F32)
    eidi = sb4("eidi", [128, NBL, NSL], I32)
    gate = sb4("gate", [128, NBL, NSL], F32)
    es_ab, sb_ab, ps_ab = new_phase()
    h2T = sb_ab("h2T", [128, KC, TL], BF16)
    es, sb, ps = new_phase()
    b_h2T = Buf()
    xt = [sb("xt4_%d" % i, [128, D], F32) for i in range(2)]
    b_xt = [Buf() for _ in range(2)]
    tr_x = [k.dtrack() for _ in range(2)]
    xn = [sb("xn4_%d" % i, [128, D], BF16) for i in range(2)]
    b_xn = [Buf() for _ in range(2)]
    junk = sb("junk4", [128, D], BF16)
    b_junk = Buf()
    stat = sb("stat4", [128, 4], F32)
    b_stat = Buf()
    b_rstd2 = Buf()
    HK = KC // 2
    pT = [ps("pT4_%d" % i, [128, HK * 128], BF16) for i in range(2)]
    b_pT = [Buf() for _ in range(2)]
    cnt = {"x": 0}
    for t in range(NBL):
        i = cnt["x"] % 2
        cnt["x"] += 1
        k.op("sp", lambda i=i, t=t: S.dma_start(out=xt[i][:], in_=io["x1"][t * 128:(t + 1) * 128, :]),
             reads=[b_x1], writes=[b_xt[i]], track=tr_x[i])
        k.op("dve", lambda i=i: V.scalar_tensor_tensor(out=junk[:], in0=xt[i][:], scalar=1.0, in1=xt[i][:], op0=ALU.mult,
                                                      op1=ALU.mult, accum_out=stat[:, 0:1]), reads=[b_xt[i]], writes=[b_junk, b_stat])
        k.op("act", lambda: A.activation(out=stat[:, 1:2], in_=stat[:, 0:1], func=AF.Ln, scale=1.0 / D, bias=EPS),
             reads=[b_stat], writes=[b_stat])
        k.op("act", lambda t=t: A.activation(out=rstd2[:, t:t + 1], in_=stat[:, 1:2], func=AF.Exp, scale=-0.5),
             reads=[b_stat], writes=[b_rstd2])
        k.op("pool", lambda i=i, t=t: P.tensor_scalar(out=xn[i][:], in0=xt[i][:], scalar1=rstd2[:, t:t + 1], scalar2=None, op0=ALU.mult),
             reads=[b_xt[i], b_rstd2], writes=[b_xn[i]])
        for hh in range(2):
            for q in range(HK):
                kk = hh * HK + q
                k.op("pe", lambda i=i, kk=kk, q=q, hh=hh: T.transpose(
                    out=pT[hh][:, q * 128:(q + 1) * 128], in_=xn[i][:, kk * 128:(kk + 1) * 128], identity=ident[:]),
                    reads=[b_xn[i], b_const], writes=[b_pT[hh]])
            for q in range(HK):
                kk = hh * HK + q
                if q % 2 == 0:
                    k.op("act", lambda kk=kk, q=q, hh=hh, t=t: A.activation(
                        out=h2T[:, kk, t * 128:(t + 1) * 128], in_=pT[hh][:, q * 128:(q + 1) * 128],
                        func=AF.Identity, scale=A2[:, kk:kk + 1], bias=B2[:, kk:kk + 1]),
                        reads=[b_pT[hh], b_A, b_modT], writes=[b_h2T])
                else:
                    k.op("dve", lambda kk=kk, q=q, hh=hh, t=t: V.tensor_scalar(
                        out=h2T[:, kk, t * 128:(t + 1) * 128], in0=pT[hh][:, q * 128:(q + 1) * 128],
                        scalar1=A2[:, kk:kk + 1], scalar2=B2[:, kk:kk + 1], op0=ALU.mult, op1=ALU.add),
                        reads=[b_pT[hh], b_A, b_modT], writes=[b_h2T])
    k.barrier()
    es.close()
    es, sb, ps = new_phase()
    b_top = Buf()
    wqf = [sb("wqf%d" % i, [128, KC, 128], F32) for i in range(2)]
    b_wqf = [Buf() for _ in range(2)]
    tr_wqf = [k.dtrack() for _ in range(2)]
    wqb = [sb("wqb%d" % i, [128, KC, 128], BF16) for i in range(2)]
    b_wqb = [Buf() for _ in range(2)]
    skf = sb("skf", [128, NKH, NK], F32)
    skb = sb("skb", [128, NKH, NK], BF16)
    b_sk = Buf()
    trc = k.dtrack()
    skv = io["skT"].rearrange("j kk n -> kk j n")
    for j0 in range(0, NKH, 8):
        k.op("sp", lambda j0=j0: S.dma_start(out=skf[:, j0:j0 + 8, :], in_=skv[:, j0:j0 + 8, :]), writes=[b_sk], track=trc)
    k.op("dve", lambda: V.tensor_copy(out=skb[:], in_=skf[:]), reads=[b_sk], writes=[b_sk])
    qTs = sb("qTs", [128, TL], BF16)
    b_qTs = Buf()
    pq = [ps("pq%d" % i, [128, 512], F32) for i in range(2)]
    b_pq = [Buf() for _ in range(2)]
    psc = [ps("psc%d" % i, [128, NK], F32) for i in range(2)]
    b_psc = [Buf() for _ in range(2)]
    scs = [sb("scs%d" % i, [128, NK], F32) for i in range(2)]
    b_scs = [Buf() for _ in range(2)]
    wqv = io["peer_wq"].rearrange("(kk p) c -> p kk c", p=128)
    nn = min(512, TL)
    c4 = {"q": 0, "s": 0}
    for j in range(NKH):
        wi = j % 2
        for c0 in range(0, KC, 8):
            k.op("sp", lambda wi=wi, j=j, c0=c0: S.dma_start(out=wqf[wi][:, c0:c0 + 8, :], in_=wqv[:, c0:c0 + 8, j * 128:(j + 1) * 128]),
                 writes=[b_wqf[wi]], track=tr_wqf[wi])
        if j % 2 == 0:
            k.op("act", lambda wi=wi: A.copy(out=wqb[wi][:], in_=wqf[wi][:]), reads=[b_wqf[wi]], writes=[b_wqb[wi]])
        else:
            k.op("pool", lambda wi=wi: P.tensor_copy(out=wqb[wi][:], in_=wqf[wi][:]), reads=[b_wqf[wi]], writes=[b_wqb[wi]])
        for hf in range(TL // nn):
            qi = c4["q"] % 2
            c4["q"] += 1
            for kk in range(KC):
                k.op("pe", lambda wi=wi, kk=kk, hf=hf, qi=qi: T.matmul(
                    pq[qi][:, :nn], lhsT=wqb[wi][:, kk, :], rhs=h2T[:, kk, hf * nn:(hf + 1) * nn],
                    start=(kk == 0), stop=(kk == KC - 1)), reads=[b_wqb[wi], b_h2T], writes=[b_pq[qi]])
            k.op("act", lambda qi=qi, hf=hf: A.copy(out=qTs[:, hf * nn:(hf + 1) * nn], in_=pq[qi][:, :nn]),
                 reads=[b_pq[qi]], writes=[b_qTs])
        for t in range(NBL):
            si = c4["s"] % 2
            c4["s"] += 1
            k.op("pe", lambda si=si, t=t, j=j: T.matmul(psc[si][:], lhsT=qTs[:, t * 128:(t + 1) * 128], rhs=skb[:, j, :],
                                                        start=True, stop=True), reads=[b_qTs, b_sk], writes=[b_psc[si]])
            k.op("act", lambda si=si: A.copy(out=scs[si][:], in_=psc[si][:]), reads=[b_psc[si]], writes=[b_scs[si]])
            dv = lambda fn, si=si: k.op("dve", fn, reads=[b_scs[si], b_top], writes=[b_scs[si], b_top])
            dv(lambda si=si, t=t, j=j: V.max(out=topv[:, t, j, 0:8], in_=scs[si][:]))
            dv(lambda si=si, t=t, j=j: V.max_index(out=topi[:, t, j, 0:8], in_max=topv[:, t, j, 0:8], in_values=scs[si][:]))
            dv(lambda si=si, t=t, j=j: V.match_replace(out=scs[si][:], in_to_replace=topv[:, t, j, 0:8], in_values=scs[si][:], imm_value=-1e30))
            dv(lambda si=si, t=t, j=j: V.max(out=topv[:, t, j, 8:16], in_=scs[si][:]))
            dv(lambda si=si, t=t, j=j: V.max_index(out=topi[:, t, j, 8:16], in_max=topv[:, t, j, 8:16], in_values=scs[si][:]))
    k.barrier()
    es.close()
    es_ab.close()
    es, sb, ps = new_phase()
    b_sel = Buf()
    topif = sb("topif", [128, NBL, NKH, 16], F32)
    iota16 = sb("iota16_sb", [128, 16], F32)
    k.op("sp", lambda: S.dma_start(out=iota16[:], in_=io["iota16"]), writes=[b_sk], track=trc)
    k.op("dve", lambda: V.tensor_copy(out=topif[:], in_=topi[:]), reads=[b_top], writes=[b_top])
    cand = sb("cand", [128, 256], F32)
    cand2 = sb("cand2", [128, 256], F32)
    fv = sb("fv", [128, 16], F32)
    fpos = sb("fpos", [128, 16], U32)
    fa = sb("fa", [128, 16], U32)
    fbb = sb("fbb", [128, 16], U32)
    faf = sb("faf", [128, 16], F32)
    fbf = sb("fbf", [128, 16], F32)
    oh = sb("oh", [128, 16, 16], F32)
    sel0 = sb("sel0", [128, 16], F32)
    sel1 = sb("sel1", [128, 16], F32)
    gw = sb("gw", [128, 4], F32)
    b_w = Buf()
    for t in range(NBL):
        for h in range(PH):
            dv = lambda fn: k.op("dve", fn, reads=[b_w, b_top, b_sk], writes=[b_w])
            v0 = topv[:, t, 2 * h, :]
            v1 = topv[:, t, 2 * h + 1, :]
            i0 = topif[:, t, 2 * h, :]
            i1 = topif[:, t, 2 * h + 1, :]
            cand3 = cand[:].rearrange("p (a b) -> p a b", b=16)
            dv(lambda v0=v0, v1=v1, cand3=cand3: V.tensor_tensor(out=cand3, in0=v0.unsqueeze(2).to_broadcast([128, 16, 16]),
                                                                 in1=v1.unsqueeze(1).to_broadcast([128, 16, 16]), op=ALU.add))
            dv(lambda: V.max(out=fv[:, 0:8], in_=cand[:]))
            dv(lambda: V.max_index(out=fpos[:, 0:8], in_max=fv[:, 0:8], in_values=cand[:]))
            dv(lambda: V.match_replace(out=cand2[:], in_to_replace=fv[:, 0:8], in_values=cand[:], imm_value=-1e30))
            dv(lambda: V.max(out=fv[:, 8:16], in_=cand2[:]))
            dv(lambda: V.max_index(out=fpos[:, 8:16], in_max=fv[:, 8:16], in_values=cand2[:]))
            dv(lambda: V.tensor_single_scalar(out=fa[:], in_=fpos[:], scalar=4, op=ALU.logical_shift_right))
            dv(lambda: V.tensor_single_scalar(out=fbb[:], in_=fpos[:], scalar=15, op=ALU.bitwise_and))
            dv(lambda: V.tensor_copy(out=faf[:], in_=fa[:]))
            dv(lambda: V.tensor_copy(out=fbf[:], in_=fbb[:]))
            for (pf, ix, sel) in ((faf, i0, sel0), (fbf, i1, sel1)):
                dv(lambda pf=pf: V.tensor_tensor(out=oh[:], in0=pf[:].unsqueeze(2).to_broadcast([128, 16, 16]),
                                                 in1=iota16[:].unsqueeze(1).to_broadcast([128, 16, 16]), op=ALU.is_equal))
                dv(lambda ix=ix: V.tensor_tensor(out=oh[:], in0=oh[:], in1=ix.unsqueeze(1).to_broadcast([128, 16, 16]), op=ALU.mult))
                dv(lambda sel=sel: V.tensor_reduce(out=sel[:], in_=oh[:], axis=AX.X, op=ALU.add))
            k.op("dve", lambda t=t, h=h: V.scalar_tensor_tensor(out=eidf[:, t, h * 16:(h + 1) * 16], in0=sel0[:], scalar=float(NK),
                                                              in1=sel1[:], op0=ALU.mult, op1=ALU.add),
                 reads=[b_w], writes=[b_sel])
            dv(lambda: V.tensor_scalar(out=gw[:, 0:1], in0=fv[:, 0:1], scalar1=-1.0, scalar2=None, op0=ALU.mult))
            k.op("act", lambda: A.activation(out=cand2[:, 0:16], in_=fv[:], func=AF.Exp, bias=gw[:, 0:1], scale=1.0,
                                             accum_out=gw[:, 1:2]), reads=[b_w], writes=[b_w])
            dv(lambda: V.reciprocal(out=gw[:, 2:3], in_=gw[:, 1:2]))
            k.op("dve", lambda t=t, h=h: V.tensor_scalar(out=gate[:, t, h * 16:(h + 1) * 16], in0=cand2[:, 0:16],
                                                       scalar1=gw[:, 2:3], scalar2=None, op0=ALU.mult),
                 reads=[b_w], writes=[b_sel])
    k.op("dve", lambda: V.tensor_copy(out=eidi[:], in_=eidf[:]), reads=[b_sel], writes=[b_sel])
    if debug:
        dout("dbg_eid", [TL, NSL], I32)
        dout("dbg_gate", [TL, NSL], F32)
        k.op("sp", lambda: S.dma_start(out=io["dbg_eid"].rearrange("(t p) s -> p t s", p=128), in_=eidi[:]), reads=[b_sel], track=trc)
        k.op("sp", lambda: S.dma_start(out=io["dbg_gate"].rearrange("(t p) s -> p t s", p=128), in_=gate[:]), reads=[b_sel], track=trc)
    k.barrier()
    es.close()
    es, sb, ps = new_phase()
    xt = [sb("xt4d%d" % i, [128, D], F32) for i in range(2)]
    b_xt = [Buf() for _ in range(2)]
    junk = sb("junk4d", [128, D], BF16)
    b_junk = Buf()
    stat = sb("stat4d", [128, 4], F32)
    b_stat = Buf()
    A2row = sb("A2row", [128, D], F32)
    B2row = sb("B2row", [128, D], F32)
    g2row = sb("g2row", [128, D], F32)
    fgrow = sb("fgrow", [128, D], F32)
    b_rows = Buf()
    mrow = lambda q: io["modf"][q * D:(q + 1) * D].rearrange("(o d) -> o d", o=1).to_broadcast([128, D])
    k.op("sp", lambda: S.dma_start(out=A2row[:], in_=mrow(4)), reads=[b_modf], writes=[b_rows], track=trc)
    k.op("sp", lambda: S.dma_start(out=B2row[:], in_=mrow(3)), reads=[b_modf], writes=[b_rows], track=trc)
    k.op("sp", lambda: S.dma_start(out=g2row[:], in_=mrow(5)), reads=[b_modf], writes=[b_rows], track=trc)
    k.op("sp", lambda: S.dma_start(out=fgrow[:], in_=io["n2g_row"].to_broadcast([128, D])), writes=[b_rows], track=trc)
    k.op("dve", lambda: V.scalar_tensor_tensor(out=A2row[:], in0=A2row[:], scalar=1.0, in1=fgrow[:], op0=ALU.add, op1=ALU.mult),
         reads=[b_rows], writes=[b_rows])
    b_fg = Buf()
    k.op("sp", lambda: S.dma_start(out=fgrow[:], in_=io["fg_row"].to_broadcast([128, D])), reads=[b_rows], writes=[b_rows, b_fg], track=trc)
    h2 = [sb("h2_%d" % i, [128, D], BF16) for i in range(2)]
    b_h2 = [Buf() for _ in range(2)]
    NUG = 4
    ug = [sb("ug%d" % i, [128, D], BF16) for i in range(NUG)]
    b_ug = [Buf() for _ in range(NUG)]
    tr_ug = [k.dtrack() for _ in range(NUG)]
    pre = [sb("pre%d" % i, [128, NSL], F32) for i in range(2)]
    coef = [sb("coef%d" % i, [128, NSL], F32) for i in range(2)]
    b_pre = [Buf() for _ in range(2)]
    dg = [sb("dg%d" % i, [128, 128], BF16) for i in range(2)]
    b_dg = [Buf() for _ in range(2)]
    NPB = D // 512
    assert NPB <= 8
    pacc = [ps("pacc%d" % i, [128, 512], F32) for i in range(NPB)]
    b_pacc = Buf()
    x2 = sb("x2", [128, D], F32)
    b_x2 = Buf()
    tr_o = k.dtrack()
    uv = io["u_bf"]
    vv = io["v_bf"]
    c5 = {"g": 0, "d": 0}

    def prep_tile(t):
        i = t % 2
        k.op("sp", lambda i=i, t=t: S.dma_start(out=xt[i][:], in_=io["x1"][t * 128:(t + 1) * 128, :]),
             reads=[b_x1], writes=[b_xt[i]], track=tr_x[i])
        k.op("dve", lambda i=i, t=t: V.scalar_tensor_tensor(out=x2[:], in0=xt[i][:], scalar=rstd2[:, t:t + 1], in1=A2row[:],
                                                          op0=ALU.mult, op1=ALU.mult), reads=[b_xt[i], b_rstd2, b_rows], writes=[b_x2])
        k.op("dve", lambda i=i: V.tensor_tensor(out=h2[i][:], in0=x2[:], in1=B2row[:], op=ALU.add), reads=[b_x2, b_rows], writes=[b_h2[i]])

    def u_slot(t, s):
        i = t % 2
        gi = c5["g"] % NUG
        c5["g"] += 1
        k.op("pool", lambda gi=gi, t=t, s=s: P.indirect_dma_start(
            out=ug[gi][:], out_offset=None, in_=uv, in_offset=bass.IndirectOffsetOnAxis(ap=eidi[:, t, s:s + 1], axis=0)),
            reads=[b_sel, b_tabs], writes=[b_ug[gi]], track=tr_ug[gi])
        k.op("dve", lambda gi=gi, s=s, i=i: V.scalar_tensor_tensor(out=junk[:], in0=ug[gi][:], scalar=1.0, in1=h2[i][:], op0=ALU.mult,
                                                                 op1=ALU.mult, accum_out=pre[i][:, s:s + 1]),
             reads=[b_ug[gi], b_h2[i]], writes=[b_junk, b_pre[i]])

    def act_tile(t):
        i = t % 2
        k.op("act", lambda i=i: A.activation(out=coef[i][:], in_=pre[i][:], func=AF.Gelu), reads=[b_pre[i]], writes=[b_pre[i]])
        k.op("dve", lambda i=i, t=t: V.tensor_tensor(out=coef[i][:], in0=coef[i][:], in1=gate[:, t, :], op=ALU.mult),
             reads=[b_pre[i], b_sel], writes=[b_pre[i]])

    def v_slot(t, s):
        i = t % 2
        gi = c5["g"] % NUG
        c5["g"] += 1
        di = c5["d"] % 2
        c5["d"] += 1
        k.op("pool", lambda gi=gi, t=t, s=s: P.indirect_dma_start(
            out=ug[gi][:], out_offset=None, in_=vv, in_offset=bass.IndirectOffsetOnAxis(ap=eidi[:, t, s:s + 1], axis=0)),
            reads=[b_sel, b_tabs], writes=[b_ug[gi]], track=tr_ug[gi])
        k.op("act", lambda di=di, s=s, i=i: A.activation(out=dg[di][:], in_=ident[:], func=AF.Copy, scale=coef[i][:, s:s + 1]),
             reads=[b_pre[i], b_const], writes=[b_dg[di]])
        for q in range(NPB):
            k.op("pe", lambda q=q, gi=gi, di=di, s=s: T.matmul(
                pacc[q][:], lhsT=dg[di][:], rhs=ug[gi][:, q * 512:(q + 1) * 512],
                start=(s == 0), stop=(s == NSL - 1)), reads=[b_dg[di], b_ug[gi]], writes=[b_pacc])

    def fin_tile(t):
        i = t % 2
        for q in range(NPB):
            sl = slice(q * 512, (q + 1) * 512)
            k.op("dve", lambda q=q, sl=sl: V.tensor_tensor(out=x2[:, sl], in0=pacc[q][:], in1=g2row[:, sl], op=ALU.mult),
                 reads=[b_pacc, b_rows], writes=[b_x2])
        k.op("dve", lambda i=i: V.tensor_tensor(out=x2[:], in0=x2[:], in1=xt[i][:], op=ALU.add), reads=[b_x2, b_xt[i]], writes=[b_x2])
        k.op("dve", lambda: V.scalar_tensor_tensor(out=junk[:], in0=x2[:], scalar=1.0, in1=x2[:], op0=ALU.mult, op1=ALU.mult,
                                                   accum_out=stat[:, 0:1]), reads=[b_x2], writes=[b_junk, b_stat])
        k.op("act", lambda: A.activation(out=stat[:, 1:2], in_=stat[:, 0:1], func=AF.Ln, scale=1.0 / D, bias=EPS),
             reads=[b_stat], writes=[b_stat])
        k.op("act", lambda: A.activation(out=stat[:, 2:3], in_=stat[:, 1:2], func=AF.Exp, scale=-0.5), reads=[b_stat], writes=[b_stat])
        k.op("dve", lambda: V.scalar_tensor_tensor(out=x2[:], in0=x2[:], scalar=stat[:, 2:3], in1=fgrow[:], op0=ALU.mult, op1=ALU.mult),
             reads=[b_x2, b_stat, b_fg], writes=[b_x2])
        k.op("sp", lambda t=t: S.dma_start(out=io["out"][t * 128:(t + 1) * 128, :], in_=x2[:]), reads=[b_x2], track=tr_o)

    for t in range(NBL + 1):
        if t < NBL:
            prep_tile(t)
        for s in range(NSL):
            if t < NBL:
                u_slot(t, s)
            if t >= 1:
                v_slot(t - 1, s)
        if t < NBL:
            act_tile(t)
        if t >= 1:
            fin_tile(t - 1)
    k.barrier()
    es.close()
    end_phase(es4)
    return finish()


def _dil_mult(d):
    m = ((d >= 0) & (d <= 128)).astype(np.float32)
    m += ((d >= 0) & (d <= 512) & (d % 4 == 0)).astype(np.float32)
    m += ((d >= 0) & (d <= 2048) & (d % 16 == 0)).astype(np.float32)
    return m


def make_in_maps(inputs, cfg=None):
    cfg = cfg or Cfg()
    D, SEQ, KC, TL, NBL, NK = cfg.D, cfg.SEQ, cfg.KC, cfg.TL, cfg.NBL, cfg.NK
    f = lambda a: np.ascontiguousarray(np.asarray(a))
    x = np.asarray(inputs["x"])[0]
    pos = np.asarray(inputs["positions"])[0].astype(np.int32)
    w_in = np.asarray(inputs["w_in"])[0]
    DW, LW = cfg.DIFFW, cfg.DILW
    sh = {}
    sh["x_all"] = f(x)
    sh["pos_all"] = f(pos.reshape(1, SEQ))
    sh["c_row"] = f(np.asarray(inputs["c"]).reshape(1, D))
    sh["w_adaT"] = f(np.asarray(inputs["w_ada"])[0].T).reshape(cfg.NMOD, 128, D)
    sh["b_adaT"] = f(np.asarray(inputs["b_ada"])[0].reshape(cfg.NMOD, 128).T)
    sh["n1gT"] = f(np.asarray(inputs["norm1_g"])[0].reshape(KC, 128).T)
    sh["n2gT"] = f(np.asarray(inputs["norm2_g"])[0].reshape(KC, 128).T)
    sh["n2g_row"] = f(np.asarray(inputs["norm2_g"])[0].reshape(1, D))
    sh["fg_row"] = f(np.asarray(inputs["final_g"]).reshape(1, D))
    sh["w_q"] = f(np.concatenate([w_in[:, 0:DW], w_in[:, 3 * DW:3 * DW + LW]], axis=1))
    sh["w_k"] = f(np.concatenate([w_in[:, DW:2 * DW], w_in[:, 3 * DW + LW:3 * DW + 2 * LW]], axis=1))
    sh["w_v"] = f(np.concatenate([w_in[:, 2 * DW:3 * DW], w_in[:, 3 * DW + 2 * LW:3 * DW + 3 * LW]], axis=1))
    sh["w_out"] = f(np.asarray(inputs["w_out"])[0])
    sh["lam4"] = f(np.stack([np.asarray(inputs[n])[0] for n in ("lam_q1", "lam_k1", "lam_q2", "lam_k2")]))
    sh["subln_row"] = f(np.asarray(inputs["diff_subln_g"])[0].reshape(1, 256))
    sh["dilg_row"] = f(np.asarray(inputs["dil_out_g"])[0].reshape(1, LW))
    sh["peer_wq"] = f(np.asarray(inputs["peer_wq"])[0])
    sk = np.asarray(inputs["peer_subkeys"])[0]
    sh["skT"] = f(sk.reshape(2 * PH, NK, 128).transpose(0, 2, 1))
    sh["peer_u"] = f(np.asarray(inputs["peer_u"])[0])
    sh["peer_v"] = f(np.asarray(inputs["peer_v"])[0])
    bf = ml_dtypes.bfloat16
    sh["ident"] = np.eye(128, dtype=np.float32).astype(bf)
    pm = np.zeros((128, 128), np.float32)
    for m_ in range(16):
        pm[m_ + 16, m_] = -1.0
        pm[m_, m_ + 16] = 1.0
    sh["perm"] = pm.astype(bf)
    inv = np.power(np.float32(500000.0), -np.arange(0, 32, 2, dtype=np.float32) / np.float32(32)).astype(np.float32)
    sh["invf"] = f(np.tile(inv, 2).reshape(32, 1))
    sh["iota16"] = f(np.tile(np.arange(16, dtype=np.float32), (128, 1)))
    ki = np.arange(128)[:, None]
    qi = np.arange(128)[None, :]
    maps = []
    for c in range(NCORES):
        m = dict(sh)
        m["x_own"] = f(x.reshape(cfg.NB, 128, D)[c::NCORES].reshape(TL, D))
        m["pos_own"] = f(pos.reshape(cfg.NB, 128)[c::NCORES].reshape(1, TL))
        md = np.zeros((128, 8, 128), np.float32)
        for r in range(8):
            d = (c - r) * 128 + qi - ki
            md[:, r, :] = (d >= 0)
        m["mdiff"] = md.astype(bf)
        ml = np.zeros((128, 24, 128), np.float32)
        for r in range(24):
            d = (16 + c - r) * 128 + qi - ki
            ml[:, r, :] = _dil_mult(d)
        m["mdil"] = ml.astype(bf)
        maps.append(m)
    return maps


def assemble(results, cfg=None):
    cfg = cfg or Cfg()
    out = np.zeros((cfg.NB, 128, cfg.D), np.float32)
    for c in range(NCORES):
        out[c::NCORES] = np.asarray(results[c]["out"]).reshape(cfg.NBL, 128, cfg.D)
    return out.reshape(1, cfg.SEQ, cfg.D)


def kernel(**inputs):
    cfg = Cfg()
    nc = build(cfg)
    in_maps = make_in_maps(inputs, cfg)
    res = run_bass_kernel_spmd(nc, in_maps, core_ids=list(range(NCORES)))
    return assemble(res.results, cfg)
```

```python
from contextlib import ExitStack
import math
import numpy as np
import ml_dtypes
import concourse.bass as bass
import concourse.mybir as mybir
from concourse.bass_utils import run_bass_kernel_spmd

F32 = mybir.dt.float32
BF16 = mybir.dt.bfloat16
I32 = mybir.dt.int32
U32 = mybir.dt.uint32
AF = mybir.ActivationFunctionType
ALU = mybir.AluOpType
AX = mybir.AxisListType

NCORES = 8
EPS = 1e-6
LAM_INIT = 0.2
TOPK = 16
PH = 8
SCALE = 128 ** -0.5
TWO_PI = 2.0 * math.pi
C1 = 6.28125
C2 = TWO_PI - C1


class Cfg:
    def __init__(self, D=4096, SEQ=8192, NK=128):
        self.D, self.SEQ, self.NK = D, SEQ, NK
        self.KC = D // 128
        self.DIFFW = D // 2
        self.DILW = D - self.DIFFW
        self.NDH = self.DIFFW // 256
        self.NSH = self.DILW // 128
        self.NB = SEQ // 128
        self.NBL = self.NB // NCORES
        self.TL = self.NBL * 128
        self.NEXP = NK * NK
        self.NMOD = 6 * self.KC
        self.CH = min(512, SEQ)
        self.NCH = SEQ // self.CH


class Track:
    def __init__(self, nc, name, unit):
        self.sem = nc.semaphore(name).__enter__()
        self.unit = unit
        self.cnt = 0


class Buf:
    __slots__ = ("w", "r")

    def __init__(self):
        self.w = {}
        self.r = {}


class Eng:
    def __init__(self, name, h, track):
        self.name, self.h, self.track, self.waited = name, h, track, {}


class K:
    def __init__(self, nc, n_dma_tracks=48):
        self.nc = nc
        self.eng = {}
        for name, h in (("pe", nc.tensor), ("act", nc.scalar), ("dve", nc.vector),
                        ("pool", nc.gpsimd), ("sp", nc.sync)):
            self.eng[name] = Eng(name, h, Track(nc, "t_" + name, 1))
        self.free_dma = [Track(nc, "d%d" % i, 16) for i in range(n_dma_tracks)]
        self.used_dma = []
        self.all_tracks = [e.track for e in self.eng.values()] + list(self.free_dma)

    def dtrack(self):
        t = self.free_dma.pop()
        self.used_dma.append(t)
        return t

    def release_dma(self):
        self.free_dma.extend(self.used_dma)
        self.used_dma = []

    def _wait(self, e, deps):
        for tr, c in deps.items():
            if e.waited.get(tr, 0) < c:
                e.h.wait_ge(tr.sem, c * tr.unit)
                e.waited[tr] = c

    def op(self, eng, fn, reads=(), writes=(), track=None):
        e = self.eng[eng]
        tr = track or e.track
        deps = {}
        for b in reads:
            for t, c in b.w.items():
                if deps.get(t, 0) < c:
                    deps[t] = c
        for b in writes:
            for t, c in b.w.items():
                if deps.get(t, 0) < c:
                    deps[t] = c
            for t, c in b.r.items():
                if deps.get(t, 0) < c:
                    deps[t] = c
        if eng == "pe":
            deps.pop(e.track, None)
        self._wait(e, deps)
        ins = fn()
        ins.then_inc(tr.sem, tr.unit)
        tr.cnt += 1
        for b in reads:
            b.r[tr] = tr.cnt
        for b in writes:
            b.w[tr] = tr.cnt

    def barrier(self):
        for e in self.eng.values():
            self._wait(e, {t: t.cnt for t in self.all_tracks if t.cnt > 0})


def build(cfg=None, debug=False, upto="all"):
    cfg = cfg or Cfg()
    D, SEQ, KC, TL, NB, NBL, NK = cfg.D, cfg.SEQ, cfg.KC, cfg.TL, cfg.NB, cfg.NBL, cfg.NK
    nc = bass.Bass("TRN2", target_bir_lowering=False)
    k = K(nc)
    io = {}
    V, A, P, T, S = nc.vector, nc.scalar, nc.gpsimd, nc.tensor, nc.sync
    DBG = ("modf", "KT", "QT", "Vs", "yT", "x1") if debug else ()

    def din(name, shape, dt):
        io[name] = nc.dram_tensor(name, list(shape), dt, kind="ExternalInput").ap()

    def dout(name, shape, dt):
        io[name] = nc.dram_tensor(name, list(shape), dt, kind="ExternalOutput").ap()

    def dint(name, shape, dt):
        kind = "ExternalOutput" if name in DBG else "Internal"
        io[name] = nc.dram_tensor(name, list(shape), dt, kind=kind).ap()

    din("x_all", [SEQ, D], F32)
    din("x_own", [TL, D], F32)
    din("pos_all", [1, SEQ], I32)
    din("pos_own", [1, TL], I32)
    din("c_row", [1, D], F32)
    din("w_adaT", [cfg.NMOD, 128, D], F32)
    din("b_adaT", [128, cfg.NMOD], F32)
    din("n1gT", [128, KC], F32)
    din("n2gT", [128, KC], F32)
    din("n2g_row", [1, D], F32)
    din("fg_row", [1, D], F32)
    din("w_q", [D, D], F32)
    din("w_k", [D, D], F32)
    din("w_v", [D, D], F32)
    din("w_out", [D, D], F32)
    din("lam4", [4, 128], F32)
    din("subln_row", [1, 256], F32)
    din("dilg_row", [1, cfg.DILW], F32)
    din("peer_wq", [D, PH * 256], F32)
    din("skT", [2 * PH, 128, NK], F32)
    din("peer_u", [cfg.NEXP, D], F32)
    din("peer_v", [cfg.NEXP, D], F32)
    din("ident", [128, 128], BF16)
    din("perm", [128, 128], BF16)
    din("invf", [32, 1], F32)
    din("mdiff", [128, 8, 128], BF16)
    din("mdil", [128, 24, 128], BF16)
    din("iota16", [128, 16], F32)
    dout("out", [TL, D], F32)

    dint("modf", [cfg.NMOD * 128], F32)
    dint("wq_bf", [KC, 128, KC, 128], BF16)
    dint("wk_bf", [KC, 128, KC, 128], BF16)
    dint("wv_bf", [D // 256, 128, KC, 256], BF16)
    dint("KT", [KC, 128, SEQ], BF16)
    dint("QT", [KC, 128, TL], BF16)
    dint("Vs", [SEQ, D], BF16)
    dint("yT", [KC, 128, TL], BF16)
    dint("x1", [TL, D], F32)
    dint("u_bf", [cfg.NEXP, D], BF16)
    dint("v_bf", [cfg.NEXP, D], BF16)

    gs = ExitStack()

    def gsb(name, shape, dt):
        return gs.enter_context(nc.sbuf_tensor(name, list(shape), dt))

    ident = gsb("ident_sb", [128, 128], BF16)
    perm = gsb("perm_sb", [128, 128], BF16)
    modT = gsb("modT", [128, cfg.NMOD], F32)
    A1 = gsb("A1", [128, KC], F32)
    A2 = gsb("A2", [128, KC], F32)
    invf = gsb("invf_sb", [32, 1], F32)
    b_const, b_modT, b_A = Buf(), Buf(), Buf()
    tr0 = k.free_dma.pop()
    k.op("sp", lambda: S.dma_start(out=ident[:], in_=io["ident"]), writes=[b_const], track=tr0)
    k.op("sp", lambda: S.dma_start(out=perm[:], in_=io["perm"]), writes=[b_const], track=tr0)
    k.op("sp", lambda: S.dma_start(out=invf[:], in_=io["invf"]), writes=[b_const], track=tr0)

    def new_phase():
        es = ExitStack()

        def sb(name, shape, dt):
            return es.enter_context(nc.sbuf_tensor(name, list(shape), dt))

        def ps(name, shape, dt):
            return es.enter_context(nc.psum_tensor(name, list(shape), dt))
        return es, sb, ps

    def end_phase(es):
        k.barrier()
        es.close()
        k.release_dma()

    def finish():
        k.barrier()
        gs.close()
        return nc

    es, sb, ps = new_phase()
    cact = sb("cact", [128, D], F32)
    b_cact = Buf()
    trc = k.dtrack()
    k.op("sp", lambda: S.dma_start(out=cact[:], in_=io["c_row"].to_broadcast([128, D])), writes=[b_cact], track=trc)
    k.op("act", lambda: A.activation(out=cact[:], in_=cact[:], func=AF.Silu), reads=[b_cact], writes=[b_cact])
    junk = sb("junk0", [128, D], BF16)
    b_junk = Buf()
    badaT = sb("badaT", [128, cfg.NMOD], F32)
    n1gT = sb("n1gT_sb", [128, KC], F32)
    n2gT = sb("n2gT_sb", [128, KC], F32)
    b_ld = Buf()
    k.op("sp", lambda: S.dma_start(out=badaT[:], in_=io["b_adaT"]), writes=[b_ld], track=trc)
    k.op("sp", lambda: S.dma_start(out=n1gT[:], in_=io["n1gT"]), writes=[b_ld], track=trc)
    k.op("sp", lambda: S.dma_start(out=n2gT[:], in_=io["n2gT"]), writes=[b_ld], track=trc)
    NWB = 4
    wa = [sb("wa%d" % i, [128, D], F32) for i in range(NWB)]
    b_wa = [Buf() for _ in range(NWB)]
    tr_wa = [k.dtrack() for _ in range(NWB)]
    for j in range(cfg.NMOD):
        i = j % NWB
        k.op("sp", lambda i=i, j=j: S.dma_start(out=wa[i][:], in_=io["w_adaT"][j]), writes=[b_wa[i]], track=tr_wa[i])
        k.op("dve", lambda i=i, j=j: V.scalar_tensor_tensor(
            out=junk[:], in0=wa[i][:], scalar=1.0, in1=cact[:], op0=ALU.mult, op1=ALU.mult,
            accum_out=modT[:, j:j + 1]), reads=[b_wa[i], b_cact], writes=[b_junk, b_modT])
    k.op("dve", lambda: V.tensor_tensor(out=modT[:], in0=modT[:], in1=badaT[:], op=ALU.add),
         reads=[b_ld, b_modT], writes=[b_modT])
    k.op("dve", lambda: V.scalar_tensor_tensor(out=A1[:], in0=modT[:, KC:2 * KC], scalar=1.0, in1=n1gT[:],
                                               op0=ALU.add, op1=ALU.mult), reads=[b_modT, b_ld], writes=[b_A])
    k.op("dve", lambda: V.scalar_tensor_tensor(out=A2[:], in0=modT[:, 4 * KC:5 * KC], scalar=1.0, in1=n2gT[:],
                                               op0=ALU.add, op1=ALU.mult), reads=[b_modT, b_ld], writes=[b_A])
    B1 = modT[:, 0:KC]
    B2 = modT[:, 3 * KC:4 * KC]
    b_modf = Buf()
    with nc.allow_non_contiguous_dma(reason="one-off mod vector relayout"):
        modfv = io["modf"].rearrange("(j p) -> p j", p=128)
        for j0 in range(0, cfg.NMOD, 8):
            k.op("sp", lambda j0=j0: S.dma_start(out=modfv[:, j0:j0 + 8], in_=modT[:, j0:j0 + 8]),
                 reads=[b_modT], writes=[b_modf], track=trc)
    end_phase(es)
    if upto == "P0":
        return finish()

    es, sb, ps = new_phase()
    b_wbf = Buf()
    stg = [sb("pc_f%d" % i, [128, 2, D], F32) for i in range(2)]
    stb = [sb("pc_b%d" % i, [128, 2, D], BF16) for i in range(2)]
    b_stg = [Buf() for _ in range(2)]
    b_stb = [Buf() for _ in range(2)]
    tr_stg = [k.dtrack() for _ in range(2)]
    tr_stb = [k.dtrack() for _ in range(2)]
    wjobs = []
    for (src, dst, cw) in (("w_q", "wq_bf", 128), ("w_k", "wk_bf", 128), ("w_v", "wv_bf", 256)):
        srcv = io[src].rearrange("(kk p) c -> p kk c", p=128)
        dstv = io[dst].rearrange("m p kk c -> p kk m c")
        for k0 in range(0, KC, 2):
            wjobs.append((srcv, dstv, cw, k0))

    def w_load(j):
        srcv, dstv, cw, k0 = wjobs[j]
        i = j % 2
        k.op("sp", lambda i=i, k0=k0, srcv=srcv: S.dma_start(out=stg[i][:], in_=srcv[:, k0:k0 + 2, :]),
             writes=[b_stg[i]], track=tr_stg[i])

    w_load(0)
    for j in range(len(wjobs)):
        srcv, dstv, cw, k0 = wjobs[j]
        i = j % 2
        if j + 1 < len(wjobs):
            w_load(j + 1)
        for kk in range(2):
            if (j + kk) % 2 == 0:
                k.op("act", lambda i=i, kk=kk: A.copy(out=stb[i][:, kk, :], in_=stg[i][:, kk, :]), reads=[b_stg[i]], writes=[b_stb[i]])
            else:
                k.op("dve", lambda i=i, kk=kk: V.tensor_copy(out=stb[i][:, kk, :], in_=stg[i][:, kk, :]), reads=[b_stg[i]], writes=[b_stb[i]])
        nm = D // cw
        ms = min(8, nm)
        for kk in range(2):
            for m0 in range(0, nm, ms):
                k.op("sp", lambda i=i, kk=kk, k0=k0, dstv=dstv, cw=cw, m0=m0, ms=ms: S.dma_start(
                    out=dstv[:, k0 + kk, m0:m0 + ms],
                    in_=stb[i][:, kk, m0 * cw:(m0 + ms) * cw].rearrange("p (m c) -> p m c", c=cw)),
                    reads=[b_stb[i]], writes=[b_wbf], track=tr_stb[i])
    end_phase(es)

    es, sb, ps = new_phase()
    CHm = cfg.CH
    hT = sb("hT", [128, KC, CHm], BF16)
    b_hT = Buf()
    xt = [sb("xt%d" % i, [128, D], F32) for i in range(2)]
    b_xt = [Buf() for _ in range(2)]
    tr_x = [k.dtrack() for _ in range(2)]
    xn = [sb("xn%d" % i, [128, D], BF16) for i in range(2)]
    b_xn = [Buf() for _ in range(2)]
    junk = sb("junk1", [128, D], BF16)
    b_junk = Buf()
    stat = sb("stat1", [128, 4], F32)
    b_stat = Buf()
    HK = KC // 2
    pT = [ps("pT%d" % i, [128, HK * 128], BF16) for i in range(2)]
    b_pT = [Buf() for _ in range(2)]
    pp = [ps("pp%d" % i, [128, 512], F32) for i in range(3)]
    b_pp = [Buf() for _ in range(3)]
    psw = ps("psw", [128, 512], F32)
    b_psw = Buf()
    posi = sb("posi", [32, CHm], I32)
    rw = [sb("rw%d" % i, [32, CHm], F32) for i in range(5)]
    rki = sb("rki", [32, CHm], I32)
    cos_t = sb("cos_t", [32, CHm], F32)
    sin_t = sb("sin_t", [32, CHm], F32)
    b_rot, b_tab = Buf(), Buf()
    tr_pos = k.dtrack()
    wkb = [sb("wkb%d" % i, [128, KC, 128], BF16) for i in range(2)]
    b_wkb = [Buf() for _ in range(2)]
    tr_wkb = [k.dtrack() for _ in range(2)]
    wvb = [sb("wvb%d" % i, [128, KC, 256], BF16) for i in range(2)]
    b_wvb = [Buf() for _ in range(2)]
    tr_wvb = [k.dtrack() for _ in range(2)]
    kst = [sb("kst%d" % i, [128, 512], BF16) for i in range(3)]
    b_kst = [Buf() for _ in range(3)]
    tr_kst = [k.dtrack() for _ in range(3)]
    vst = [sb("vst%d" % i, [128, 256], BF16) for i in range(3)]
    b_vst = [Buf() for _ in range(3)]
    tr_vst = [k.dtrack() for _ in range(3)]
    rt = [sb("rt%d" % i, [32, 512], F32) for i in range(2)]
    b_rt = Buf()
    b_KT, b_QT, b_Vs = Buf(), Buf(), Buf()
    cnt = {"x": 0, "pp": 0, "kst": 0, "vst": 0, "wk": 0, "wv": 0, "ev": 0}

    def rms_to_hT(xsrc, ntok, Acol, Bcol, hT_t, b_hT_t, readsA):
        for t in range(ntok // 128):
            i = cnt["x"] % 2
            cnt["x"] += 1
            k.op("sp", lambda i=i, t=t: S.dma_start(out=xt[i][:], in_=xsrc[t * 128:(t + 1) * 128, :]),
                 writes=[b_xt[i]], track=tr_x[i])
            k.op("dve", lambda i=i: V.scalar_tensor_tensor(
                out=junk[:], in0=xt[i][:], scalar=1.0, in1=xt[i][:], op0=ALU.mult, op1=ALU.mult,
                accum_out=stat[:, 0:1]), reads=[b_xt[i]], writes=[b_junk, b_stat])
            k.op("act", lambda: A.activation(out=stat[:, 1:2], in_=stat[:, 0:1], func=AF.Ln, scale=1.0 / D, bias=EPS),
                 reads=[b_stat], writes=[b_stat])
            k.op("act", lambda: A.activation(out=stat[:, 2:3], in_=stat[:, 1:2], func=AF.Exp, scale=-0.5),
                 reads=[b_stat], writes=[b_stat])
            k.op("pool", lambda i=i: P.tensor_scalar(out=xn[i][:], in0=xt[i][:], scalar1=stat[:, 2:3], scalar2=None,
                                                    op0=ALU.mult), reads=[b_xt[i], b_stat], writes=[b_xn[i]])
            for hh in range(2):
                for q in range(HK):
                    kk = hh * HK + q
                    k.op("pe", lambda i=i, kk=kk, q=q, hh=hh: T.transpose(
                        out=pT[hh][:, q * 128:(q + 1) * 128], in_=xn[i][:, kk * 128:(kk + 1) * 128], identity=ident[:]),
                        reads=[b_xn[i], b_const], writes=[b_pT[hh]])
                for q in range(HK):
                    kk = hh * HK + q
                    if q % 2 == 0:
                        k.op("act", lambda kk=kk, q=q, hh=hh, t=t: A.activation(
                            out=hT_t[:, kk, t * 128:(t + 1) * 128], in_=pT[hh][:, q * 128:(q + 1) * 128],
                            func=AF.Identity, scale=Acol[:, kk:kk + 1], bias=Bcol[:, kk:kk + 1]),
                            reads=[b_pT[hh]] + readsA, writes=[b_hT_t])
                    else:
                        k.op("dve", lambda kk=kk, q=q, hh=hh, t=t: V.tensor_scalar(
                            out=hT_t[:, kk, t * 128:(t + 1) * 128], in0=pT[hh][:, q * 128:(q + 1) * 128],
                            scalar1=Acol[:, kk:kk + 1], scalar2=Bcol[:, kk:kk + 1], op0=ALU.mult, op1=ALU.add),
                            reads=[b_pT[hh]] + readsA, writes=[b_hT_t])

    def rot_tables(pos_ap, ntok):
        k.op("sp", lambda: S.dma_start(out=posi[:, :ntok], in_=pos_ap.to_broadcast([32, ntok])),
             reads=[b_rot], writes=[b_rot], track=tr_pos)
        ang, a, y, kf, r = [w[:, :ntok] for w in rw]
        dv = lambda fn: k.op("dve", fn, reads=[b_rot, b_const], writes=[b_rot])
        dv(lambda: V.tensor_copy(out=ang, in_=posi[:, :ntok]))
        dv(lambda: V.tensor_scalar(out=ang, in0=ang, scalar1=invf[:, 0:1], scalar2=None, op0=ALU.mult))
        for tab, shift in ((sin_t, 0.0), (cos_t, 0.5 * math.pi)):
            dv(lambda shift=shift: V.tensor_scalar(out=a, in0=ang, scalar1=shift, scalar2=None, op0=ALU.add))
            dv(lambda: V.tensor_scalar(out=y, in0=a, scalar1=1.0 / TWO_PI, scalar2=0.5, op0=ALU.mult, op1=ALU.add))
            dv(lambda: V.tensor_copy(out=rki[:, :ntok], in_=y))
            dv(lambda: V.tensor_copy(out=kf, in_=rki[:, :ntok]))
            dv(lambda: V.scalar_tensor_tensor(out=r, in0=kf, scalar=-C1, in1=a, op0=ALU.mult, op1=ALU.add))
            dv(lambda: V.scalar_tensor_tensor(out=r, in0=kf, scalar=-C2, in1=r, op0=ALU.mult, op1=ALU.add))
            dv(lambda: V.tensor_scalar(out=y, in0=r, scalar1=-math.pi, scalar2=None, op0=ALU.is_lt))
            dv(lambda: V.scalar_tensor_tensor(out=r, in0=y, scalar=TWO_PI, in1=r, op0=ALU.mult, op1=ALU.add))
            dv(lambda: V.tensor_scalar(out=y, in0=r, scalar1=math.pi, scalar2=None, op0=ALU.is_gt))
            dv(lambda: V.scalar_tensor_tensor(out=r, in0=y, scalar=-TWO_PI, in1=r, op0=ALU.mult, op1=ALU.add))
            dv(lambda: V.tensor_scalar(out=r, in0=r, scalar1=-3.14159, scalar2=3.14159, op0=ALU.max, op1=ALU.min))
            k.op("act", lambda tab=tab: A.activation(out=tab[:, :ntok], in_=r, func=AF.Sin),
                 reads=[b_rot], writes=[b_tab])

    def proj_rot(wsrc, dst, b_dst, ntok, tok0):
        nn = min(512, ntok)
        for mp in range(KC):
            wi = cnt["wk"] % 2
            cnt["wk"] += 1
            k.op("sp", lambda wi=wi, mp=mp: S.dma_start(out=wkb[wi][:], in_=io[wsrc][mp]),
                 reads=[b_wbf], writes=[b_wkb[wi]], track=tr_wkb[wi])
            for hf in range(ntok // nn):
                pi = cnt["pp"] % 3
                cnt["pp"] += 1
                si = cnt["kst"] % 3
                cnt["kst"] += 1
                for kk in range(KC):
                    k.op("pe", lambda wi=wi, kk=kk, hf=hf, pi=pi: T.matmul(
                        pp[pi][:, :nn], lhsT=wkb[wi][:, kk, :], rhs=hT[:, kk, hf * nn:(hf + 1) * nn],
                        start=(kk == 0), stop=(kk == KC - 1)), reads=[b_wkb[wi], b_hT], writes=[b_pp[pi]])
                k.op("act", lambda pi=pi, si=si: A.copy(out=kst[si][:, :nn], in_=pp[pi][:, :nn]),
                     reads=[b_pp[pi]], writes=[b_kst[si]])
                k.op("pe", lambda si=si: T.matmul(psw[:, :nn], lhsT=perm[:], rhs=kst[si][:, :nn], start=True, stop=True),
                     reads=[b_kst[si], b_const], writes=[b_psw])
                sl = slice(hf * nn, (hf + 1) * nn)
                k.op("dve", lambda sl=sl: V.tensor_tensor(out=rt[0][:, :nn], in0=psw[0:32, :nn], in1=sin_t[:, sl], op=ALU.mult),
                     reads=[b_psw, b_tab], writes=[b_rt])
                k.op("dve", lambda sl=sl, pi=pi: V.tensor_tensor(out=rt[1][:, :nn], in0=pp[pi][0:32, :nn], in1=cos_t[:, sl], op=ALU.mult),
                     reads=[b_pp[pi], b_tab], writes=[b_rt])
                k.op("dve", lambda si=si: V.tensor_tensor(out=kst[si][0:32, :nn], in0=rt[0][:, :nn], in1=rt[1][:, :nn], op=ALU.add),
                     reads=[b_rt], writes=[b_kst[si]])
                k.op("pool", lambda si=si, mp=mp, hf=hf: P.dma_start(
                    out=io[dst][mp][:, tok0 + hf * nn: tok0 + (hf + 1) * nn], in_=kst[si][:, :nn]),
                    reads=[b_kst[si]], writes=[b_dst], track=tr_kst[si])

    def proj_v(ntok, tok0):
        for vc in range(D // 256):
            wi = cnt["wv"] % 2
            cnt["wv"] += 1
            k.op("sp", lambda wi=wi, vc=vc: S.dma_start(out=wvb[wi][:], in_=io["wv_bf"][vc]),
                 reads=[b_wbf], writes=[b_wvb[wi]], track=tr_wvb[wi])
            for tt in range(ntok // 128):
                pi = cnt["pp"] % 3
                cnt["pp"] += 1
                si = cnt["vst"] % 3
                cnt["vst"] += 1
                for kk in range(KC):
                    k.op("pe", lambda wi=wi, kk=kk, tt=tt, pi=pi: T.matmul(
                        pp[pi][:, :256], lhsT=hT[:, kk, tt * 128:(tt + 1) * 128], rhs=wvb[wi][:, kk, :],
                        start=(kk == 0), stop=(kk == KC - 1)), reads=[b_wvb[wi], b_hT], writes=[b_pp[pi]])
                cnt["ev"] += 1
                if cnt["ev"] % 2 == 0:
                    k.op("act", lambda pi=pi, si=si: A.copy(out=vst[si][:], in_=pp[pi][:, :256]),
                         reads=[b_pp[pi]], writes=[b_vst[si]])
                else:
                    k.op("dve", lambda pi=pi, si=si: V.tensor_copy(out=vst[si][:], in_=pp[pi][:, :256]),
                         reads=[b_pp[pi]], writes=[b_vst[si]])
                k.op("pool", lambda si=si, vc=vc, tt=tt: P.dma_start(
                    out=io["Vs"][tok0 + tt * 128: tok0 + (tt + 1) * 128, vc * 256:(vc + 1) * 256], in_=vst[si][:]),
                    reads=[b_vst[si]], writes=[b_Vs], track=tr_vst[si])

    OCH = min(cfg.CH, TL)
    for oc in range(TL // OCH):
        t0 = oc * OCH
        rms_to_hT(io["x_own"][t0:t0 + OCH, :], OCH, A1, B1, hT, b_hT, [b_A, b_modT])
        rot_tables(io["pos_own"][:, t0:t0 + OCH], OCH)
        proj_rot("wq_bf", "QT", b_QT, OCH, t0)
    for ch in range(cfg.NCH):
        t0 = ch * cfg.CH
        rms_to_hT(io["x_all"][t0:t0 + cfg.CH, :], cfg.CH, A1, B1, hT, b_hT, [b_A, b_modT])
        rot_tables(io["pos_all"][:, t0:t0 + cfg.CH], cfg.CH)
        proj_rot("wk_bf", "KT", b_KT, cfg.CH, t0)
        proj_v(cfg.CH, t0)
    end_phase(es)
    if upto == "P1":
        return finish()

    es, sb, ps = new_phase()
    mdiff = sb("mdiff_sb", [128, 8, 128], BF16)
    mdil = sb("mdil_sb", [128, 24, 128], BF16)
    lamt = sb("lamt", [128, 4, 128], F32)
    lamw = sb("lamw", [128, 8], F32)
    gsub = sb("gsub", [128, 256], F32)
    gdil = sb("gdil", [128, cfg.DILW], F32)
    b_c2 = Buf()
    trc = k.dtrack()
    k.op("sp", lambda: S.dma_start(out=mdiff[:], in_=io["mdiff"]), writes=[b_c2], track=trc)
    k.op("sp", lambda: S.dma_start(out=mdil[:], in_=io["mdil"]), writes=[b_c2], track=trc)
    for i in range(4):
        k.op("sp", lambda i=i: S.dma_start(out=lamt[:, i, :], in_=io["lam4"][i:i + 1, :].to_broadcast([128, 128])),
             writes=[b_c2], track=trc)
    k.op("sp", lambda: S.dma_start(out=gsub[:], in_=io["subln_row"].to_broadcast([128, 256])), writes=[b_c2], track=trc)
    k.op("sp", lambda: S.dma_start(out=gdil[:], in_=io["dilg_row"].to_broadcast([128, cfg.DILW])), writes=[b_c2], track=trc)
    junk2 = sb("junk2", [128, 256], F32)
    b_j2 = Buf()
    for i in range(2):
        k.op("dve", lambda i=i: V.scalar_tensor_tensor(out=junk2[:, :128], in0=lamt[:, 2 * i, :], scalar=1.0,
                                                      in1=lamt[:, 2 * i + 1, :], op0=ALU.mult, op1=ALU.mult,
                                                      accum_out=lamw[:, i:i + 1]), reads=[b_c2], writes=[b_c2, b_j2])
    k.op("act", lambda: A.activation(out=lamw[:, 2:4], in_=lamw[:, 0:2], func=AF.Exp), reads=[b_c2], writes=[b_c2])
    k.op("dve", lambda: V.tensor_tensor(out=lamw[:, 4:5], in0=lamw[:, 3:4], in1=lamw[:, 2:3], op=ALU.subtract),
         reads=[b_c2], writes=[b_c2])
    k.op("dve", lambda: V.tensor_scalar(out=lamw[:, 5:6], in0=lamw[:, 4:5], scalar1=-LAM_INIT, scalar2=None, op0=ALU.add),
         reads=[b_c2], writes=[b_c2])
    k.op("dve", lambda: V.tensor_scalar(out=gsub[:], in0=gsub[:], scalar1=1.0 - LAM_INIT, scalar2=None, op0=ALU.mult),
         reads=[b_c2], writes=[b_c2])
    nlam = lamw[:, 5:6]

    ktb = [sb("ktb%d" % i, [128, SEQ], BF16) for i in range(4)]
    b_ktb = [Buf() for _ in range(4)]
    tr_ktb = [k.dtrack() for _ in range(4)]
    qtb = [sb("qtb%d" % i, [128, TL], BF16) for i in range(4)]
    b_qtb = [Buf() for _ in range(4)]
    tr_qtb = [k.dtrack() for _ in range(4)]
    vab = [sb("vab%d" % i, [128, NB, 257], BF16) for i in range(2)]
    b_vab = [Buf() for _ in range(2)]
    tr_vab = [k.dtrack() for _ in range(2)]
    for i in range(2):
        k.op("pool", lambda i=i: P.memset(vab[i][:], 1.0), writes=[b_vab[i]])
    sps = [ps("sps%d" % i, [128, 512], F32) for i in range(3)]
    b_sps = [Buf() for _ in range(3)]
    Ops = [ps("Ops%d" % i, [128, 512], F32) for i in range(4)]
    b_O = [Buf() for _ in range(4)]
    tps = ps("tps", [128, 256], BF16)
    b_tps = Buf()
    ptb = [sb("ptb%d" % i, [128, 512], BF16) for i in range(3)]
    b_ptb = [Buf() for _ in range(3)]
    fw = sb("fw", [128, 16], F32)
    b_fw = Buf()
    ft = [sb("ft%d" % i, [128, 256], F32) for i in range(2)]
    b_ft = Buf()
    yb = sb("yb", [128, 256], BF16)
    b_yb = Buf()
    yts = [sb("yts%d" % i, [128, 2, 128], BF16) for i in range(2)]
    b_yts = [Buf() for _ in range(2)]
    tr_yts = [k.dtrack() for _ in range(2)]
    b_yT = Buf()
    c2 = {"s": 0, "kq": 0, "v": 0, "y": 0, "o": 0}
    pcf = [sb("pcf%d" % i, [128, D], F32) for i in range(2)]
    pcb = [sb("pcb0", [128, D], BF16)] * 2
    b_pcf = [Buf() for _ in range(2)]
    b_pcb = [Buf()] * 2
    tr_pcf = [k.dtrack() for _ in range(2)]
    tr_pcb = [k.dtrack()] * 2
    b_tabs = Buf()
    NBLK = cfg.NEXP // 128
    pc_jobs = [(src, dst, b) for (src, dst) in (("peer_u", "u_bf"), ("peer_v", "v_bf")) for b in range(NBLK)]
    pc_state = {"next": 0}
    n_iter = (cfg.NDH + cfg.NSH) * NBL
    pc_per_iter = -(-len(pc_jobs) // n_iter)

    def pc_finish(jn):
        src, dst, b = pc_jobs[jn]
        i = jn % 2
        if jn % 2 == 0:
            k.op("pool", lambda i=i: P.tensor_copy(out=pcb[i][:], in_=pcf[i][:]), reads=[b_pcf[i]], writes=[b_pcb[i]])
        else:
            for q in range(4):
                k.op("dve", lambda i=i, q=q: V.tensor_copy(out=pcb[i][:, q * (D // 4):(q + 1) * (D // 4)],
                                                          in_=pcf[i][:, q * (D // 4):(q + 1) * (D // 4)]),
                     reads=[b_pcf[i]], writes=[b_pcb[i]])
        k.op("pool", lambda i=i, dst=dst, b=b: P.dma_start(out=io[dst][b * 128:(b + 1) * 128, :], in_=pcb[i][:]),
             reads=[b_pcb[i]], writes=[b_tabs], track=tr_pcb[i])

    def pc_steps(n):
        for _ in range(n):
            jn = pc_state["next"]
            if jn > len(pc_jobs):
                return
            if jn < len(pc_jobs):
                src, dst, b = pc_jobs[jn]
                i = jn % 2
                k.op("pool", lambda i=i, src=src, b=b: P.dma_start(out=pcf[i][:], in_=io[src][b * 128:(b + 1) * 128, :]),
                     writes=[b_pcf[i]], track=tr_pcf[i])
            if jn >= 1:
                pc_finish(jn - 1)
            pc_state["next"] = jn + 1

    def attend(kt, b_kt, qt, b_qt, va, b_va, dv, m, kbs, mask_of, O, b_Oa, mid_hook=None):
        groups = [kbs[i:i + 4] for i in range(0, len(kbs), 4)]
        hook_at = min(1, len(groups) - 1)
        first = True
        for gi, grp in enumerate(groups):
            si = c2["s"] % 3
            c2["s"] += 1
            n = len(grp) * 128
            for j, kb in enumerate(grp):
                k.op("pe", lambda j=j, kb=kb, si=si: T.matmul(
                    sps[si][:, j * 128:(j + 1) * 128], lhsT=kt[:, kb * 128:(kb + 1) * 128],
                    rhs=qt[:, m * 128:(m + 1) * 128], start=True, stop=True),
                    reads=[b_kt, b_qt], writes=[b_sps[si]])
            k.op("act", lambda si=si, n=n: A.activation(out=ptb[si][:, :n], in_=sps[si][:, :n], func=AF.Exp, scale=SCALE),
                 reads=[b_sps[si]], writes=[b_ptb[si]])
            mk = mask_of(grp[0], len(grp))
            if mk is not None:
                k.op("dve", lambda si=si, n=n, mk=mk: V.tensor_tensor(out=ptb[si][:, :n], in0=ptb[si][:, :n], in1=mk, op=ALU.mult),
                     reads=[b_ptb[si], b_c2], writes=[b_ptb[si]])
            for j, kb in enumerate(grp):
                last = (gi == len(groups) - 1) and (j == len(grp) - 1)
                k.op("pe", lambda j=j, kb=kb, si=si, first=first, last=last: T.matmul(
                    O[:, :dv + 1], lhsT=ptb[si][:, j * 128:(j + 1) * 128], rhs=va[:, kb, 0:dv + 1],
                    start=first, stop=last), reads=[b_ptb[si], b_va], writes=[b_Oa])
                first = False
            if mid_hook is not None and gi == hook_at:
                mid_hook()

    def rstd_from_ss(ss_ap, n, out_ap):
        k.op("act", lambda: A.activation(out=fw[:, 8:9], in_=ss_ap, func=AF.Ln, scale=1.0 / n, bias=EPS),
             reads=[b_fw], writes=[b_fw])
        k.op("act", lambda: A.activation(out=out_ap, in_=fw[:, 8:9], func=AF.Exp, scale=-0.5),
             reads=[b_fw], writes=[b_fw])

    def emit_yT(width, chunk0, m):
        yi = c2["y"] % 2
        c2["y"] += 1
        nj = width // 128
        for j in range(nj):
            k.op("pe", lambda j=j: T.transpose(out=tps[:, j * 128:(j + 1) * 128], in_=yb[:, j * 128:(j + 1) * 128], identity=ident[:]),
                 reads=[b_yb, b_const], writes=[b_tps])
        k.op("act", lambda yi=yi, nj=nj: A.copy(out=yts[yi][:, :nj, :].rearrange("p j q -> p (j q)"), in_=tps[:, :nj * 128]),
             reads=[b_tps], writes=[b_yts[yi]])
        for j in range(nj):
            k.op("pool", lambda yi=yi, j=j: P.dma_start(out=io["yT"][chunk0 + j][:, m * 128:(m + 1) * 128], in_=yts[yi][:, j, :]),
                 reads=[b_yts[yi]], writes=[b_yT], track=tr_yts[yi])

    pend = {"f": None}
    for h in range(cfg.NDH):
        vi = c2["v"] % 2
        c2["v"] += 1
        for b0 in range(0, NB, 8):
            k.op("sp", lambda vi=vi, h=h, b0=b0: S.dma_start(
                out=vab[vi][:, b0:b0 + 8, 0:256],
                in_=io["Vs"][b0 * 128:(b0 + 8) * 128, 256 * h:256 * (h + 1)].rearrange("(b p) c -> p b c", p=128)),
                reads=[b_Vs], writes=[b_vab[vi]], track=tr_vab[vi])
        kq = []
        for s in range(2):
            bi = c2["kq"] % 4
            c2["kq"] += 1
            k.op("sp", lambda bi=bi, h=h, s=s: S.dma_start(out=ktb[bi][:], in_=io["KT"][2 * h + s]),
                 reads=[b_KT], writes=[b_ktb[bi]], track=tr_ktb[bi])
            k.op("sp", lambda bi=bi, h=h, s=s: S.dma_start(out=qtb[bi][:], in_=io["QT"][2 * h + s]),
                 reads=[b_QT], writes=[b_qtb[bi]], track=tr_qtb[bi])
            kq.append(bi)
        for m in range(NBL):
            kbs = list(range(0, 8 * m + 8))

            def mask_of(kb0, n, m=m):
                r0 = kb0 - 8 * m
                if r0 < 0:
                    return None
                return mdiff[:, r0:r0 + n, :].rearrange("p r q -> p (r q)")
            ob = (m % 2) * 2
            for s in range(2):
                bi = kq[s]
                hook = None
                if s == 0 and pend["f"] is not None:
                    hook, pend["f"] = pend["f"], None
                attend(ktb[bi], b_ktb[bi], qtb[bi], b_qtb[bi], vab[vi], b_vab[vi], 256, m, kbs, mask_of, Ops[ob + s], b_O[ob + s],
                       mid_hook=hook)

            def fin_diff(h=h, m=m, O1=Ops[ob], O2=Ops[ob + 1], bO1=b_O[ob], bO2=b_O[ob + 1]):
                fin = lambda fn, r=(), w=(): k.op("dve", fn, reads=[b_fw, b_ft, b_c2] + list(r), writes=[b_fw, b_ft] + list(w))
                fin(lambda: V.reciprocal(out=fw[:, 0:1], in_=O1[:, 256:257]), r=[bO1])
                fin(lambda: V.reciprocal(out=fw[:, 1:2], in_=O2[:, 256:257]), r=[bO2])
                fin(lambda: V.tensor_tensor(out=fw[:, 2:3], in0=fw[:, 1:2], in1=nlam, op=ALU.mult))
                fin(lambda: V.tensor_scalar(out=ft[0][:], in0=O2[:, 0:256], scalar1=fw[:, 2:3], scalar2=None, op0=ALU.mult), r=[bO2])
                fin(lambda: V.scalar_tensor_tensor(out=ft[1][:], in0=O1[:, 0:256], scalar=fw[:, 0:1], in1=ft[0][:],
                                                   op0=ALU.mult, op1=ALU.add), r=[bO1])
                fin(lambda: V.scalar_tensor_tensor(out=junk2[:], in0=ft[1][:], scalar=1.0, in1=ft[1][:], op0=ALU.mult, op1=ALU.mult,
                                                   accum_out=fw[:, 3:4]), w=[b_j2])
                rstd_from_ss(fw[:, 3:4], 256, fw[:, 4:5])
                fin(lambda: V.scalar_tensor_tensor(out=yb[:], in0=ft[1][:], scalar=fw[:, 4:5], in1=gsub[:], op0=ALU.mult, op1=ALU.mult),
                    w=[b_yb])
                emit_yT(256, 2 * h, m)
            pend["f"] = fin_diff
            pc_steps(pc_per_iter)

    for hd in range(cfg.NSH):
        vi = c2["v"] % 2
        c2["v"] += 1
        for b0 in range(0, NB, 8):
            k.op("sp", lambda vi=vi, hd=hd, b0=b0: S.dma_start(
                out=vab[vi][:, b0:b0 + 8, 0:128],
                in_=io["Vs"][b0 * 128:(b0 + 8) * 128, cfg.DIFFW + 128 * hd: cfg.DIFFW + 128 * (hd + 1)].rearrange("(b p) c -> p b c", p=128)),
                reads=[b_Vs], writes=[b_vab[vi]], track=tr_vab[vi])
        k.op("pool", lambda vi=vi: P.memset(vab[vi][:, :, 128:129], 1.0), writes=[b_vab[vi]])
        bi = c2["kq"] % 4
        c2["kq"] += 1
        mpi = cfg.DIFFW // 128 + hd
        k.op("sp", lambda bi=bi, mpi=mpi: S.dma_start(out=ktb[bi][:], in_=io["KT"][mpi]),
             reads=[b_KT], writes=[b_ktb[bi]], track=tr_ktb[bi])
        k.op("sp", lambda bi=bi, mpi=mpi: S.dma_start(out=qtb[bi][:], in_=io["QT"][mpi]),
             reads=[b_QT], writes=[b_qtb[bi]], track=tr_qtb[bi])
        for m in range(NBL):
            kb_lo = max(0, 8 * m - 16)
            kbs = list(range(kb_lo, 8 * m + 8))

            def mask_of(kb0, n, m=m):
                r0 = kb0 - (8 * m - 16)
                return mdil[:, r0:r0 + n, :].rearrange("p r q -> p (r q)")
            oi = c2["o"] % 4
            c2["o"] += 1
            O1, bO1 = Ops[oi], b_O[oi]
            hook, pend["f"] = pend["f"], None
            attend(ktb[bi], b_ktb[bi], qtb[bi], b_qtb[bi], vab[vi], b_vab[vi], 128, m, kbs, mask_of, O1, bO1, mid_hook=hook)

            def fin_dil(hd=hd, m=m, O1=O1, bO1=bO1):
                fin = lambda fn, r=(), w=(): k.op("dve", fn, reads=[b_fw, b_ft, b_c2] + list(r), writes=[b_fw, b_ft] + list(w))
                fin(lambda: V.reciprocal(out=fw[:, 0:1], in_=O1[:, 128:129]), r=[bO1])
                fin(lambda: V.tensor_scalar(out=ft[1][:, :128], in0=O1[:, 0:128], scalar1=fw[:, 0:1], scalar2=None, op0=ALU.mult),
                    r=[bO1])
                fin(lambda: V.scalar_tensor_tensor(out=junk2[:, :128], in0=ft[1][:, :128], scalar=1.0, in1=ft[1][:, :128],
                                                   op0=ALU.mult, op1=ALU.mult, accum_out=fw[:, 3:4]), w=[b_j2])
                rstd_from_ss(fw[:, 3:4], 128, fw[:, 4:5])
                fin(lambda: V.scalar_tensor_tensor(out=yb[:, :128], in0=ft[1][:, :128], scalar=fw[:, 4:5],
                                                   in1=gdil[:, 128 * hd:128 * (hd + 1)], op0=ALU.mult, op1=ALU.mult), w=[b_yb])
                emit_yT(128, cfg.DIFFW // 128 + hd, m)
            pend["f"] = fin_dil
            pc_steps(pc_per_iter)
    if pend["f"] is not None:
        pend["f"]()
        pend["f"] = None
    pc_steps(len(pc_jobs) + 2)
    end_phase(es)
    if upto == "P2":
        return finish()

    es, sb, ps = new_phase()
    yTs = sb("yTs", [128, KC, TL], BF16)
    b_yTs = Buf()
    trc = k.dtrack()
    yTv = io["yT"].rearrange("c p t -> p c t")
    for c0 in range(0, KC, 8):
        k.op("sp", lambda c0=c0: S.dma_start(out=yTs[:, c0:c0 + 8, :], in_=yTv[:, c0:c0 + 8, :]), reads=[b_yT], writes=[b_yTs], track=trc)
    g1row = sb("g1row", [128, D], F32)
    b_g1 = Buf()
    k.op("sp", lambda: S.dma_start(out=g1row[:], in_=io["modf"][2 * D:3 * D].rearrange("(o d) -> o d", o=1).to_broadcast([128, D])),
         reads=[b_modf], writes=[b_g1], track=trc)
    wof = [sb("wof%d" % i, [128, 4, 512], F32) for i in range(2)]
    b_wof = [Buf() for _ in range(2)]
    tr_wof = [k.dtrack() for _ in range(2)]
    wob = [sb("wob%d" % i, [128, KC, 512], BF16) for i in range(2)]
    b_wob = [Buf() for _ in range(2)]
    xs = [sb("xs%d" % i, [128, 512], F32) for i in range(2)]
    b_xs = [Buf() for _ in range(2)]
    tr_xs = [k.dtrack() for _ in range(2)]
    x1s = [sb("x1s%d" % i, [128, 512], F32) for i in range(2)]
    b_x1s = [Buf() for _ in range(2)]
    tr_x1s = [k.dtrack() for _ in range(2)]
    po = [ps("po%d" % i, [128, 512], F32) for i in range(2)]
    b_po = [Buf() for _ in range(2)]
    b_x1 = Buf()
    wov = io["w_out"].rearrange("(kk p) c -> p kk c", p=128)
    c3 = {"f": 0, "x": 0}
    for dc in range(D // 512):
        wi = dc % 2
        for k0 in range(0, KC, 4):
            fi = c3["f"] % 2
            c3["f"] += 1
            k.op("sp", lambda fi=fi, k0=k0, dc=dc: S.dma_start(out=wof[fi][:], in_=wov[:, k0:k0 + 4, dc * 512:(dc + 1) * 512]),
                 writes=[b_wof[fi]], track=tr_wof[fi])
            if c3["f"] % 2 == 0:
                k.op("pool", lambda fi=fi, wi=wi, k0=k0: P.tensor_copy(out=wob[wi][:, k0:k0 + 4, :], in_=wof[fi][:]),
                     reads=[b_wof[fi]], writes=[b_wob[wi]])
            else:
                k.op("act", lambda fi=fi, wi=wi, k0=k0: A.copy(out=wob[wi][:, k0:k0 + 4, :], in_=wof[fi][:]),
                     reads=[b_wof[fi]], writes=[b_wob[wi]])
        for t in range(NBL):
            xi = c3["x"] % 2
            c3["x"] += 1
            k.op("sp", lambda xi=xi, t=t, dc=dc: S.dma_start(out=xs[xi][:], in_=io["x_own"][t * 128:(t + 1) * 128, dc * 512:(dc + 1) * 512]),
                 writes=[b_xs[xi]], track=tr_xs[xi])
            for kk in range(KC):
                k.op("pe", lambda xi=xi, kk=kk, t=t, wi=wi: T.matmul(
                    po[xi][:], lhsT=yTs[:, kk, t * 128:(t + 1) * 128], rhs=wob[wi][:, kk, :],
                    start=(kk == 0), stop=(kk == KC - 1)), reads=[b_yTs, b_wob[wi]], writes=[b_po[xi]])
            k.op("dve", lambda xi=xi, dc=dc: V.tensor_tensor(out=x1s[xi][:], in0=po[xi][:], in1=g1row[:, dc * 512:(dc + 1) * 512], op=ALU.mult),
                 reads=[b_po[xi], b_g1], writes=[b_x1s[xi]])
            k.op("dve", lambda xi=xi: V.tensor_tensor(out=x1s[xi][:], in0=x1s[xi][:], in1=xs[xi][:], op=ALU.add),
                 reads=[b_xs[xi], b_x1s[xi]], writes=[b_x1s[xi]])
            k.op("pool", lambda xi=xi, t=t, dc=dc: P.dma_start(out=io["x1"][t * 128:(t + 1) * 128, dc * 512:(dc + 1) * 512], in_=x1s[xi][:]),
                 reads=[b_x1s[xi]], writes=[b_x1], track=tr_x1s[xi])
    end_phase(es)
    if upto == "P3":
        return finish()

    es4, sb4, ps4 = new_phase()
    NKH = 2 * PH
    NSL = PH * TOPK
    rstd2 = sb4("rstd2", [128, NBL], F32)
    topv = sb4("topv", [128, NBL, NKH, 16], F32)
    topi = sb4("topi", [128, NBL, NKH, 16], U32)
    eidf = sb4("eidf", [128, NBL, NSL], F32)
    eidi = sb4("eidi", [128, NBL, NSL], I32)
    gate = sb4("gate", [128, NBL, NSL], F32)
    es_ab, sb_ab, ps_ab = new_phase()
    h2T = sb_ab("h2T", [128, KC, TL], BF16)
    es, sb, ps = new_phase()
    b_h2T = Buf()
    xt = [sb("xt4_%d" % i, [128, D], F32) for i in range(2)]
    b_xt = [Buf() for _ in range(2)]
    tr_x = [k.dtrack() for _ in range(2)]
    xn = [sb("xn4_%d" % i, [128, D], BF16) for i in range(2)]
    b_xn = [Buf() for _ in range(2)]
    junk = sb("junk4", [128, D], BF16)
    b_junk = Buf()
    stat = sb("stat4", [128, 4], F32)
    b_stat = Buf()
    b_rstd2 = Buf()
    HK = KC // 2
    pT = [ps("pT4_%d" % i, [128, HK * 128], BF16) for i in range(2)]
    b_pT = [Buf() for _ in range(2)]
    cnt = {"x": 0}
    for t in range(NBL):
        i = cnt["x"] % 2
        cnt["x"] += 1
        k.op("sp", lambda i=i, t=t: S.dma_start(out=xt[i][:], in_=io["x1"][t * 128:(t + 1) * 128, :]),
             reads=[b_x1], writes=[b_xt[i]], track=tr_x[i])
        k.op("dve", lambda i=i: V.scalar_tensor_tensor(out=junk[:], in0=xt[i][:], scalar=1.0, in1=xt[i][:], op0=ALU.mult,
                                                      op1=ALU.mult, accum_out=stat[:, 0:1]), reads=[b_xt[i]], writes=[b_junk, b_stat])
        k.op("act", lambda: A.activation(out=stat[:, 1:2], in_=stat[:, 0:1], func=AF.Ln, scale=1.0 / D, bias=EPS),
             reads=[b_stat], writes=[b_stat])
        k.op("act", lambda t=t: A.activation(out=rstd2[:, t:t + 1], in_=stat[:, 1:2], func=AF.Exp, scale=-0.5),
             reads=[b_stat], writes=[b_rstd2])
        k.op("pool", lambda i=i, t=t: P.tensor_scalar(out=xn[i][:], in0=xt[i][:], scalar1=rstd2[:, t:t + 1], scalar2=None, op0=ALU.mult),
             reads=[b_xt[i], b_rstd2], writes=[b_xn[i]])
        for hh in range(2):
            for q in range(HK):
                kk = hh * HK + q
                k.op("pe", lambda i=i, kk=kk, q=q, hh=hh: T.transpose(
                    out=pT[hh][:, q * 128:(q + 1) * 128], in_=xn[i][:, kk * 128:(kk + 1) * 128], identity=ident[:]),
                    reads=[b_xn[i], b_const], writes=[b_pT[hh]])
            for q in range(HK):
                kk = hh * HK + q
                if q % 2 == 0:
                    k.op("act", lambda kk=kk, q=q, hh=hh, t=t: A.activation(
                        out=h2T[:, kk, t * 128:(t + 1) * 128], in_=pT[hh][:, q * 128:(q + 1) * 128],
                        func=AF.Identity, scale=A2[:, kk:kk + 1], bias=B2[:, kk:kk + 1]),
                        reads=[b_pT[hh], b_A, b_modT], writes=[b_h2T])
                else:
                    k.op("dve", lambda kk=kk, q=q, hh=hh, t=t: V.tensor_scalar(
                        out=h2T[:, kk, t * 128:(t + 1) * 128], in0=pT[hh][:, q * 128:(q + 1) * 128],
                        scalar1=A2[:, kk:kk + 1], scalar2=B2[:, kk:kk + 1], op0=ALU.mult, op1=ALU.add),
                        reads=[b_pT[hh], b_A, b_modT], writes=[b_h2T])
    k.barrier()
    es.close()
    es, sb, ps = new_phase()
    b_top = Buf()
    wqf = [sb("wqf%d" % i, [128, KC, 128], F32) for i in range(2)]
    b_wqf = [Buf() for _ in range(2)]
    tr_wqf = [k.dtrack() for _ in range(2)]
    wqb = [sb("wqb%d" % i, [128, KC, 128], BF16) for i in range(2)]
    b_wqb = [Buf() for _ in range(2)]
    skf = sb("skf", [128, NKH, NK], F32)
    skb = sb("skb", [128, NKH, NK], BF16)
    b_sk = Buf()
    trc = k.dtrack()
    skv = io["skT"].rearrange("j kk n -> kk j n")
    for j0 in range(0, NKH, 8):
        k.op("sp", lambda j0=j0: S.dma_start(out=skf[:, j0:j0 + 8, :], in_=skv[:, j0:j0 + 8, :]), writes=[b_sk], track=trc)
    k.op("dve", lambda: V.tensor_copy(out=skb[:], in_=skf[:]), reads=[b_sk], writes=[b_sk])
    qTs = sb("qTs", [128, TL], BF16)
    b_qTs = Buf()
    pq = [ps("pq%d" % i, [128, 512], F32) for i in range(2)]
    b_pq = [Buf() for _ in range(2)]
    psc = [ps("psc%d" % i, [128, NK], F32) for i in range(2)]
    b_psc = [Buf() for _ in range(2)]
    scs = [sb("scs%d" % i, [128, NK], F32) for i in range(2)]
    b_scs = [Buf() for _ in range(2)]
    wqv = io["peer_wq"].rearrange("(kk p) c -> p kk c", p=128)
    nn = min(512, TL)
    c4 = {"q": 0, "s": 0}
    for j in range(NKH):
        wi = j % 2
        for c0 in range(0, KC, 8):
            k.op("sp", lambda wi=wi, j=j, c0=c0: S.dma_start(out=wqf[wi][:, c0:c0 + 8, :], in_=wqv[:, c0:c0 + 8, j * 128:(j + 1) * 128]),
                 writes=[b_wqf[wi]], track=tr_wqf[wi])
        if j % 2 == 0:
            k.op("act", lambda wi=wi: A.copy(out=wqb[wi][:], in_=wqf[wi][:]), reads=[b_wqf[wi]], writes=[b_wqb[wi]])
        else:
            k.op("pool", lambda wi=wi: P.tensor_copy(out=wqb[wi][:], in_=wqf[wi][:]), reads=[b_wqf[wi]], writes=[b_wqb[wi]])
        for hf in range(TL // nn):
            qi = c4["q"] % 2
            c4["q"] += 1
            for kk in range(KC):
                k.op("pe", lambda wi=wi, kk=kk, hf=hf, qi=qi: T.matmul(
                    pq[qi][:, :nn], lhsT=wqb[wi][:, kk, :], rhs=h2T[:, kk, hf * nn:(hf + 1) * nn],
                    start=(kk == 0), stop=(kk == KC - 1)), reads=[b_wqb[wi], b_h2T], writes=[b_pq[qi]])
            k.op("act", lambda qi=qi, hf=hf: A.copy(out=qTs[:, hf * nn:(hf + 1) * nn], in_=pq[qi][:, :nn]),
                 reads=[b_pq[qi]], writes=[b_qTs])
        for t in range(NBL):
            si = c4["s"] % 2
            c4["s"] += 1
            k.op("pe", lambda si=si, t=t, j=j: T.matmul(psc[si][:], lhsT=qTs[:, t * 128:(t + 1) * 128], rhs=skb[:, j, :],
                                                        start=True, stop=True), reads=[b_qTs, b_sk], writes=[b_psc[si]])
            k.op("act", lambda si=si: A.copy(out=scs[si][:], in_=psc[si][:]), reads=[b_psc[si]], writes=[b_scs[si]])
            dv = lambda fn, si=si: k.op("dve", fn, reads=[b_scs[si], b_top], writes=[b_scs[si], b_top])
            dv(lambda si=si, t=t, j=j: V.max(out=topv[:, t, j, 0:8], in_=scs[si][:]))
            dv(lambda si=si, t=t, j=j: V.max_index(out=topi[:, t, j, 0:8], in_max=topv[:, t, j, 0:8], in_values=scs[si][:]))
            dv(lambda si=si, t=t, j=j: V.match_replace(out=scs[si][:], in_to_replace=topv[:, t, j, 0:8], in_values=scs[si][:], imm_value=-1e30))
            dv(lambda si=si, t=t, j=j: V.max(out=topv[:, t, j, 8:16], in_=scs[si][:]))
            dv(lambda si=si, t=t, j=j: V.max_index(out=topi[:, t, j, 8:16], in_max=topv[:, t, j, 8:16], in_values=scs[si][:]))
    k.barrier()
    es.close()
    es_ab.close()
    es, sb, ps = new_phase()
    b_sel = Buf()
    topif = sb("topif", [128, NBL, NKH, 16], F32)
    iota16 = sb("iota16_sb", [128, 16], F32)
    k.op("sp", lambda: S.dma_start(out=iota16[:], in_=io["iota16"]), writes=[b_sk], track=trc)
    k.op("dve", lambda: V.tensor_copy(out=topif[:], in_=topi[:]), reads=[b_top], writes=[b_top])
    cand = sb("cand", [128, 256], F32)
    cand2 = sb("cand2", [128, 256], F32)
    fv = sb("fv", [128, 16], F32)
    fpos = sb("fpos", [128, 16], U32)
    fa = sb("fa", [128, 16], U32)
    fbb = sb("fbb", [128, 16], U32)
    faf = sb("faf", [128, 16], F32)
    fbf = sb("fbf", [128, 16], F32)
    oh = sb("oh", [128, 16, 16], F32)
    sel0 = sb("sel0", [128, 16], F32)
    sel1 = sb("sel1", [128, 16], F32)
    gw = sb("gw", [128, 4], F32)
    b_w = Buf()
    for t in range(NBL):
        for h in range(PH):
            dv = lambda fn: k.op("dve", fn, reads=[b_w, b_top, b_sk], writes=[b_w])
            v0 = topv[:, t, 2 * h, :]
            v1 = topv[:, t, 2 * h + 1, :]
            i0 = topif[:, t, 2 * h, :]
            i1 = topif[:, t, 2 * h + 1, :]
            cand3 = cand[:].rearrange("p (a b) -> p a b", b=16)
            dv(lambda v0=v0, v1=v1, cand3=cand3: V.tensor_tensor(out=cand3, in0=v0.unsqueeze(2).to_broadcast([128, 16, 16]),
                                                                 in1=v1.unsqueeze(1).to_broadcast([128, 16, 16]), op=ALU.add))
            dv(lambda: V.max(out=fv[:, 0:8], in_=cand[:]))
            dv(lambda: V.max_index(out=fpos[:, 0:8], in_max=fv[:, 0:8], in_values=cand[:]))
            dv(lambda: V.match_replace(out=cand2[:], in_to_replace=fv[:, 0:8], in_values=cand[:], imm_value=-1e30))
            dv(lambda: V.max(out=fv[:, 8:16], in_=cand2[:]))
            dv(lambda: V.max_index(out=fpos[:, 8:16], in_max=fv[:, 8:16], in_values=cand2[:]))
            dv(lambda: V.tensor_single_scalar(out=fa[:], in_=fpos[:], scalar=4, op=ALU.logical_shift_right))
            dv(lambda: V.tensor_single_scalar(out=fbb[:], in_=fpos[:], scalar=15, op=ALU.bitwise_and))
            dv(lambda: V.tensor_copy(out=faf[:], in_=fa[:]))
            dv(lambda: V.tensor_copy(out=fbf[:], in_=fbb[:]))
            for (pf, ix, sel) in ((faf, i0, sel0), (fbf, i1, sel1)):
                dv(lambda pf=pf: V.tensor_tensor(out=oh[:], in0=pf[:].unsqueeze(2).to_broadcast([128, 16, 16]),
                                                 in1=iota16[:].unsqueeze(1).to_broadcast([128, 16, 16]), op=ALU.is_equal))
                dv(lambda ix=ix: V.tensor_tensor(out=oh[:], in0=oh[:], in1=ix.unsqueeze(1).to_broadcast([128, 16, 16]), op=ALU.mult))
                dv(lambda sel=sel: V.tensor_reduce(out=sel[:], in_=oh[:], axis=AX.X, op=ALU.add))
            k.op("dve", lambda t=t, h=h: V.scalar_tensor_tensor(out=eidf[:, t, h * 16:(h + 1) * 16], in0=sel0[:], scalar=float(NK),
                                                              in1=sel1[:], op0=ALU.mult, op1=ALU.add),
                 reads=[b_w], writes=[b_sel])
            dv(lambda: V.tensor_scalar(out=gw[:, 0:1], in0=fv[:, 0:1], scalar1=-1.0, scalar2=None, op0=ALU.mult))
            k.op("act", lambda: A.activation(out=cand2[:, 0:16], in_=fv[:], func=AF.Exp, bias=gw[:, 0:1], scale=1.0,
                                             accum_out=gw[:, 1:2]), reads=[b_w], writes=[b_w])
            dv(lambda: V.reciprocal(out=gw[:, 2:3], in_=gw[:, 1:2]))
            k.op("dve", lambda t=t, h=h: V.tensor_scalar(out=gate[:, t, h * 16:(h + 1) * 16], in0=cand2[:, 0:16],
                                                       scalar1=gw[:, 2:3], scalar2=None, op0=ALU.mult),
                 reads=[b_w], writes=[b_sel])
    k.op("dve", lambda: V.tensor_copy(out=eidi[:], in_=eidf[:]), reads=[b_sel], writes=[b_sel])
    if debug:
        dout("dbg_eid", [TL, NSL], I32)
        dout("dbg_gate", [TL, NSL], F32)
        k.op("sp", lambda: S.dma_start(out=io["dbg_eid"].rearrange("(t p) s -> p t s", p=128), in_=eidi[:]), reads=[b_sel], track=trc)
        k.op("sp", lambda: S.dma_start(out=io["dbg_gate"].rearrange("(t p) s -> p t s", p=128), in_=gate[:]), reads=[b_sel], track=trc)
    k.barrier()
    es.close()
    es, sb, ps = new_phase()
    xt = [sb("xt4d%d" % i, [128, D], F32) for i in range(2)]
    b_xt = [Buf() for _ in range(2)]
    junk = sb("junk4d", [128, D], BF16)
    b_junk = Buf()
    stat = sb("stat4d", [128, 4], F32)
    b_stat = Buf()
    A2row = sb("A2row", [128, D], F32)
    B2row = sb("B2row", [128, D], F32)
    g2row = sb("g2row", [128, D], F32)
    fgrow = sb("fgrow", [128, D], F32)
    b_rows = Buf()
    mrow = lambda q: io["modf"][q * D:(q + 1) * D].rearrange("(o d) -> o d", o=1).to_broadcast([128, D])
    k.op("sp", lambda: S.dma_start(out=A2row[:], in_=mrow(4)), reads=[b_modf], writes=[b_rows], track=trc)
    k.op("sp", lambda: S.dma_start(out=B2row[:], in_=mrow(3)), reads=[b_modf], writes=[b_rows], track=trc)
    k.op("sp", lambda: S.dma_start(out=g2row[:], in_=mrow(5)), reads=[b_modf], writes=[b_rows], track=trc)
    k.op("sp", lambda: S.dma_start(out=fgrow[:], in_=io["n2g_row"].to_broadcast([128, D])), writes=[b_rows], track=trc)
    k.op("dve", lambda: V.scalar_tensor_tensor(out=A2row[:], in0=A2row[:], scalar=1.0, in1=fgrow[:], op0=ALU.add, op1=ALU.mult),
         reads=[b_rows], writes=[b_rows])
    b_fg = Buf()
    k.op("sp", lambda: S.dma_start(out=fgrow[:], in_=io["fg_row"].to_broadcast([128, D])), reads=[b_rows], writes=[b_rows, b_fg], track=trc)
    h2 = [sb("h2_%d" % i, [128, D], BF16) for i in range(2)]
    b_h2 = [Buf() for _ in range(2)]
    NUG = 4
    ug = [sb("ug%d" % i, [128, D], BF16) for i in range(NUG)]
    b_ug = [Buf() for _ in range(NUG)]
    tr_ug = [k.dtrack() for _ in range(NUG)]
    pre = [sb("pre%d" % i, [128, NSL], F32) for i in range(2)]
    coef = [sb("coef%d" % i, [128, NSL], F32) for i in range(2)]
    b_pre = [Buf() for _ in range(2)]
    dg = [sb("dg%d" % i, [128, 128], BF16) for i in range(2)]
    b_dg = [Buf() for _ in range(2)]
    NPB = D // 512
    assert NPB <= 8
    pacc = [ps("pacc%d" % i, [128, 512], F32) for i in range(NPB)]
    b_pacc = Buf()
    x2 = sb("x2", [128, D], F32)
    b_x2 = Buf()
    tr_o = k.dtrack()
    uv = io["u_bf"]
    vv = io["v_bf"]
    c5 = {"g": 0, "d": 0}

    def prep_tile(t):
        i = t % 2
        k.op("sp", lambda i=i, t=t: S.dma_start(out=xt[i][:], in_=io["x1"][t * 128:(t + 1) * 128, :]),
             reads=[b_x1], writes=[b_xt[i]], track=tr_x[i])
        k.op("dve", lambda i=i, t=t: V.scalar_tensor_tensor(out=x2[:], in0=xt[i][:], scalar=rstd2[:, t:t + 1], in1=A2row[:],
                                                          op0=ALU.mult, op1=ALU.mult), reads=[b_xt[i], b_rstd2, b_rows], writes=[b_x2])
        k.op("dve", lambda i=i: V.tensor_tensor(out=h2[i][:], in0=x2[:], in1=B2row[:], op=ALU.add), reads=[b_x2, b_rows], writes=[b_h2[i]])

    def u_slot(t, s):
        i = t % 2
        gi = c5["g"] % NUG
        c5["g"] += 1
        k.op("pool", lambda gi=gi, t=t, s=s: P.indirect_dma_start(
            out=ug[gi][:], out_offset=None, in_=uv, in_offset=bass.IndirectOffsetOnAxis(ap=eidi[:, t, s:s + 1], axis=0)),
            reads=[b_sel, b_tabs], writes=[b_ug[gi]], track=tr_ug[gi])
        k.op("dve", lambda gi=gi, s=s, i=i: V.scalar_tensor_tensor(out=junk[:], in0=ug[gi][:], scalar=1.0, in1=h2[i][:], op0=ALU.mult,
                                                                 op1=ALU.mult, accum_out=pre[i][:, s:s + 1]),
             reads=[b_ug[gi], b_h2[i]], writes=[b_junk, b_pre[i]])

    def act_tile(t):
        i = t % 2
        k.op("act", lambda i=i: A.activation(out=coef[i][:], in_=pre[i][:], func=AF.Gelu), reads=[b_pre[i]], writes=[b_pre[i]])
        k.op("dve", lambda i=i, t=t: V.tensor_tensor(out=coef[i][:], in0=coef[i][:], in1=gate[:, t, :], op=ALU.mult),
             reads=[b_pre[i], b_sel], writes=[b_pre[i]])

    def v_slot(t, s):
        i = t % 2
        gi = c5["g"] % NUG
        c5["g"] += 1
        di = c5["d"] % 2
        c5["d"] += 1
        k.op("pool", lambda gi=gi, t=t, s=s: P.indirect_dma_start(
            out=ug[gi][:], out_offset=None, in_=vv, in_offset=bass.IndirectOffsetOnAxis(ap=eidi[:, t, s:s + 1], axis=0)),
            reads=[b_sel, b_tabs], writes=[b_ug[gi]], track=tr_ug[gi])
        k.op("act", lambda di=di, s=s, i=i: A.activation(out=dg[di][:], in_=ident[:], func=AF.Copy, scale=coef[i][:, s:s + 1]),
             reads=[b_pre[i], b_const], writes=[b_dg[di]])
        for q in range(NPB):
            k.op("pe", lambda q=q, gi=gi, di=di, s=s: T.matmul(
                pacc[q][:], lhsT=dg[di][:], rhs=ug[gi][:, q * 512:(q + 1) * 512],
                start=(s == 0), stop=(s == NSL - 1)), reads=[b_dg[di], b_ug[gi]], writes=[b_pacc])

    def fin_tile(t):
        i = t % 2
        for q in range(NPB):
            sl = slice(q * 512, (q + 1) * 512)
            k.op("dve", lambda q=q, sl=sl: V.tensor_tensor(out=x2[:, sl], in0=pacc[q][:], in1=g2row[:, sl], op=ALU.mult),
                 reads=[b_pacc, b_rows], writes=[b_x2])
        k.op("dve", lambda i=i: V.tensor_tensor(out=x2[:], in0=x2[:], in1=xt[i][:], op=ALU.add), reads=[b_x2, b_xt[i]], writes=[b_x2])
        k.op("dve", lambda: V.scalar_tensor_tensor(out=junk[:], in0=x2[:], scalar=1.0, in1=x2[:], op0=ALU.mult, op1=ALU.mult,
                                                   accum_out=stat[:, 0:1]), reads=[b_x2], writes=[b_junk, b_stat])
        k.op("act", lambda: A.activation(out=stat[:, 1:2], in_=stat[:, 0:1], func=AF.Ln, scale=1.0 / D, bias=EPS),
             reads=[b_stat], writes=[b_stat])
        k.op("act", lambda: A.activation(out=stat[:, 2:3], in_=stat[:, 1:2], func=AF.Exp, scale=-0.5), reads=[b_stat], writes=[b_stat])
        k.op("dve", lambda: V.scalar_tensor_tensor(out=x2[:], in0=x2[:], scalar=stat[:, 2:3], in1=fgrow[:], op0=ALU.mult, op1=ALU.mult),
             reads=[b_x2, b_stat, b_fg], writes=[b_x2])
        k.op("sp", lambda t=t: S.dma_start(out=io["out"][t * 128:(t + 1) * 128, :], in_=x2[:]), reads=[b_x2], track=tr_o)

    for t in range(NBL + 1):
        if t < NBL:
            prep_tile(t)
        for s in range(NSL):
            if t < NBL:
                u_slot(t, s)
            if t >= 1:
                v_slot(t - 1, s)
        if t < NBL:
            act_tile(t)
        if t >= 1:
            fin_tile(t - 1)
    k.barrier()
    es.close()
    end_phase(es4)
    return finish()


def _dil_mult(d):
    m = ((d >= 0) & (d <= 128)).astype(np.float32)
    m += ((d >= 0) & (d <= 512) & (d % 4 == 0)).astype(np.float32)
    m += ((d >= 0) & (d <= 2048) & (d % 16 == 0)).astype(np.float32)
    return m


def make_in_maps(inputs, cfg=None):
    cfg = cfg or Cfg()
    D, SEQ, KC, TL, NBL, NK = cfg.D, cfg.SEQ, cfg.KC, cfg.TL, cfg.NBL, cfg.NK
    f = lambda a: np.ascontiguousarray(np.asarray(a))
    x = np.asarray(inputs["x"])[0]
    pos = np.asarray(inputs["positions"])[0].astype(np.int32)
    w_in = np.asarray(inputs["w_in"])[0]
    DW, LW = cfg.DIFFW, cfg.DILW
    sh = {}
    sh["x_all"] = f(x)
    sh["pos_all"] = f(pos.reshape(1, SEQ))
    sh["c_row"] = f(np.asarray(inputs["c"]).reshape(1, D))
    sh["w_adaT"] = f(np.asarray(inputs["w_ada"])[0].T).reshape(cfg.NMOD, 128, D)
    sh["b_adaT"] = f(np.asarray(inputs["b_ada"])[0].reshape(cfg.NMOD, 128).T)
    sh["n1gT"] = f(np.asarray(inputs["norm1_g"])[0].reshape(KC, 128).T)
    sh["n2gT"] = f(np.asarray(inputs["norm2_g"])[0].reshape(KC, 128).T)
    sh["n2g_row"] = f(np.asarray(inputs["norm2_g"])[0].reshape(1, D))
    sh["fg_row"] = f(np.asarray(inputs["final_g"]).reshape(1, D))
    sh["w_q"] = f(np.concatenate([w_in[:, 0:DW], w_in[:, 3 * DW:3 * DW + LW]], axis=1))
    sh["w_k"] = f(np.concatenate([w_in[:, DW:2 * DW], w_in[:, 3 * DW + LW:3 * DW + 2 * LW]], axis=1))
    sh["w_v"] = f(np.concatenate([w_in[:, 2 * DW:3 * DW], w_in[:, 3 * DW + 2 * LW:3 * DW + 3 * LW]], axis=1))
    sh["w_out"] = f(np.asarray(inputs["w_out"])[0])
    sh["lam4"] = f(np.stack([np.asarray(inputs[n])[0] for n in ("lam_q1", "lam_k1", "lam_q2", "lam_k2")]))
    sh["subln_row"] = f(np.asarray(inputs["diff_subln_g"])[0].reshape(1, 256))
    sh["dilg_row"] = f(np.asarray(inputs["dil_out_g"])[0].reshape(1, LW))
    sh["peer_wq"] = f(np.asarray(inputs["peer_wq"])[0])
    sk = np.asarray(inputs["peer_subkeys"])[0]
    sh["skT"] = f(sk.reshape(2 * PH, NK, 128).transpose(0, 2, 1))
    sh["peer_u"] = f(np.asarray(inputs["peer_u"])[0])
    sh["peer_v"] = f(np.asarray(inputs["peer_v"])[0])
    bf = ml_dtypes.bfloat16
    sh["ident"] = np.eye(128, dtype=np.float32).astype(bf)
    pm = np.zeros((128, 128), np.float32)
    for m_ in range(16):
        pm[m_ + 16, m_] = -1.0
        pm[m_, m_ + 16] = 1.0
    sh["perm"] = pm.astype(bf)
    inv = np.power(np.float32(500000.0), -np.arange(0, 32, 2, dtype=np.float32) / np.float32(32)).astype(np.float32)
    sh["invf"] = f(np.tile(inv, 2).reshape(32, 1))
    sh["iota16"] = f(np.tile(np.arange(16, dtype=np.float32), (128, 1)))
    ki = np.arange(128)[:, None]
    qi = np.arange(128)[None, :]
    maps = []
    for c in range(NCORES):
        m = dict(sh)
        m["x_own"] = f(x.reshape(cfg.NB, 128, D)[c::NCORES].reshape(TL, D))
        m["pos_own"] = f(pos.reshape(cfg.NB, 128)[c::NCORES].reshape(1, TL))
        md = np.zeros((128, 8, 128), np.float32)
        for r in range(8):
            d = (c - r) * 128 + qi - ki
            md[:, r, :] = (d >= 0)
        m["mdiff"] = md.astype(bf)
        ml = np.zeros((128, 24, 128), np.float32)
        for r in range(24):
            d = (16 + c - r) * 128 + qi - ki
            ml[:, r, :] = _dil_mult(d)
        m["mdil"] = ml.astype(bf)
        maps.append(m)
    return maps


def assemble(results, cfg=None):
    cfg = cfg or Cfg()
    out = np.zeros((cfg.NB, 128, cfg.D), np.float32)
    for c in range(NCORES):
        out[c::NCORES] = np.asarray(results[c]["out"]).reshape(cfg.NBL, 128, cfg.D)
    return out.reshape(1, cfg.SEQ, cfg.D)


def kernel(**inputs):
    cfg = Cfg()
    nc = build(cfg)
    in_maps = make_in_maps(inputs, cfg)
    res = run_bass_kernel_spmd(nc, in_maps, core_ids=list(range(NCORES)))
    return assemble(res.results, cfg)
```
